# Optimizing a Trainium2 kernel written in Bass

```python
import math
import jax, jax.numpy as jnp
from jax import lax
import numpy as np

D_MODEL = 2048
BATCH = 8
SEQ = 2048
DEPTH = 2

CHUNK = 64
Q_BLOCK = 128
N_MEM = 256
EPS = 1e-6
NEG = -1e30

A_WIDTH = D_MODEL // 2
CONV_WIDTH = 31
MLA_HEADS = 8
MLA_NOPE = 128
MLA_ROPE = 64
MLA_V = 128
MLA_Q_RANK = 512
MLA_KV_RANK = 512
ROPE_THETA = 10000.0
EV_IN = 2 * A_WIDTH + MLA_Q_RANK + MLA_KV_RANK + MLA_ROPE
EV_OUT = A_WIDTH + MLA_HEADS * MLA_V
DIFF_HEADS = 8
DIFF_HD = D_MODEL // (2 * DIFF_HEADS)
OD_IN = 3 * D_MODEL
REL_BUCKETS = 32
REL_MAX_DIST = 128
CROSS_HEADS = 4
CROSS_HD = 128
D_FF = 5632
N_EXPERTS = 8
TOP_K = 2
D_FF_EXPERT = 7168
MOE_BLOCK = 128

kernel_name = "hybrid_conv_mla_diffattn_moe_encoder"


def rmsnorm(x, g):
    xf = x.astype(jnp.float32)
    y = xf * lax.rsqrt(jnp.mean(xf * xf, axis=-1, keepdims=True) + EPS)
    return (y * g.astype(jnp.float32)).astype(x.dtype)


def layernorm(x, g, b):
    xf = x.astype(jnp.float32)
    mu = jnp.mean(xf, axis=-1, keepdims=True)
    xc = xf - mu
    y = xc * lax.rsqrt(jnp.mean(xc * xc, axis=-1, keepdims=True) + EPS)
    return (y * g.astype(jnp.float32) + b.astype(jnp.float32)).astype(x.dtype)


def rope_tables(seq):
    pos = jnp.arange(seq, dtype=jnp.float32)
    inv = jnp.power(ROPE_THETA, -jnp.arange(0, MLA_ROPE, 2, dtype=jnp.float32) / MLA_ROPE)
    ang = pos[:, None] * inv[None, :]
    return jnp.cos(ang)[:, None, :], jnp.sin(ang)[:, None, :]


def rope(x, cos, sin):
    half = x.shape[-1] // 2
    c, s = cos.astype(x.dtype), sin.astype(x.dtype)
    x1, x2 = x[..., :half], x[..., half:]
    return jnp.concatenate([x1 * c - x2 * s, x1 * s + x2 * c], axis=-1)


def t5_bucket(rel):
    half = REL_BUCKETS // 2
    max_exact = half // 2
    ret = (rel > 0).astype(jnp.int32) * half
    n = jnp.abs(rel)
    nf = jnp.maximum(n, 1).astype(jnp.float32)
    large = max_exact + (jnp.log(nf / max_exact) / math.log(REL_MAX_DIST / max_exact)
                         * (half - max_exact)).astype(jnp.int32)
    large = jnp.minimum(large, half - 1)
    return ret + jnp.where(n < max_exact, n, large)


def chunk_causal_sweep(block_fn, seq):
    outs = []
    for qb in range(seq // Q_BLOCK):
        q0 = qb * Q_BLOCK
        kv_end = q0 + Q_BLOCK
        q_pos = jnp.arange(q0, kv_end, dtype=jnp.int32)
        k_pos = jnp.arange(kv_end, dtype=jnp.int32)
        mask = (k_pos[None, :] // CHUNK) <= (q_pos[:, None] // CHUNK)
        rel = k_pos[None, :] - q_pos[:, None]
        outs.append(block_fn(q0, kv_end, rel, mask))
    return jnp.concatenate(outs, axis=1)


def conformer_conv(val, gate, conv_w, conv_b, ln_g, ln_b):
    u = val * jax.nn.sigmoid(gate)
    u = lax.conv_general_dilated(
        u, conv_w[:, None, :], window_strides=(1,), padding=[(CONV_WIDTH - 1, 0)],
        dimension_numbers=("NWC", "WIO", "NWC"), feature_group_count=A_WIDTH) + conv_b
    return jax.nn.silu(layernorm(u, ln_g, ln_b))


def mla_attention(cq_in, ckv_in, kr_in, q_norm_g, w_uq, kv_norm_g, w_ukv, cos, sin):
    B, S, _ = cq_in.shape
    q = (rmsnorm(cq_in, q_norm_g) @ w_uq).reshape(B, S, MLA_HEADS, MLA_NOPE + MLA_ROPE)
    q_nope, q_rope = q[..., :MLA_NOPE], rope(q[..., MLA_NOPE:], cos, sin)
    kv = (rmsnorm(ckv_in, kv_norm_g) @ w_ukv).reshape(B, S, MLA_HEADS, MLA_NOPE + MLA_V)
    k_nope, v = kv[..., :MLA_NOPE], kv[..., MLA_NOPE:]
    k_rope = rope(kr_in[:, :, None, :], cos, sin)[:, :, 0, :]
    scale = (MLA_NOPE + MLA_ROPE) ** -0.5

    def block(q0, kv_end, rel, mask):
        s = (jnp.einsum("bqhd,bkhd->bhqk", q_nope[:, q0:kv_end], k_nope[:, :kv_end])
             + jnp.einsum("bqhr,bkr->bhqk", q_rope[:, q0:kv_end], k_rope[:, :kv_end]))
        p = jax.nn.softmax(jnp.where(mask, s.astype(jnp.float32) * scale, NEG), axis=-1)
        return jnp.einsum("bhqk,bkhd->bqhd", p.astype(v.dtype), v[:, :kv_end])

    o = chunk_causal_sweep(block, S)
    return o.reshape(B, S, MLA_HEADS * MLA_V)


def diff_attention(qkv, lq1, lk1, lq2, lk2, subln_g, rel_bias, lambda_init):
    B, S, _ = qkv.shape
    q = qkv[..., :D_MODEL].reshape(B, S, DIFF_HEADS, 2, DIFF_HD)
    k = qkv[..., D_MODEL:2 * D_MODEL].reshape(B, S, DIFF_HEADS, 2, DIFF_HD)
    v = qkv[..., 2 * D_MODEL:].reshape(B, S, DIFF_HEADS, 2 * DIFF_HD)
    f32 = jnp.float32
    lam = (jnp.exp(jnp.sum(lq1.astype(f32) * lk1.astype(f32)))
           - jnp.exp(jnp.sum(lq2.astype(f32) * lk2.astype(f32))) + lambda_init)
    scale = DIFF_HD ** -0.5

    def block(q0, kv_end, rel, mask):
        bias = jnp.moveaxis(rel_bias[t5_bucket(rel)], -1, 0).astype(f32)
        s = jnp.einsum("bqhcd,bkhcd->bchqk", q[:, q0:kv_end], k[:, :kv_end]).astype(f32) * scale + bias
        p = jax.nn.softmax(jnp.where(mask, s, NEG), axis=-1)
        w = p[:, 0] - lam * p[:, 1]
        return jnp.einsum("bhqk,bkhd->bqhd", w.astype(v.dtype), v[:, :kv_end])

    o = chunk_causal_sweep(block, S)
    o = rmsnorm(o, subln_g) * (1.0 - lambda_init)
    return o.reshape(B, S, DIFF_HEADS * 2 * DIFF_HD)


def memory_cross_attention(h, memn, wq, wkv, wo):
    B, S, _ = h.shape
    M = memn.shape[1]
    q = (h @ wq).reshape(B, S, CROSS_HEADS, CROSS_HD)
    kv = (memn @ wkv).reshape(B, M, 2, CROSS_HEADS, CROSS_HD)
    k, v = kv[:, :, 0], kv[:, :, 1]
    s = jnp.einsum("bqhd,bkhd->bhqk", q, k).astype(jnp.float32) * (CROSS_HD ** -0.5)
    p = jax.nn.softmax(s, axis=-1)
    o = jnp.einsum("bhqk,bkhd->bqhd", p.astype(v.dtype), v).reshape(B, S, CROSS_HEADS * CROSS_HD)
    return o @ wo


def swiglu(x, wg, wu, wd):
    return (jax.nn.silu(x @ wg) * (x @ wu)) @ wd


def moe_swiglu(x, w_router, wg, wu, wd):
    B, S, D = x.shape
    T = B * S
    A = T * TOP_K
    xf = x.reshape(T, D)
    logits = (xf @ w_router).astype(jnp.float32)
    top_v, top_e = lax.top_k(logits, TOP_K)
    gates = jax.nn.softmax(top_v, axis=-1)
    flat_e = top_e.reshape(A)
    flat_tok = jnp.repeat(jnp.arange(T, dtype=jnp.int32), TOP_K)
    flat_g = gates.reshape(A)
    order = jnp.argsort(flat_e)
    sorted_e = flat_e[order]
    counts = jnp.bincount(flat_e, length=N_EXPERTS)
    starts = jnp.cumsum(counts) - counts
    padded = ((counts + MOE_BLOCK - 1) // MOE_BLOCK) * MOE_BLOCK
    padded_ends = jnp.cumsum(padded)
    padded_starts = padded_ends - padded
    dest = padded_starts[sorted_e] + (jnp.arange(A, dtype=jnp.int32) - starts[sorted_e])
    P = A + N_EXPERTS * MOE_BLOCK
    n_blocks = P // MOE_BLOCK
    slot_tok = jnp.full((P,), T, jnp.int32).at[dest].set(flat_tok[order])
    slot_gate = jnp.zeros((P,), jnp.float32).at[dest].set(flat_g[order])
    block_e = jnp.minimum(
        jnp.searchsorted(padded_ends, jnp.arange(n_blocks, dtype=jnp.int32) * MOE_BLOCK, side="right"),
        N_EXPERTS - 1)
    x_pad = jnp.concatenate([xf, jnp.zeros((1, D), xf.dtype)], axis=0)
    xb = x_pad[slot_tok].reshape(n_blocks, MOE_BLOCK, D)

    def expert_block(args):
        xblk, e = args
        return swiglu(xblk, wg[e], wu[e], wd[e])

    yb = lax.map(expert_block, (xb, block_e)).reshape(P, D)
    y = jnp.zeros((T + 1, D), jnp.float32).at[slot_tok].add(yb.astype(jnp.float32) * slot_gate[:, None])[:T]
    return y.astype(x.dtype).reshape(B, S, D)


def setup_inputs(seed: int = 0) -> dict:
    key = jax.random.key(seed)
    keys = iter(jax.random.split(key, 48))
    f32 = jnp.float32
    NE = (DEPTH + 1) // 2
    NO = DEPTH // 2

    def w(shape, fan_in):
        return jax.random.normal(next(keys), shape, f32) * (fan_in ** -0.5)

    def gain(shape):
        return 1.0 + 0.02 * jax.random.normal(next(keys), shape, f32)

    def small(shape, scale):
        return scale * jax.random.normal(next(keys), shape, f32)

    return {
        "x": jax.random.normal(next(keys), (BATCH, SEQ, D_MODEL), f32),
        "mem": jax.random.normal(next(keys), (BATCH, N_MEM, D_MODEL), f32),
        "rel_bias": small((REL_BUCKETS, DIFF_HEADS), 0.2),
        "mem_norm_g": gain((D_MODEL,)),
        "norm_mix_g": gain((DEPTH, D_MODEL)),
        "norm_cross_g": gain((DEPTH, D_MODEL)),
        "norm_ffn_g": gain((DEPTH, D_MODEL)),
        "cross_wq": w((DEPTH, D_MODEL, CROSS_HEADS * CROSS_HD), D_MODEL),
        "cross_wkv": w((DEPTH, D_MODEL, 2 * CROSS_HEADS * CROSS_HD), D_MODEL),
        "cross_wo": w((DEPTH, CROSS_HEADS * CROSS_HD, D_MODEL), CROSS_HEADS * CROSS_HD),
        "ev_w_in": w((NE, D_MODEL, EV_IN), D_MODEL),
        "ev_conv_w": w((NE, CONV_WIDTH, A_WIDTH), CONV_WIDTH),
        "ev_conv_b": small((NE, A_WIDTH), 0.02),
        "ev_ln_g": gain((NE, A_WIDTH)),
        "ev_ln_b": small((NE, A_WIDTH), 0.02),
        "ev_q_norm_g": gain((NE, MLA_Q_RANK)),
        "ev_w_uq": w((NE, MLA_Q_RANK, MLA_HEADS * (MLA_NOPE + MLA_ROPE)), MLA_Q_RANK),
        "ev_kv_norm_g": gain((NE, MLA_KV_RANK)),
        "ev_w_ukv": w((NE, MLA_KV_RANK, MLA_HEADS * (MLA_NOPE + MLA_V)), MLA_KV_RANK),
        "ev_w_out": w((NE, EV_OUT, D_MODEL), EV_OUT),
        "ev_ffn_wg": w((NE, D_MODEL, D_FF), D_MODEL),
        "ev_ffn_wu": w((NE, D_MODEL, D_FF), D_MODEL),
        "ev_ffn_wd": w((NE, D_FF, D_MODEL), D_FF),
        "od_w_in": w((NO, D_MODEL, OD_IN), D_MODEL),
        "od_lambda_q1": small((NO, DIFF_HD), 0.1),
        "od_lambda_k1": small((NO, DIFF_HD), 0.1),
        "od_lambda_q2": small((NO, DIFF_HD), 0.1),
        "od_lambda_k2": small((NO, DIFF_HD), 0.1),
        "od_subln_g": gain((NO, 2 * DIFF_HD)),
        "od_w_out": w((NO, D_MODEL, D_MODEL), D_MODEL),
        "od_router": w((NO, D_MODEL, N_EXPERTS), D_MODEL),
        "od_moe_wg": w((NO, N_EXPERTS, D_MODEL, D_FF_EXPERT), D_MODEL),
        "od_moe_wu": w((NO, N_EXPERTS, D_MODEL, D_FF_EXPERT), D_MODEL),
        "od_moe_wd": w((NO, N_EXPERTS, D_FF_EXPERT, D_MODEL), D_FF_EXPERT),
        "final_norm_g": gain((D_MODEL,)),
    }


def reference(x, mem, rel_bias, mem_norm_g, norm_mix_g, norm_cross_g, norm_ffn_g,
              cross_wq, cross_wkv, cross_wo,
              ev_w_in, ev_conv_w, ev_conv_b, ev_ln_g, ev_ln_b, ev_q_norm_g, ev_w_uq,
              ev_kv_norm_g, ev_w_ukv, ev_w_out, ev_ffn_wg, ev_ffn_wu, ev_ffn_wd,
              od_w_in, od_lambda_q1, od_lambda_k1, od_lambda_q2, od_lambda_k2, od_subln_g,
              od_w_out, od_router, od_moe_wg, od_moe_wu, od_moe_wd, final_norm_g):
    S = x.shape[1]
    cos, sin = rope_tables(S)
    memn = rmsnorm(mem, mem_norm_g)
    h = x
    o1 = 2 * A_WIDTH
    o2 = o1 + MLA_Q_RANK
    o3 = o2 + MLA_KV_RANK
    for layer in range(DEPTH):
        i = layer // 2
        hn = rmsnorm(h, norm_mix_g[layer])
        if layer % 2 == 0:
            z = hn @ ev_w_in[i]
            a_out = conformer_conv(z[..., :A_WIDTH], z[..., A_WIDTH:o1],
                                   ev_conv_w[i], ev_conv_b[i], ev_ln_g[i], ev_ln_b[i])
            b_out = mla_attention(z[..., o1:o2], z[..., o2:o3], z[..., o3:],
                                  ev_q_norm_g[i], ev_w_uq[i], ev_kv_norm_g[i], ev_w_ukv[i], cos, sin)
            mix = jnp.concatenate([a_out, b_out], axis=-1) @ ev_w_out[i]
        else:
            lambda_init = 0.8 - 0.6 * math.exp(-0.3 * layer)
            mix = diff_attention(hn @ od_w_in[i], od_lambda_q1[i], od_lambda_k1[i],
                                 od_lambda_q2[i], od_lambda_k2[i], od_subln_g[i],
                                 rel_bias, lambda_init) @ od_w_out[i]
        h = h + mix
        h = h + memory_cross_attention(rmsnorm(h, norm_cross_g[layer]), memn,
                                       cross_wq[layer], cross_wkv[layer], cross_wo[layer])
        hn = rmsnorm(h, norm_ffn_g[layer])
        if layer % 2 == 0:
            h = h + swiglu(hn, ev_ffn_wg[i], ev_ffn_wu[i], ev_ffn_wd[i])
        else:
            h = h + moe_swiglu(hn, od_router[i], od_moe_wg[i], od_moe_wu[i], od_moe_wd[i])
    return rmsnorm(h, final_norm_g)
```

```python
import math
from contextlib import ExitStack
import numpy as np
import concourse.bass as bass
import concourse.mybir as mybir
from concourse.bass_utils import run_bass_kernel_spmd

F32, BF16 = mybir.dt.float32, mybir.dt.bfloat16
ALU = mybir.AluOpType
AF = mybir.ActivationFunctionType
AX = mybir.AxisListType

S = 2048
D = 2048
NT = S // 128
EPS = 1e-6
NEGM = -30000.0
DFF = 5632
DFE = 7168
NEXP = 8


class Buf:
    __slots__ = ("name", "writes", "reads")

    def __init__(self, name=""):
        self.name = name
        self.writes = {}
        self.reads = {}


def _merge(dst, src):
    for k, (s, v) in src.items():
        if k not in dst or dst[k][1] < v:
            dst[k] = (s, v)


class Prog:
    def __init__(self, nc, es, ndma=12):
        self.nc = nc
        self.eng = {"pe": nc.tensor, "act": nc.scalar, "dve": nc.vector, "pool": nc.gpsimd, "sp": nc.sync}
        self.sem = {}
        self.cnt = {}
        self.pending = {}
        self.waited = {e: {} for e in self.eng}
        for e in self.eng:
            self.sem[e] = es.enter_context(nc.semaphore("c_" + e))
            self.cnt[e] = 0
            self.pending[e] = False
        self.dsem = {"sp": [], "pool": []}
        self.dval = {}
        self.dnext = {"sp": 0, "pool": 0}
        for q in ("sp", "pool"):
            for i in range(ndma):
                key = f"d_{q}{i}"
                self.dsem[q].append((key, es.enter_context(nc.semaphore(key))))
                self.dval[key] = 0
        self.ninst = 0
        self.marks = []

    def _wait(self, e, toks):
        w = self.waited[e]
        for key, (sem, val) in toks.items():
            if val <= 0 or (e == "pe" and key == "pe"):
                continue
            if w.get(key, 0) >= val:
                continue
            if key in self.cnt:
                assert val <= self.cnt[key], f"wait on future signal {key} {val}>{self.cnt[key]}"
            self.eng[e].wait_ge(sem, val)
            w[key] = val

    def _deps(self, reads, writes, partial):
        toks = {}
        for b in reads:
            _merge(toks, b.writes)
        for b in writes:
            _merge(toks, b.reads)
            if not partial:
                _merge(toks, b.writes)
        return toks

    def _commit(self, key, tok, reads, writes, partial):
        for b in reads:
            b.reads[key] = tok
        for b in writes:
            if not partial:
                b.writes = {}
                b.reads = {}
            b.writes[key] = tok

    def op(self, e, fn, reads=(), writes=(), signal=True, partial=False):
        self._wait(e, self._deps(reads, writes, partial))
        ins = fn(self.eng[e])
        self.ninst += 1
        if signal:
            self.cnt[e] += 1
            assert self.cnt[e] < 60000, "semaphore count too large"
            ins.then_inc(self.sem[e], 1)
            val = self.cnt[e]
            self.pending[e] = False
        else:
            val = self.cnt[e] + 1
            self.pending[e] = True
        self._commit(e, (self.sem[e], val), reads, writes, partial)

    def dma(self, q, out, in_, reads=(), writes=(), partial=False):
        toks = self._deps(reads, writes, partial)
        pool = self.dsem[q]
        i = self.dnext[q]
        self.dnext[q] = (i + 1) % len(pool)
        key, sem = pool[i]
        prev = self.dval[key]
        toks[key] = (sem, prev)
        self._wait(q, toks)
        self.eng[q].dma_start(out=out, in_=in_).then_inc(sem, 16)
        self.ninst += 1
        self.dval[key] = prev + 16
        assert prev + 16 < 60000
        self._commit(key, (sem, prev + 16), reads, writes, partial)

    def cond_region(self, regs, cnt_ap, cnt_buf, thresh, body):
        for e in self.eng:
            assert not self.pending[e]
        toks = {}
        _merge(toks, cnt_buf.writes)
        for e in self.eng:
            self._wait(e, toks)
        self.nc.regs_load(regs, cnt_ap)
        before = dict(self.cnt)
        dbefore = dict(self.dval)
        dnext = dict(self.dnext)
        wsnap = {e: dict(w) for e, w in self.waited.items()}
        with self.nc.If_cmp(regs, thresh, "IS_GT"):
            body()
            for e in self.eng:
                assert not self.pending[e]
        after = dict(self.cnt)
        dafter = dict(self.dval)
        with self.nc.Else():
            for e in self.eng:
                if after[e] > before[e]:
                    if before[e] > 0:
                        self.eng[e].wait_ge(self.sem[e], before[e])
                    self.eng[e].sem_inc(self.sem[e], after[e] - before[e])
            for q in self.dsem:
                for key, sem in self.dsem[q]:
                    if dafter[key] > dbefore[key]:
                        if dbefore[key] > 0:
                            self.eng[q].wait_ge(sem, dbefore[key])
                        self.eng[q].sem_inc(sem, dafter[key] - dbefore[key])
        self.waited = wsnap

    def barrier(self):
        import traceback
        fr = traceback.extract_stack(limit=3)[0]
        self.marks.append((f"{fr.name}:{fr.lineno}", self.cnt["pe"]))
        toks = {}
        for e in self.eng:
            assert not self.pending[e], f"pending unsignaled op on {e}"
            if self.cnt[e] > 0:
                toks[e] = (self.sem[e], self.cnt[e])
        for q in self.dsem:
            for key, sem in self.dsem[q]:
                if self.dval[key] > 0:
                    toks[key] = (sem, self.dval[key])
        for e in self.eng:
            self._wait(e, toks)


class Ring:
    def __init__(self, items):
        self.items = items
        self.i = 0

    def next(self):
        it = self.items[self.i]
        self.i = (self.i + 1) % len(self.items)
        return it


class Ctx:
    pass


_uid = [0]
C_EPS = [None]


def sb(es, nc, name, shape, dt):
    _uid[0] += 1
    t = es.enter_context(nc.sbuf_tensor(f"{name}_{_uid[0]}", list(shape), dt))
    return t, Buf(name)


def sbring(es, nc, name, shape, dt, n):
    return Ring([sb(es, nc, f"{name}{i}", shape, dt) for i in range(n)])


def evac_copy(P, C, out_ap, in_ap, reads, writes, partial=True):
    C.flip ^= 1
    if C.flip:
        P.op("act", lambda e: e.copy(out_ap, in_ap), reads=reads, writes=writes, partial=partial)
    else:
        P.op("dve", lambda e: e.tensor_copy(out_ap, in_ap), reads=reads, writes=writes, partial=partial)


def rstd_from_ss(P, out_ap, in_ap, n, reads, writes):
    P.op("act", lambda e: e.activation(out_ap, in_ap, AF.Sqrt, bias=C_EPS[0][0:out_ap.shape[0], 0:1], scale=1.0 / n), reads=reads, writes=writes)
    P.op("dve", lambda e: e.reciprocal(out_ap, out_ap), reads=writes, writes=writes)


def bcast_row(P, C, row_dram, n, dst, dstb, mul=None):
    rt, rb = C.rowring.next()
    P.dma("sp", rt[0:1, 0:n], row_dram, writes=[rb])
    for c0 in range(0, n, 512):
        w = min(512, n - c0)
        ps, pb = C.psf.next()
        P.op("pe", lambda e: e.matmul(ps[:, 0:w], C.ones_f[0:1, 0:128], rt[0:1, c0:c0 + w], start=True, stop=True),
             reads=[rb, C.constb], writes=[pb])
        if mul is None:
            evac_copy(P, C, dst[:, c0:c0 + w], ps[:, 0:w], [pb], [dstb])
        else:
            P.op("dve", lambda e: e.tensor_scalar(dst[:, c0:c0 + w], ps[:, 0:w], mul, None, ALU.mult),
                 reads=[pb], writes=[dstb], partial=True)


def norm_T(P, C, src, T, grow_dram, dstT, dstTb, f32_cb=None):
    gb_t, gb_b = C.gbc
    bcast_row(P, C, grow_dram, D, gb_t, gb_b)
    for tt in range(T // 128):
        xt, xb = C.xring.next()
        P.dma("sp", xt[:, :], src[tt * 128:(tt + 1) * 128, :], writes=[xb])
        ss, ssb = C.ssring.next()
        xs, xsb = C.xsring.next()
        P.op("act", lambda e: e.activation(xs[:, :], xt[:, :], AF.Square, accum_out=ss[:, 0:1]),
             reads=[xb], writes=[xsb, ssb])
        rstd_from_ss(P, ss[:, 1:2], ss[:, 0:1], D, [ssb], [ssb])
        P.op("dve", lambda e: e.scalar_tensor_tensor(xs[:, :], xt[:, :], ss[:, 1:2], gb_t[:, :], ALU.mult, ALU.mult),
             reads=[xb, ssb, gb_b], writes=[xsb])
        if f32_cb is not None:
            f32_cb(tt, xs, xsb)
        for g4 in range(4):
            ps, pb = C.psf.next()
            for j in range(4):
                kc = g4 * 4 + j
                P.op("pe", lambda e: e.transpose(ps[:, j * 128:(j + 1) * 128], xs[:, kc * 128:(kc + 1) * 128], C.ident_f[:, :]),
                     reads=[xsb, C.constb], writes=[pb], partial=True, signal=(j == 3))
            evac_copy(P, C, dstT[:, g4 * 4:(g4 + 1) * 4, tt * 128:(tt + 1) * 128],
                      ps[:, :].rearrange("p (a b) -> p a b", a=4), [pb], [dstTb])


def load_panel(P, C, wdram, k0, K, c0, w, ring):
    wt, wb = ring.next()
    KC = K // 128
    P.dma("pool", wt[:, 0:KC, 0:w], wdram[k0:k0 + K, c0:c0 + w].rearrange("(kc p) n -> p kc n", p=128), writes=[wb])
    return wt, wb


def proj_fm(P, C, xT, xTb, KC, T, wlist, ncols, M, evac, panel_ring):
    PW = 512
    for p0 in range(0, ncols, PW):
        pw = min(PW, ncols - p0)
        pans = [load_panel(P, C, w, 0, KC * 128, p0, pw, panel_ring) for w in wlist]
        for m0 in range(0, pw, M):
            ci = (p0 + m0) // M
            for t0 in range(0, T, 512):
                tw = min(512, T - t0)
                pss = []
                for (wt, wb) in pans:
                    ps, pb = C.psf.next()
                    for kc in range(KC):
                        P.op("pe", lambda e: e.matmul(ps[0:M, 0:tw], wt[:, kc, m0:m0 + M], xT[:, kc, t0:t0 + tw],
                                                      start=(kc == 0), stop=(kc == KC - 1)),
                             reads=[wb, xTb], writes=[pb], partial=True, signal=(kc == KC - 1))
                    pss.append((ps, pb))
                evac(ci, t0, tw, pss)


def proj_tm(P, C, xT, xTb, KC, T, wdram, ncols, evac, panel_ring, col_of=None):
    for p0 in range(0, ncols, 512):
        pw = min(512, ncols - p0)
        wt, wb = load_panel(P, C, wdram, 0, KC * 128, p0, pw, panel_ring)
        for tt in range(T // 128):
            ps, pb = C.psf.next()
            for kc in range(KC):
                P.op("pe", lambda e: e.matmul(ps[:, 0:pw], xT[:, kc, tt * 128:(tt + 1) * 128], wt[:, kc, 0:pw],
                                              start=(kc == 0), stop=(kc == KC - 1)),
                     reads=[wb, xTb], writes=[pb], partial=True, signal=(kc == KC - 1))
            evac(tt, p0, pw, ps, pb)


def store_fm_bf16(P, C, dst_dram, row0):
    def ev(ci, t0, tw, pss, M=128):
        ps, pb = pss[0]
        st, stb = C.stbf.next()
        evac_copy(P, C, st[0:M, 0:tw], ps[0:M, 0:tw], [pb], [stb], partial=False)
        P.dma("sp", dst_dram[row0 + ci * M: row0 + (ci + 1) * M, t0:t0 + tw], st[0:M, 0:tw], reads=[stb])
    return ev


def residual_linear(P, C, xT, xTb, KC, wdram, h):
    def ev(tt, p0, pw, ps, pb):
        ht, hb = C.hpiece.next()
        P.dma("sp", ht[:, 0:pw], h[tt * 128:(tt + 1) * 128, p0:p0 + pw], writes=[hb])
        P.op("dve", lambda e: e.tensor_tensor(ht[:, 0:pw], ps[:, 0:pw], ht[:, 0:pw], ALU.add), reads=[pb, hb], writes=[hb])
        P.dma("sp", h[tt * 128:(tt + 1) * 128, p0:p0 + pw], ht[:, 0:pw], reads=[hb])
    proj_tm(P, C, xT, xTb, KC, S, wdram, D, ev, C.panel)


def attention_gen(P, C, qt, L, s_terms, v_of, dv, scale, out_cb, opnd_bufs):
    chunks = [(c0, min(512, L - c0)) for c0 in range(0, L, 512)]
    st_t, st_b = C.stat.next()

    def scores(c0, n):
        ps, pb = C.psf.next()
        terms = s_terms(qt, c0, n)
        for i, (lhsT, rhs, off, w) in enumerate(terms):
            P.op("pe", lambda e: e.matmul(ps[:, off:off + w], lhsT, rhs, start=(i == 0), stop=(i == len(terms) - 1),
                                          skip_group_check=True),
                 reads=opnd_bufs, writes=[pb], partial=True, signal=(i == len(terms) - 1))
        return ps, pb

    for ci, (c0, n) in enumerate(chunks):
        ps, pb = scores(c0, n)
        P.op("dve", lambda e: e.reduce_max(st_t[:, ci:ci + 1], ps[:, 0:n], AX.X), reads=[pb], writes=[st_b], partial=True)
        yield
    if len(chunks) > 1:
        P.op("dve", lambda e: e.reduce_max(st_t[:, 4:5], st_t[:, 0:len(chunks)], AX.X), reads=[st_b], writes=[st_b], partial=True)
        mcol = st_t[:, 4:5]
    else:
        mcol = st_t[:, 0:1]
    P.op("dve", lambda e: e.tensor_scalar(st_t[:, 5:6], mcol, -scale, None, ALU.mult), reads=[st_b], writes=[st_b], partial=True)
    o_ps, o_pb = C.pso_free.pop()
    nblk = L // 128
    for ci, (c0, n) in enumerate(chunks):
        ps, pb = scores(c0, n)
        et, eb = C.ering.next()
        P.op("act", lambda e: e.activation(et[:, 0:n], ps[:, 0:n], AF.Exp, bias=st_t[:, 5:6], scale=scale,
                                           accum_out=st_t[:, 6 + ci:7 + ci]),
             reads=[pb, st_b], writes=[eb, st_b], partial=True)
        yield
        tp, tpb = C.psb.next()
        nb = n // 128
        for j in range(nb):
            P.op("pe", lambda e: e.transpose(tp[:, j * 128:(j + 1) * 128], et[:, j * 128:(j + 1) * 128], C.ident_b[:, :]),
                 reads=[eb, C.constb], writes=[tpb], partial=True, signal=(j == nb - 1))
        pt, ptb = C.ptring.next()
        P.op("dve", lambda e: e.tensor_copy(pt[:, 0:n], tp[:, 0:n]), reads=[tpb], writes=[ptb])
        yield
        for j in range(nb):
            kb = c0 // 128 + j
            P.op("pe", lambda e: e.matmul(o_ps[:, 0:dv], pt[:, j * 128:(j + 1) * 128], v_of(kb),
                                          start=(kb == 0), stop=(kb == nblk - 1), skip_group_check=True),
                 reads=[ptb] + opnd_bufs, writes=[o_pb], partial=True, signal=(j == nb - 1))
        yield
    nc_ = len(chunks)
    if nc_ > 1:
        P.op("dve", lambda e: e.reduce_sum(st_t[:, 10:11], st_t[:, 6:6 + nc_], AX.X), reads=[st_b], writes=[st_b], partial=True)
        scol = st_t[:, 10:11]
    else:
        scol = st_t[:, 6:7]
    P.op("dve", lambda e: e.reciprocal(st_t[:, 11:12], scol), reads=[st_b], writes=[st_b], partial=True)
    out_cb(qt, o_ps, o_pb, st_t[:, 11:12], st_b)
    C.pso_free.append((o_ps, o_pb))


def run_gens(gens, width):
    active = []
    it = iter(gens)
    done = False
    while True:
        while not done and len(active) < width:
            g = next(it, None)
            if g is None:
                done = True
                break
            active.append(g)
        if not active:
            break
        for g in list(active):
            try:
                next(g)
            except StopIteration:
                active.remove(g)


class AttnPools:
    def __init__(self, C):
        self.C = C

    def __enter__(self):
        C = self.C
        self.saved = C.psf
        items = C.psf.items
        C.psf = Ring(items[0:4])
        C.pso_free = [items[4], items[5]]

    def __exit__(self, *a):
        self.C.psf = self.saved
        return False


def attention(P, C, nq, L_of, s_terms, v_of, dv, scale, out_cb, opnd_bufs, width=2):
    run_gens((attention_gen(P, C, qt, L_of(qt), s_terms, v_of, dv, scale, out_cb, opnd_bufs) for qt in range(nq)), width)


def out_T_store(P, C, src, srcb, ncol, dst_dram, row0, qt):
    for c0 in range(0, ncol, 128):
        ps, pb = C.psf.next()
        P.op("pe", lambda e: e.transpose(ps[:, 0:128], src[:, c0:c0 + 128], C.ident_f[:, :]), reads=[srcb, C.constb], writes=[pb])
        st, stb = C.stbf.next()
        evac_copy(P, C, st[:, 0:128], ps[:, 0:128], [pb], [stb], partial=False)
        P.dma("sp", dst_dram[row0 + c0: row0 + c0 + 128, qt * 128:(qt + 1) * 128], st[:, 0:128], reads=[stb])


def load_fm(P, C, dst, dstb, src_dram, row0, nch, T=S):
    for c in range(nch):
        P.dma("sp", dst[:, c, 0:T], src_dram[row0 + c * 128: row0 + (c + 1) * 128, 0:T], writes=[dstb], partial=True)


def cross_attention_layer(P, C, nc, A, layer):
    with ExitStack() as ph:
        qT, qTb = sb(ph, nc, "ca_qT", [128, 4, S], BF16)
        with ExitStack() as ph1:
            hnT, hnTb = sb(ph1, nc, "ca_hnT", [128, 16, S], BF16)
            norm_T(P, C, A.h, S, A.norm_cross_g[layer:layer + 1, :], hnT, hnTb)

            def evq(ci, t0, tw, pss):
                ps, pb = pss[0]
                evac_copy(P, C, qT[:, ci, t0:t0 + tw], ps[:, 0:tw], [pb], [qTb])
            proj_fm(P, C, hnT, hnTb, 16, S, [A.cross_wq[layer]], 512, 128, evq, C.panel)
            P.barrier()
        ocT, ocTb = sb(ph, nc, "ca_ocT", [128, 4, S], BF16)
        osb = sbring(ph, nc, "ca_o", [128, 128], F32, 2)
        sc = 128 ** -0.5
        for hh in range(4):
            def s_terms(qt, c0, n):
                return [(qT[:, hh, qt * 128:(qt + 1) * 128], C.cKT[:, layer, hh, c0:c0 + n], 0, n)]

            def v_of(kb):
                return C.cV[:, layer, kb, hh * 128:(hh + 1) * 128]

            def out_cb(qt, o_ps, o_pb, rinv, rb):
                ot, ob = osb.next()
                P.op("dve", lambda e: e.tensor_scalar(ot[:, :], o_ps[:, 0:128], rinv, None, ALU.mult), reads=[o_pb, rb], writes=[ob])
                ps, pb = C.psf.next()
                P.op("pe", lambda e: e.transpose(ps[:, 0:128], ot[:, :], C.ident_f[:, :]), reads=[ob, C.constb], writes=[pb])
                evac_copy(P, C, ocT[:, hh, qt * 128:(qt + 1) * 128], ps[:, 0:128], [pb], [ocTb])
            with AttnPools(C):
                attention(P, C, NT, lambda qt: 256, s_terms, v_of, 128, sc, out_cb, [qTb, C.cKVb])
        residual_linear(P, C, ocT, ocTb, 4, A.cross_wo[layer], A.h)
        P.barrier()


def ffn_dense(P, C, nc, A, g_row, experts, F, router=None, final=None):
    TB = 512
    NH = 2
    FCh = F // 128 // NH
    assert FCh * NH * 128 == F and FCh % 2 == 0
    with ExitStack() as ph:
        hnT, hnTb = sb(ph, nc, "f_hnT", [128, 16, TB], BF16)
        hT, hTb = sb(ph, nc, "f_hT", [128, FCh, TB], BF16)
        yacc, yb = sb(ph, nc, "f_yacc", [128, 4, D], F32)
        wa = sbring(ph, nc, "f_wa", [128, 16, 256], BF16, 4)
        wbr = sbring(ph, nc, "f_wb", [128, FCh, 128], BF16, 2)
        sg = sbring(ph, nc, "f_sg", [128, TB], F32, 2)
        gates, gb = sb(ph, nc, "f_gates", [128, 4, 8], F32)
        rt, rtb = sb(ph, nc, "f_rt", [128, 32], F32)
        wr, wrb = sb(ph, nc, "f_wr", [128, 16, 8], F32)
        xsT, xsTb = sb(ph, nc, "f_xsT", [128, 16, 128], F32)
        if router is not None:
            P.dma("sp", wr[:, :, :], router.rearrange("(kc p) e -> p kc e", p=128), writes=[wrb])

        def f32_cb(tt, xs, xsb):
            if router is None:
                return
            for g4 in range(4):
                ps, pb = C.psf.next()
                for j in range(4):
                    kc = g4 * 4 + j
                    P.op("pe", lambda e: e.transpose(ps[:, j * 128:(j + 1) * 128], xs[:, kc * 128:(kc + 1) * 128], C.ident_f[:, :]),
                         reads=[xsb, C.constb], writes=[pb], partial=True, signal=(j == 3))
                P.op("dve", lambda e: e.tensor_copy(xsT[:, g4 * 4:(g4 + 1) * 4, :], ps[:, :].rearrange("p (a b) -> p a b", a=4)),
                     reads=[pb], writes=[xsTb], partial=True)
            ps, pb = C.psf.next()
            for kc in range(16):
                P.op("pe", lambda e: e.matmul(ps[:, 0:8], xsT[:, kc, :], wr[:, kc, :], start=(kc == 0), stop=(kc == 15)),
                     reads=[xsTb, wrb], writes=[pb], partial=True, signal=(kc == 15))
            lg = rt[:, 0:8]
            P.op("dve", lambda e: e.tensor_copy(lg, ps[:, 0:8]), reads=[pb], writes=[rtb])
            P.op("dve", lambda e: e.max(rt[:, 8:16], lg), reads=[rtb], writes=[rtb])
            P.op("dve", lambda e: e.tensor_scalar(rt[:, 16:17], rt[:, 8:9], -1.0, None, ALU.mult), reads=[rtb], writes=[rtb])
            P.op("act", lambda e: e.activation(rt[:, 24:32], lg, AF.Exp, bias=rt[:, 16:17], scale=1.0), reads=[rtb], writes=[rtb])
            P.op("dve", lambda e: e.tensor_scalar(rt[:, 0:8], lg, rt[:, 9:10], None, ALU.is_ge), reads=[rtb], writes=[rtb])
            P.op("dve", lambda e: e.tensor_tensor(rt[:, 24:32], rt[:, 24:32], rt[:, 0:8], ALU.mult), reads=[rtb], writes=[rtb])
            P.op("dve", lambda e: e.reduce_sum(rt[:, 17:18], rt[:, 24:32], AX.X), reads=[rtb], writes=[rtb])
            P.op("dve", lambda e: e.reciprocal(rt[:, 18:19], rt[:, 17:18]), reads=[rtb], writes=[rtb])
            P.op("dve", lambda e: e.tensor_scalar(gates[:, tt, :], rt[:, 24:32], rt[:, 18:19], None, ALU.mult),
                 reads=[rtb], writes=[gb], partial=True)

        for blk in range(S // TB):
            t0 = blk * TB
            norm_T(P, C, A.h[t0:t0 + TB, :], TB, g_row, hnT, hnTb, f32_cb=f32_cb)
            first = True
            for ei, (wg, wu, wd) in enumerate(experts):
                for half in range(NH):
                    f0 = half * FCh * 128
                    for fp in range(FCh // 2):
                        gt, gbuf = load_panel(P, C, wg, 0, D, f0 + fp * 256, 256, wa)
                        ut, ubuf = load_panel(P, C, wu, 0, D, f0 + fp * 256, 256, wa)
                        for sub in range(2):
                            fc = fp * 2 + sub
                            psg, pgb = C.psf.next()
                            psu, pub = C.psf.next()
                            for (wt_, wb_, ps_, pb_) in ((gt, gbuf, psg, pgb), (ut, ubuf, psu, pub)):
                                for kc in range(16):
                                    P.op("pe", lambda e: e.matmul(ps_[:, 0:TB], wt_[:, kc, sub * 128:(sub + 1) * 128], hnT[:, kc, :],
                                                                  start=(kc == 0), stop=(kc == 15)),
                                         reads=[wb_, hnTb], writes=[pb_], partial=True, signal=(kc == 15))
                            st, stb = sg.next()
                            P.op("act", lambda e: e.activation(st[:, :], psg[:, 0:TB], AF.Silu), reads=[pgb], writes=[stb])
                            P.op("dve", lambda e: e.tensor_tensor(hT[:, fc, :], st[:, :], psu[:, 0:TB], ALU.mult),
                                 reads=[stb, pub], writes=[hTb], partial=True)
                    for npan in range(D // 128):
                        wt, wb = wbr.next()
                        P.dma("pool", wt[:, :, :],
                              wd[f0:f0 + FCh * 128, npan * 128:(npan + 1) * 128].rearrange("(fc p) n -> p fc n", p=128), writes=[wb])
                        for tt in range(4):
                            ps, pb = C.psf.next()
                            for fc in range(FCh):
                                P.op("pe", lambda e: e.matmul(ps[:, 0:128], hT[:, fc, tt * 128:(tt + 1) * 128], wt[:, fc, :],
                                                              start=(fc == 0), stop=(fc == FCh - 1)),
                                     reads=[wb, hTb], writes=[pb], partial=True, signal=(fc == FCh - 1))
                            ya = yacc[:, tt, npan * 128:(npan + 1) * 128]
                            if router is None:
                                if first:
                                    evac_copy(P, C, ya, ps[:, 0:128], [pb], [yb])
                                else:
                                    P.op("dve", lambda e: e.tensor_tensor(ya, ps[:, 0:128], ya, ALU.add), reads=[pb, yb], writes=[yb], partial=True)
                            elif first:
                                P.op("dve", lambda e: e.tensor_scalar(ya, ps[:, 0:128], gates[:, tt, ei:ei + 1], None, ALU.mult),
                                     reads=[pb, gb], writes=[yb], partial=True)
                            else:
                                P.op("dve", lambda e: e.scalar_tensor_tensor(ya, ps[:, 0:128], gates[:, tt, ei:ei + 1], ya, ALU.mult, ALU.add),
                                     reads=[pb, gb, yb], writes=[yb], partial=True)
                    first = False
            if final is not None:
                bcast_row(P, C, final[2], D, C.gbc[0], C.gbc[1])
            for tt in range(4):
                xt, xb = C.xring.next()
                r0 = t0 + tt * 128
                P.dma("sp", xt[:, :], A.h[r0:r0 + 128, :], writes=[xb])
                P.op("dve", lambda e: e.tensor_tensor(xt[:, :], xt[:, :], yacc[:, tt, :], ALU.add), reads=[xb, yb], writes=[xb])
                if final is None:
                    P.dma("sp", A.h[r0:r0 + 128, :], xt[:, :], reads=[xb])
                else:
                    fgb_t, fgb_b = C.gbc
                    out = final[0]
                    ss, ssb = C.ssring.next()
                    xs, xsb = C.xsring.next()
                    P.op("act", lambda e: e.activation(xs[:, :], xt[:, :], AF.Square, accum_out=ss[:, 0:1]), reads=[xb], writes=[xsb, ssb])
                    rstd_from_ss(P, ss[:, 1:2], ss[:, 0:1], D, [ssb], [ssb])
                    P.op("dve", lambda e: e.scalar_tensor_tensor(xs[:, :], xt[:, :], ss[:, 1:2], fgb_t[:, :], ALU.mult, ALU.mult),
                         reads=[xb, ssb, fgb_b], writes=[xsb])
                    P.dma("sp", out[r0:r0 + 128, :], xs[:, :], reads=[xsb])
        P.barrier()


def moe_sparse(P, C, nc, A, experts, regs):
    TB = 512
    F = DFE
    FC = F // 128
    I32 = mybir.dt.int32
    with ExitStack() as mo:
        gates, gb = sb(mo, nc, "m_gates", [128, 16, 8], F32)
        rkm, rkb = sb(mo, nc, "m_rkm", [128, 16, 8], F32)
        cnt_i, cib = sb(mo, nc, "m_cnti", [1, 8], I32)
        iota, iob = sb(mo, nc, "m_iota", [128, 512], F32)
        P.dma("sp", iota[:, :], A.c_iota, writes=[iob])
        with ExitStack() as ph:
            gbt, gbb = sb(ph, nc, "m_gb", [128, D], F32)
            C.gbc = (gbt, gbb)
            C.rowring = sbring(ph, nc, "m_row", [1, D], F32, 1)
            xring = sbring(ph, nc, "m_xt", [128, D], F32, 2)
            xsr = sbring(ph, nc, "m_xs", [128, D], F32, 1)
            ssr = sbring(ph, nc, "m_ss", [128, 2], F32, 4)
            hbr = sbring(ph, nc, "m_hb", [128, D], BF16, 2)
            xsT, xsTb = sb(ph, nc, "m_xsT", [128, 16, 128], F32)
            wr, wrb = sb(ph, nc, "m_wr", [128, 16, 8], F32)
            rt, rtb = sb(ph, nc, "m_rt", [128, 32], F32)
            sel, selb_ = sb(ph, nc, "m_sel", [128, 16, 8], F32)
            selh, selhb = sb(ph, nc, "m_selh", [128, 16, 8], BF16)
            utri, utb = sb(ph, nc, "m_utri", [128, 128], BF16)
            onesb, onb = sb(ph, nc, "m_onesb", [128, 128], BF16)
            cntf, cfb = sb(ph, nc, "m_cntf", [128, 8], F32)
            P.dma("pool", utri[:, :], A.c_utri, writes=[utb])
            P.op("dve", lambda e: e.memset(onesb[:, :], 1.0), writes=[onb])
            P.dma("sp", wr[:, :, :], A.od_router.rearrange("(kc p) e -> p kc e", p=128), writes=[wrb])
            bcast_row(P, C, A.norm_ffn_g[1:2, :], D, gbt, gbb)
            for tt in range(NT):
                xt, xb = xring.next()
                P.dma("sp", xt[:, :], A.h[tt * 128:(tt + 1) * 128, :], writes=[xb])
                ss, ssb = ssr.next()
                xs, xsb = xsr.next()
                P.op("act", lambda e: e.activation(xs[:, :], xt[:, :], AF.Square, accum_out=ss[:, 0:1]), reads=[xb], writes=[xsb, ssb])
                rstd_from_ss(P, ss[:, 1:2], ss[:, 0:1], D, [ssb], [ssb])
                P.op("dve", lambda e: e.scalar_tensor_tensor(xs[:, :], xt[:, :], ss[:, 1:2], gbt[:, :], ALU.mult, ALU.mult),
                     reads=[xb, ssb, gbb], writes=[xsb])
                hb_t, hb_b = hbr.next()
                P.op("act", lambda e: e.copy(hb_t[:, :], xs[:, :]), reads=[xsb], writes=[hb_b])
                P.dma("sp", A.hn_tm[tt * 128:(tt + 1) * 128, :], hb_t[:, :], reads=[hb_b])
                for g4 in range(4):
                    ps, pb = C.psf.next()
                    for j in range(4):
                        kc = g4 * 4 + j
                        P.op("pe", lambda e: e.transpose(ps[:, j * 128:(j + 1) * 128], xs[:, kc * 128:(kc + 1) * 128], C.ident_f[:, :]),
                             reads=[xsb, C.constb], writes=[pb], partial=True, signal=(j == 3))
                    P.op("dve", lambda e: e.tensor_copy(xsT[:, g4 * 4:(g4 + 1) * 4, :], ps[:, :].rearrange("p (a b) -> p a b", a=4)),
                         reads=[pb], writes=[xsTb], partial=True)
                ps, pb = C.psf.next()
                for kc in range(16):
                    P.op("pe", lambda e: e.matmul(ps[:, 0:8], xsT[:, kc, :], wr[:, kc, :], start=(kc == 0), stop=(kc == 15)),
                         reads=[xsTb, wrb], writes=[pb], partial=True, signal=(kc == 15))
                lg = rt[:, 0:8]
                P.op("dve", lambda e: e.tensor_copy(lg, ps[:, 0:8]), reads=[pb], writes=[rtb])
                P.op("dve", lambda e: e.max(rt[:, 8:16], lg), reads=[rtb], writes=[rtb])
                P.op("dve", lambda e: e.tensor_scalar(rt[:, 16:17], rt[:, 8:9], -1.0, None, ALU.mult), reads=[rtb], writes=[rtb])
                P.op("act", lambda e: e.activation(rt[:, 24:32], lg, AF.Exp, bias=rt[:, 16:17], scale=1.0), reads=[rtb], writes=[rtb])
                P.op("dve", lambda e: e.tensor_scalar(sel[:, tt, :], lg, rt[:, 9:10], None, ALU.is_ge), reads=[rtb], writes=[selb_], partial=True)
                P.op("dve", lambda e: e.tensor_copy(selh[:, tt, :], sel[:, tt, :]), reads=[selb_], writes=[selhb], partial=True)
                P.op("dve", lambda e: e.tensor_tensor(rt[:, 24:32], rt[:, 24:32], sel[:, tt, :], ALU.mult), reads=[rtb, selb_], writes=[rtb])
                P.op("dve", lambda e: e.reduce_sum(rt[:, 17:18], rt[:, 24:32], AX.X), reads=[rtb], writes=[rtb])
                P.op("dve", lambda e: e.reciprocal(rt[:, 18:19], rt[:, 17:18]), reads=[rtb], writes=[rtb])
                P.op("dve", lambda e: e.tensor_scalar(gates[:, tt, :], rt[:, 24:32], rt[:, 18:19], None, ALU.mult),
                     reads=[rtb], writes=[gb], partial=True)
            for tt in range(NT):
                ps, pb = C.psf.next()
                for t2 in range(tt + 1):
                    lhs = utri if t2 == tt else onesb
                    P.op("pe", lambda e: e.matmul(ps[:, 0:8], lhs[:, :], selh[:, t2, :], start=(t2 == 0), stop=(t2 == tt)),
                         reads=[selhb, utb, onb], writes=[pb], partial=True, signal=(t2 == tt))
                P.op("dve", lambda e: e.scalar_tensor_tensor(rkm[:, tt, :], ps[:, 0:8], 1.0, sel[:, tt, :], ALU.add, ALU.mult),
                     reads=[pb, selb_], writes=[rkb], partial=True)
            P.op("dve", lambda e: e.tensor_scalar(rkm[:, :, :], rkm[:, :, :], -1.0, None, ALU.add), reads=[rkb], writes=[rkb])
            ps, pb = C.psf.next()
            for t2 in range(NT):
                P.op("pe", lambda e: e.matmul(ps[:, 0:8], onesb[:, :], selh[:, t2, :], start=(t2 == 0), stop=(t2 == NT - 1)),
                     reads=[selhb, onb], writes=[pb], partial=True, signal=(t2 == NT - 1))
            P.op("dve", lambda e: e.tensor_copy(cntf[:, :], ps[:, 0:8]), reads=[pb], writes=[cfb])
            P.op("dve", lambda e: e.tensor_copy(cnt_i[0:1, :], cntf[0:1, :]), reads=[cfb], writes=[cib])
            P.barrier()
        with ExitStack() as ph:
            OH, ohb = sb(ph, nc, "m_OH", [128, 16, TB], BF16)
            OHT, ohtb = sb(ph, nc, "m_OHT", [128, 4, S], BF16)
            hnT, hnTb = sb(ph, nc, "m_hnT", [128, 16, TB], BF16)
            hT, hTb = sb(ph, nc, "m_hT", [128, FC, TB], BF16)
            ysl, yslb = sb(ph, nc, "m_ysl", [128, 4, D], BF16)
            wa = sbring(ph, nc, "m_wa", [128, 16, 256], BF16, 4)
            wbr = sbring(ph, nc, "m_wb", [128, FC, 128], BF16, 2)
            sg = sbring(ph, nc, "m_sg", [128, TB], F32, 2)
            hnr = sbring(ph, nc, "m_hnr", [128, 512], BF16, 3)
            hn32 = hnT[:, :, :].bitcast(F32)
            rmw = Ring([(hn32[:, 2 * i:2 * i + 2, :], Buf(f"rmw{i}")) for i in range(8)])
            rmw_bufs = [b for (_, b) in rmw.items]
            rka, rkab = sb(ph, nc, "m_rka", [128, 16], F32)
            hdram = [Buf(f"hrow{tt}") for tt in range(NT)]
            for ei, (wg, wu, wd) in enumerate(experts):
                for g in range(S // TB):
                    def body(ei=ei, g=g, wg=wg, wu=wu, wd=wd):
                        P.op("dve", lambda e: e.tensor_scalar(rka[:, :], rkm[:, :, ei], float(-g * TB), None, ALU.add), reads=[rkb], writes=[rkab])
                        for tt in range(NT):
                            P.op("dve", lambda e: e.tensor_scalar(OH[:, tt, :], iota[:, :], rka[:, tt:tt + 1], None, ALU.is_equal),
                                 reads=[iob, rkab], writes=[ohb], partial=True)
                        for q in range(4):
                            acc = [C.psf.next() for _ in range(4)]
                            for tt in range(NT):
                                ht_, hb_ = hnr.next()
                                P.dma("sp", ht_[:, :], A.hn_tm[tt * 128:(tt + 1) * 128, q * 512:(q + 1) * 512], writes=[hb_])
                                for j in range(4):
                                    P.op("pe", lambda e: e.matmul(acc[j][0][:, 0:TB], ht_[:, j * 128:(j + 1) * 128], OH[:, tt, :],
                                                                  start=(tt == 0), stop=(tt == NT - 1)),
                                         reads=[hb_, ohb], writes=[acc[j][1]], partial=True, signal=(j == 3))
                            for j in range(4):
                                evac_copy(P, C, hnT[:, q * 4 + j, :], acc[j][0][:, 0:TB], [acc[j][1]], [hnTb] + rmw_bufs)
                        for fp in range(FC // 2):
                            gt, gbuf = load_panel(P, C, wg, 0, D, fp * 256, 256, wa)
                            ut, ubuf = load_panel(P, C, wu, 0, D, fp * 256, 256, wa)
                            for sub in range(2):
                                fc = fp * 2 + sub
                                psg, pgb = C.psf.next()
                                psu, pub = C.psf.next()
                                for (wt_, wb_, ps_, pb_) in ((gt, gbuf, psg, pgb), (ut, ubuf, psu, pub)):
                                    for kc in range(16):
                                        P.op("pe", lambda e: e.matmul(ps_[:, 0:TB], wt_[:, kc, sub * 128:(sub + 1) * 128], hnT[:, kc, :],
                                                                      start=(kc == 0), stop=(kc == 15)),
                                             reads=[wb_, hnTb], writes=[pb_], partial=True, signal=(kc == 15))
                                st, stb = sg.next()
                                P.op("act", lambda e: e.activation(st[:, :], psg[:, 0:TB], AF.Silu), reads=[pgb], writes=[stb])
                                P.op("dve", lambda e: e.tensor_tensor(hT[:, fc, :], st[:, :], psu[:, 0:TB], ALU.mult),
                                     reads=[stb, pub], writes=[hTb], partial=True)
                        for npan in range(D // 128):
                            wt, wb = wbr.next()
                            P.dma("pool", wt[:, :, :], wd[:, npan * 128:(npan + 1) * 128].rearrange("(fc p) n -> p fc n", p=128), writes=[wb])
                            for blk in range(4):
                                ps, pb = C.psf.next()
                                for fc in range(FC):
                                    P.op("pe", lambda e: e.matmul(ps[:, 0:128], hT[:, fc, blk * 128:(blk + 1) * 128], wt[:, fc, :],
                                                                  start=(fc == 0), stop=(fc == FC - 1)),
                                         reads=[wb, hTb], writes=[pb], partial=True, signal=(fc == FC - 1))
                                evac_copy(P, C, ysl[:, blk, npan * 128:(npan + 1) * 128], ps[:, 0:128], [pb], [yslb])
                        for blk in range(4):
                            for half in range(2):
                                tp, tpb = C.psb.next()
                                for t8 in range(8):
                                    tt = half * 8 + t8
                                    P.op("pe", lambda e: e.transpose(tp[:, t8 * 128:(t8 + 1) * 128], OH[:, tt, blk * 128:(blk + 1) * 128], C.ident_b[:, :]),
                                         reads=[ohb, C.constb], writes=[tpb], partial=True, signal=(t8 == 7))
                                evac_copy(P, C, OHT[:, blk, half * 1024:(half + 1) * 1024], tp[:, 0:1024], [tpb], [ohtb])
                        for tt in range(NT):
                            for nt_ in range(4):
                                ps, pb = C.psf.next()
                                for blk in range(4):
                                    P.op("pe", lambda e: e.matmul(ps[:, 0:512], OHT[:, blk, tt * 128:(tt + 1) * 128], ysl[:, blk, nt_ * 512:(nt_ + 1) * 512],
                                                                  start=(blk == 0), stop=(blk == 3)),
                                         reads=[ohtb, yslb], writes=[pb], partial=True, signal=(blk == 3))
                                rt_, rb_ = rmw.next()
                                hpc = A.h[tt * 128:(tt + 1) * 128, nt_ * 512:(nt_ + 1) * 512].rearrange("p (a b) -> p a b", a=2)
                                P.dma("sp", rt_, hpc, reads=[hdram[tt]], writes=[rb_, hnTb], partial=True)
                                P.op("dve", lambda e: e.scalar_tensor_tensor(rt_, ps[:, 0:512].rearrange("p (a b) -> p a b", a=2),
                                                                             gates[:, tt, ei:ei + 1], rt_, ALU.mult, ALU.add),
                                     reads=[pb, gb, rb_], writes=[rb_], partial=True)
                                P.dma("sp", hpc, rt_, reads=[rb_], writes=[hdram[tt]], partial=True)
                    P.cond_region(regs, cnt_i[0:1, ei:ei + 1], cib, g * TB, body)
            P.barrier()
        with ExitStack() as ph:
            gbt, gbb = sb(ph, nc, "m_fgb", [128, D], F32)
            C.rowring = sbring(ph, nc, "m_row2", [1, D], F32, 1)
            xring = sbring(ph, nc, "m_xt2", [128, D], F32, 2)
            xsr = sbring(ph, nc, "m_xs2", [128, D], F32, 2)
            ssr = sbring(ph, nc, "m_ss2", [128, 2], F32, 4)
            bcast_row(P, C, A.final_norm_g, D, gbt, gbb)
            for tt in range(NT):
                xt, xb = xring.next()
                P.dma("sp", xt[:, :], A.h[tt * 128:(tt + 1) * 128, :], writes=[xb])
                ss, ssb = ssr.next()
                xs, xsb = xsr.next()
                P.op("act", lambda e: e.activation(xs[:, :], xt[:, :], AF.Square, accum_out=ss[:, 0:1]), reads=[xb], writes=[xsb, ssb])
                rstd_from_ss(P, ss[:, 1:2], ss[:, 0:1], D, [ssb], [ssb])
                P.op("dve", lambda e: e.scalar_tensor_tensor(xs[:, :], xt[:, :], ss[:, 1:2], gbt[:, :], ALU.mult, ALU.mult),
                     reads=[xb, ssb, gbb], writes=[xsb])
                P.dma("sp", A.out[tt * 128:(tt + 1) * 128, :], xs[:, :], reads=[xsb])
            P.barrier()


def build_program(debug=False):
    nc = bass.Bass("TRN2", target_bir_lowering=False)
    A = Ctx()

    def din(name, shape, dt=F32):
        return nc.dram_tensor(name, list(shape), dt, kind="ExternalInput").ap()

    A.x = din("x", [S, D])
    A.mem = din("mem", [256, D])
    A.rel_bias = din("rel_bias", [32, 8])
    A.mem_norm_g = din("mem_norm_g", [1, D])
    A.norm_mix_g = din("norm_mix_g", [2, D])
    A.norm_cross_g = din("norm_cross_g", [2, D])
    A.norm_ffn_g = din("norm_ffn_g", [2, D])
    A.cross_wq = din("cross_wq", [2, D, 512])
    A.cross_wkv = din("cross_wkv", [2, D, 1024])
    A.cross_wo = din("cross_wo", [2, 512, D])
    A.ev_w_in = din("ev_w_in", [D, 3136])
    A.ev_w_in_krp = din("ev_w_in_krp", [D, 64])
    A.ev_conv_w = din("ev_conv_w", [248, 128])
    A.pvec = din("pvec", [32, 128])
    A.ev_w_uq_n = din("ev_w_uq_n", [512, 1024])
    A.ev_w_uq_r = din("ev_w_uq_r", [512, 512])
    A.ev_w_uq_rp = din("ev_w_uq_rp", [512, 512])
    A.ev_w_ukv_k = din("ev_w_ukv_k", [512, 1024])
    A.ev_w_ukv_v = din("ev_w_ukv_v", [512, 1024])
    A.ev_w_out = din("ev_w_out", [D, D])
    A.ev_ffn_wg = din("ev_ffn_wg", [D, DFF])
    A.ev_ffn_wu = din("ev_ffn_wu", [D, DFF])
    A.ev_ffn_wd = din("ev_ffn_wd", [DFF, D])
    A.od_w_in = din("od_w_in", [D, 3 * D])
    A.od_lam = din("od_lam", [4, 128])
    A.od_subln_g = din("od_subln_g", [1, 256])
    A.od_w_out = din("od_w_out", [D, D])
    A.od_router = din("od_router", [D, 8])
    A.od_moe_wg = din("od_moe_wg", [NEXP, D, DFE])
    A.od_moe_wu = din("od_moe_wu", [NEXP, D, DFE])
    A.od_moe_wd = din("od_moe_wd", [NEXP, DFE, D])
    A.final_norm_g = din("final_norm_g", [1, D])
    A.c_ident = din("c_ident", [128, 128])
    A.c_anti = din("c_anti", [128, 128])
    A.c_rope = din("c_rope", [128, S])
    A.c_oh = din("c_oh", [32, 384])
    A.c_mrev = din("c_mrev", [128, 256])
    A.c_mrow = din("c_mrow", [2, 128])
    A.c_iota = din("c_iota", [128, 512])
    A.c_utri = din("c_utri", [128, 128])
    A.out = nc.dram_tensor("out", [S, D], F32, kind="ExternalOutput").ap()
    A.h = nc.dram_tensor("h_res", [S, D], F32).ap()
    A.zT = nc.dram_tensor("zT", [3200, S], F32).ap()
    A.catT = nc.dram_tensor("catT", [D, S], BF16).ap()
    A.qT = nc.dram_tensor("qT", [2560, S], BF16).ap()
    A.kT = nc.dram_tensor("kT", [2176, S], BF16).ap()
    A.vtm = nc.dram_tensor("vtm", [S, D], BF16).ap()
    A.tv = nc.dram_tensor("tv", [8, 512], F32).ap()
    A.hn_tm = nc.dram_tensor("hn_tm", [S, D], BF16).ap()
    if debug:
        A.dbg_h = [nc.dram_tensor(f"dbg_h{i}", [S, D], F32, kind="ExternalOutput").ap() for i in range(5)]
        A.dbg_cat = nc.dram_tensor("dbg_cat", [D, S], BF16, kind="ExternalOutput").ap()

    with ExitStack() as es:
        P = Prog(nc, es)
        C = Ctx()
        C.flip = 0
        C.psf = Ring([])
        for i in range(6):
            t = es.enter_context(nc.psum_tensor(f"psf{i}", [128, 512], F32))
            C.psf.items.append((t, Buf(f"psf{i}")))
        C.psb = Ring([])
        for i in range(2):
            t = es.enter_context(nc.psum_tensor(f"psb{i}", [128, 1024], BF16))
            C.psb.items.append((t, Buf(f"psb{i}")))
        C.constb = Buf("const")
        C.ident_f, _ = sb(es, nc, "ident_f", [128, 128], F32)
        C.ident_b, _ = sb(es, nc, "ident_b", [128, 128], BF16)
        C.anti_b, _ = sb(es, nc, "anti_b", [128, 128], BF16)
        C.ones_f, _ = sb(es, nc, "ones_f", [128, 128], F32)
        C.mrow, _ = sb(es, nc, "mrow", [1, 256], BF16)
        C.pcol, _ = sb(es, nc, "pcol", [128, 32], F32)
        C.cwcol, _ = sb(es, nc, "cwcol", [128, 248], F32)
        C.cKT, C.cKVb = sb(es, nc, "cKT", [128, 2, 4, 256], BF16)
        C.cV, _ = sb(es, nc, "cV", [128, 2, 2, 512], BF16)
        C.epsc, _ = sb(es, nc, "epsc", [128, 2], F32)
        cs = ExitStack()
        C.gbc = sb(cs, nc, "gbc", [128, D], F32)
        C.rowring = sbring(cs, nc, "row", [1, D], F32, 1)
        C.xring = sbring(cs, nc, "xt", [128, D], F32, 2)
        C.xsring = sbring(cs, nc, "xs", [128, D], F32, 1)
        C.ssring = sbring(cs, nc, "ss", [128, 2], F32, 4)
        C.stbf = sbring(cs, nc, "stbf", [128, 512], BF16, 4)
        C.stf = sbring(cs, nc, "stf", [128, 512], F32, 2)
        C.hpiece = sbring(cs, nc, "hpc", [128, 512], F32, 3)
        C.stat = sbring(cs, nc, "stat", [128, 16], F32, 4)
        C.ering = sbring(cs, nc, "er", [128, 512], BF16, 3)
        C.ptring = sbring(cs, nc, "ptr", [128, 512], BF16, 3)

        cb = C.constb
        P.dma("sp", C.ident_f[:, :], A.c_ident, writes=[cb], partial=True)
        P.dma("pool", C.ident_b[:, :], A.c_ident, writes=[cb], partial=True)
        P.dma("pool", C.anti_b[:, :], A.c_anti, writes=[cb], partial=True)
        P.dma("pool", C.mrow[0:1, 0:128], A.c_mrow[0:1, :], writes=[cb], partial=True)
        P.dma("pool", C.mrow[0:1, 128:256], A.c_mrow[1:2, :], writes=[cb], partial=True)
        P.op("dve", lambda e: e.memset(C.ones_f[:, :], 1.0), writes=[cb], partial=True)
        P.op("dve", lambda e: e.memset(C.epsc[:, :], EPS), writes=[cb], partial=True)
        C_EPS[0] = C.epsc
        with ExitStack() as ph:
            stg, stgb = sb(ph, nc, "p0stg", [128, 128], F32)
            P.dma("sp", stg[0:32, :], A.pvec, writes=[stgb])
            ps, pb = C.psf.next()
            P.op("pe", lambda e: e.transpose(ps[:, 0:32], stg[0:32, :], C.ident_f[0:32, 0:32]), reads=[stgb, cb], writes=[pb])
            P.op("dve", lambda e: e.tensor_copy(C.pcol[:, :], ps[:, 0:32]), reads=[pb], writes=[cb], partial=True)
            for (r0, nr) in ((0, 128), (128, 120)):
                stg2, stg2b = sb(ph, nc, f"p0stg{r0}", [128, 128], F32)
                P.dma("sp", stg2[0:nr, :], A.ev_conv_w[r0:r0 + nr, :], writes=[stg2b])
                ps, pb = C.psf.next()
                P.op("pe", lambda e: e.transpose(ps[:, 0:nr], stg2[0:nr, :], C.ident_f[0:nr, 0:nr]), reads=[stg2b, cb], writes=[pb])
                P.op("dve", lambda e: e.tensor_copy(C.cwcol[:, r0:r0 + nr], ps[:, 0:nr]), reads=[pb], writes=[cb], partial=True)
            P.barrier()

        with ExitStack() as ph:
            mT, mTb = sb(ph, nc, "memT", [128, 16, 256], BF16)
            panel = sbring(ph, nc, "p1pan", [128, 16, 512], BF16, 2)
            C.panel = panel
            norm_T(P, C, A.mem, 256, A.mem_norm_g, mT, mTb)
            for layer in range(2):
                def evk(ci, t0, tw, pss):
                    ps, pb = pss[0]
                    evac_copy(P, C, C.cKT[:, layer, ci, 0:256], ps[:, 0:256], [pb], [C.cKVb])
                proj_fm(P, C, mT, mTb, 16, 256, [A.cross_wkv[layer][:, 0:512]], 512, 128, evk, panel)

                def evv(tt, p0, pw, ps, pb):
                    evac_copy(P, C, C.cV[:, layer, tt, 0:512], ps[:, 0:512], [pb], [C.cKVb])
                proj_tm(P, C, mT, mTb, 16, 256, A.cross_wkv[layer][:, 512:1024], 512, evv, panel)
            P.barrier()

        hb_ = Buf("hcopy")
        for i in range(4):
            P.dma("sp", A.h[i * 512:(i + 1) * 512, :], A.x[i * 512:(i + 1) * 512, :], writes=[hb_], partial=True)
        P.barrier()

        with ExitStack() as ph:
            hnT, hnTb = sb(ph, nc, "l0_hnT", [128, 16, S], BF16)
            C.panel = sbring(ph, nc, "l0pan", [128, 16, 512], BF16, 2)
            norm_T(P, C, A.h, S, A.norm_mix_g[0:1, :], hnT, hnTb)

            def mk_ev(row0, M):
                def ev(ci, t0, tw, pss):
                    ps, pb = pss[0]
                    st, stb = C.stf.next()
                    evac_copy(P, C, st[0:M, 0:tw], ps[0:M, 0:tw], [pb], [stb], partial=False)
                    P.dma("sp", A.zT[row0 + ci * M: row0 + (ci + 1) * M, t0:t0 + tw], st[0:M, 0:tw], reads=[stb])
                return ev
            proj_fm(P, C, hnT, hnTb, 16, S, [A.ev_w_in[:, 0:3072]], 3072, 128, mk_ev(0, 128), C.panel)
            proj_fm(P, C, hnT, hnTb, 16, S, [A.ev_w_in[:, 3072:3136]], 64, 64, mk_ev(3072, 64), C.panel)
            proj_fm(P, C, hnT, hnTb, 16, S, [A.ev_w_in_krp], 64, 64, mk_ev(3136, 64), C.panel)
            P.barrier()

        with ExitStack() as ph:
            cv, cvb = sb(ph, nc, "cv", [128, 8, S], F32)
            vg = sbring(ph, nc, "vg", [128, S], F32, 4)
            up = sbring(ph, nc, "upad", [128, S + 32], F32, 2)
            sq = sbring(ph, nc, "sq", [128, 512], F32, 2)
            mr, mrb = sb(ph, nc, "mr", [128, 3, 512], F32)
            for cc in range(8):
                vt, vb = vg.next()
                gt, gtb = vg.next()
                P.dma("sp", vt[:, :], A.zT[cc * 128:(cc + 1) * 128, :], writes=[vb])
                P.dma("sp", gt[:, :], A.zT[1024 + cc * 128:1024 + (cc + 1) * 128, :], writes=[gtb])
                P.op("act", lambda e: e.activation(gt[:, :], gt[:, :], AF.Sigmoid), reads=[gtb], writes=[gtb])
                ut, ub = up.next()
                P.op("dve", lambda e: e.memset(ut[:, 0:32], 0.0), writes=[ub])
                P.op("dve", lambda e: e.tensor_tensor(ut[:, 32:32 + S], vt[:, :], gt[:, :], ALU.mult), reads=[vb, gtb, ub], writes=[ub], partial=True)
                for j in range(31):
                    wcol = C.cwcol[:, j * 8 + cc: j * 8 + cc + 1]
                    if j == 0:
                        P.op("dve", lambda e: e.tensor_scalar(cv[:, cc, :], ut[:, 2:2 + S], wcol, C.pcol[:, cc:cc + 1], ALU.mult, ALU.add),
                             reads=[ub, cb], writes=[cvb], partial=True)
                    else:
                        P.op("dve", lambda e: e.scalar_tensor_tensor(cv[:, cc, :], ut[:, 2 + j:2 + j + S], wcol, cv[:, cc, :], ALU.mult, ALU.add),
                             reads=[ub, cb, cvb], writes=[cvb], partial=True)
            for t0 in range(0, S, 512):
                psm, pmb = C.psf.next()
                pss_, psb_ = C.psf.next()
                for cc in range(8):
                    P.op("pe", lambda e: e.matmul(psm[:, :], C.ones_f[:, :], cv[:, cc, t0:t0 + 512], start=(cc == 0), stop=(cc == 7)),
                         reads=[cvb, cb], writes=[pmb], partial=True, signal=(cc == 7))
                for cc in range(8):
                    st, stb = sq.next()
                    P.op("act", lambda e: e.activation(st[:, :], cv[:, cc, t0:t0 + 512], AF.Square), reads=[cvb], writes=[stb])
                    P.op("pe", lambda e: e.matmul(pss_[:, :], C.ones_f[:, :], st[:, :], start=(cc == 0), stop=(cc == 7)),
                         reads=[stb, cb], writes=[psb_], partial=True, signal=True)
                P.op("dve", lambda e: e.tensor_scalar(mr[:, 0, :], psm[:, :], 1.0 / 1024, None, ALU.mult), reads=[pmb], writes=[mrb])
                P.op("dve", lambda e: e.tensor_tensor(mr[:, 1, :], mr[:, 0, :], mr[:, 0, :], ALU.mult), reads=[mrb], writes=[mrb])
                P.op("dve", lambda e: e.scalar_tensor_tensor(mr[:, 1, :], pss_[:, :], 1.0 / 1024, mr[:, 1, :], ALU.mult, ALU.subtract),
                     reads=[psb_, mrb], writes=[mrb])
                P.op("act", lambda e: e.activation(mr[:, 1, :], mr[:, 1, :], AF.Sqrt, bias=C_EPS[0][:, 0:1], scale=1.0), reads=[mrb], writes=[mrb])
                P.op("dve", lambda e: e.reciprocal(mr[:, 1, :], mr[:, 1, :]), reads=[mrb], writes=[mrb])
                for cc in range(8):
                    st, stb = sq.next()
                    P.op("dve", lambda e: e.tensor_tensor(st[:, :], cv[:, cc, t0:t0 + 512], mr[:, 0, :], ALU.subtract), reads=[cvb, mrb], writes=[stb])
                    P.op("dve", lambda e: e.tensor_tensor(st[:, :], st[:, :], mr[:, 1, :], ALU.mult), reads=[stb, mrb], writes=[stb])
                    so, sob = C.stbf.next()
                    P.op("act", lambda e: e.activation(so[:, :], st[:, :], AF.Silu, bias=C.pcol[:, 16 + cc:17 + cc], scale=C.pcol[:, 8 + cc:9 + cc]),
                         reads=[stb, cb], writes=[sob])
                    P.dma("sp", A.catT[cc * 128:(cc + 1) * 128, t0:t0 + 512], so[:, :], reads=[sob])
            P.barrier()

        with ExitStack() as ph:
            cqn, cqnb = sb(ph, nc, "cqn", [128, 4, S], BF16)
            ckvn, ckvnb = sb(ph, nc, "ckvn", [128, 4, S], BF16)
            rope, ropeb = sb(ph, nc, "rope", [64, 2, S], F32)
            sq = sbring(ph, nc, "sq4", [128, 512], F32, 3)
            C.panel = sbring(ph, nc, "p4pan", [128, 4, 512], BF16, 3)
            P.dma("sp", rope[:, 0, :], A.c_rope[0:64, :], writes=[ropeb], partial=True)
            P.dma("sp", rope[:, 1, :], A.c_rope[64:128, :], writes=[ropeb], partial=True)
            for (zrow, dst, dstb, gc0) in ((2048, cqn, cqnb, 24), (2560, ckvn, ckvnb, 28)):
              with ExitStack() as ph2:
                src, srcb = sb(ph2, nc, f"csrc{zrow}", [128, 4, S], F32)
                load_fm(P, C, src, srcb, A.zT, zrow, 4)
                for t0 in range(0, S, 512):
                    ps, pb = C.psf.next()
                    for c in range(4):
                        st, stb = sq.next()
                        P.op("act", lambda e: e.activation(st[:, :], src[:, c, t0:t0 + 512], AF.Square), reads=[srcb], writes=[stb])
                        P.op("pe", lambda e: e.matmul(ps[:, :], C.ones_f[:, :], st[:, :], start=(c == 0), stop=(c == 3)),
                             reads=[stb, cb], writes=[pb], partial=True)
                    rs, rsb = sq.next()
                    rstd_from_ss(P, rs[:, :], ps[:, :], 512, [pb], [rsb])
                    for c in range(4):
                        P.op("dve", lambda e: e.scalar_tensor_tensor(dst[:, c, t0:t0 + 512], src[:, c, t0:t0 + 512], C.pcol[:, gc0 + c:gc0 + c + 1],
                                                                     rs[:, :], ALU.mult, ALU.mult),
                             reads=[srcb, rsb, cb], writes=[dstb], partial=True)
                P.barrier()
            def rope_evac(x_ap, xp_ap, rd, t0, tw, dst_rows):
                a, ab = sq.next()
                b, bb = sq.next()
                P.op("dve", lambda e: e.tensor_tensor(a[0:64, 0:tw], x_ap, rope[:, 0, t0:t0 + tw], ALU.mult), reads=rd + [ropeb], writes=[ab])
                P.op("dve", lambda e: e.tensor_tensor(b[0:64, 0:tw], xp_ap, rope[:, 1, t0:t0 + tw], ALU.mult), reads=rd + [ropeb], writes=[bb])
                so, sob = C.stbf.next()
                P.op("dve", lambda e: e.tensor_tensor(so[0:64, 0:tw], a[0:64, 0:tw], b[0:64, 0:tw], ALU.add), reads=[ab, bb], writes=[sob])
                P.dma("sp", dst_rows[:, t0:t0 + tw], so[0:64, 0:tw], reads=[sob])
            with ExitStack() as ph2:
                kr, krb = sb(ph2, nc, "kr", [64, 2, S], F32)
                P.dma("sp", kr[:, 0, :], A.zT[3072:3136, :], writes=[krb], partial=True)
                P.dma("sp", kr[:, 1, :], A.zT[3136:3200, :], writes=[krb], partial=True)
                for t0 in range(0, S, 512):
                    rope_evac(kr[:, 0, t0:t0 + 512], kr[:, 1, t0:t0 + 512], [krb], t0, 512, A.kT[1024:1088, :])
                P.barrier()
            proj_fm(P, C, cqn, cqnb, 4, S, [A.ev_w_uq_n], 1024, 128, store_fm_bf16(P, C, A.qT, 0), C.panel)

            def ev_qr(ci, t0, tw, pss):
                (p1, b1), (p2, b2) = pss
                rope_evac(p1[0:64, 0:tw], p2[0:64, 0:tw], [b1, b2], t0, tw, A.qT[1024 + ci * 64:1024 + (ci + 1) * 64, :])
            proj_fm(P, C, cqn, cqnb, 4, S, [A.ev_w_uq_r, A.ev_w_uq_rp], 512, 64, ev_qr, C.panel)
            proj_fm(P, C, ckvn, ckvnb, 4, S, [A.ev_w_ukv_k], 1024, 128, store_fm_bf16(P, C, A.kT, 0), C.panel)

            def ev_v(tt, p0, pw, ps, pb):
                st, stb = C.stbf.next()
                evac_copy(P, C, st[:, 0:pw], ps[:, 0:pw], [pb], [stb], partial=False)
                P.dma("sp", A.vtm[tt * 128:(tt + 1) * 128, p0:p0 + pw], st[:, 0:pw], reads=[stb])
            proj_tm(P, C, ckvn, ckvnb, 4, S, A.ev_w_ukv_v, 1024, ev_v, C.panel)
            P.barrier()

        with ExitStack() as ph:
            qn = sbring(ph, nc, "a_qn", [128, S], BF16, 2)
            qr = sbring(ph, nc, "a_qr", [64, S], BF16, 2)
            kn = sbring(ph, nc, "a_kn", [128, S], BF16, 2)
            vv = sbring(ph, nc, "a_v", [128, 16, 128], BF16, 2)
            krt, krtb = sb(ph, nc, "a_kr", [64, S], BF16)
            osb = sbring(ph, nc, "a_o", [128, 128], F32, 2)
            P.dma("sp", krt[:, :], A.kT[1024:1088, :], writes=[krtb])
            sc = 192 ** -0.5
            for hh in range(8):
                qn_t, qn_b = qn.next()
                qr_t, qr_b = qr.next()
                kn_t, kn_b = kn.next()
                v_t, v_b = vv.next()
                P.dma("sp", qn_t[:, :], A.qT[hh * 128:(hh + 1) * 128, :], writes=[qn_b])
                P.dma("sp", qr_t[:, :], A.qT[1024 + hh * 64:1024 + (hh + 1) * 64, :], writes=[qr_b])
                P.dma("sp", kn_t[:, :], A.kT[hh * 128:(hh + 1) * 128, :], writes=[kn_b])
                P.dma("sp", v_t[:, :, :], A.vtm[:, hh * 128:(hh + 1) * 128].rearrange("(kc p) d -> p kc d", p=128), writes=[v_b])

                def s_terms(qt, c0, n):
                    q0 = qt * 128
                    terms = [(qn_t[:, q0:q0 + 128], kn_t[:, c0:c0 + n], 0, n),
                             (qr_t[:, q0:q0 + 128], krt[:, c0:c0 + n], 0, n)]
                    if c0 + n == q0 + 128:
                        terms.append((C.mrow[0:1, 0:128], C.mrow[0:1, 128:256], n - 128, 128))
                    return terms

                def out_cb(qt, o_ps, o_pb, rinv, rb):
                    ot, ob = osb.next()
                    P.op("dve", lambda e: e.tensor_scalar(ot[:, :], o_ps[:, 0:128], rinv, None, ALU.mult), reads=[o_pb, rb], writes=[ob])
                    out_T_store(P, C, ot, ob, 128, A.catT, 1024 + hh * 128, qt)
                with AttnPools(C):
                    attention(P, C, NT, lambda qt: (qt + 1) * 128, s_terms, lambda kb: v_t[:, kb, :], 128, sc, out_cb,
                              [qn_b, qr_b, kn_b, v_b, krtb, cb])
            P.barrier()

        with ExitStack() as ph:
            catS, catSb = sb(ph, nc, "catS", [128, 16, S], BF16)
            C.panel = sbring(ph, nc, "p6pan", [128, 16, 512], BF16, 2)
            load_fm(P, C, catS, catSb, A.catT, 0, 16)
            residual_linear(P, C, catS, catSb, 16, A.ev_w_out, A.h)
            P.barrier()
            if debug:
                P.dma("sp", A.dbg_h[0], A.h, reads=[])
                P.dma("sp", A.dbg_cat, A.catT, reads=[])
                P.barrier()
        with ExitStack() as ph:
            C.panel = sbring(ph, nc, "p7pan", [128, 16, 512], BF16, 2)
            cross_attention_layer(P, C, nc, A, 0)
            if debug:
                P.dma("sp", A.dbg_h[1], A.h, reads=[])
                P.barrier()
        ffn_dense(P, C, nc, A, A.norm_ffn_g[0:1, :], [(A.ev_ffn_wg, A.ev_ffn_wu, A.ev_ffn_wd)], DFF)
        if debug:
            P.dma("sp", A.dbg_h[2], A.h, reads=[])
            P.barrier()

        lambda_init = 0.8 - 0.6 * math.exp(-0.3 * 1)
        with ExitStack() as ph:
            hnT, hnTb = sb(ph, nc, "l1_hnT", [128, 16, S], BF16)
            C.panel = sbring(ph, nc, "l1pan", [128, 16, 512], BF16, 2)
            norm_T(P, C, A.h, S, A.norm_mix_g[1:2, :], hnT, hnTb)
            proj_fm(P, C, hnT, hnTb, 16, S, [A.od_w_in[:, 0:D]], D, 128, store_fm_bf16(P, C, A.qT, 0), C.panel)
            proj_fm(P, C, hnT, hnTb, 16, S, [A.od_w_in[:, D:2 * D]], D, 128, store_fm_bf16(P, C, A.kT, 0), C.panel)

            def ev_v1(tt, p0, pw, ps, pb):
                st, stb = C.stbf.next()
                evac_copy(P, C, st[:, 0:pw], ps[:, 0:pw], [pb], [stb], partial=False)
                P.dma("sp", A.vtm[tt * 128:(tt + 1) * 128, p0:p0 + pw], st[:, 0:pw], reads=[stb])
            proj_tm(P, C, hnT, hnTb, 16, S, A.od_w_in[:, 2 * D:3 * D], D, ev_v1, C.panel)
            P.barrier()

        with ExitStack() as ph:
            sc = 128 ** -0.5
            qh = sbring(ph, nc, "d_q", [128, S], BF16, 4)
            kh = sbring(ph, nc, "d_k", [128, S], BF16, 4)
            vv = sbring(ph, nc, "d_v", [128, 16, 256], BF16, 2)
            brev, brevb = sb(ph, nc, "brev", [128, 8, 256], F32)
            bhi, bhib = sb(ph, nc, "bhi", [128, 8, 256], BF16)
            blo, blob = sb(ph, nc, "blo", [128, 8, 256], BF16)
            mrev, mrevb = sb(ph, nc, "mrev", [128, 256], F32)
            tmpf = sbring(ph, nc, "d_tmp", [128, 256], F32, 4)
            sgb, sgbb = sb(ph, nc, "sgb", [128, 256], F32)
            lam, lamb = sb(ph, nc, "lam", [128, 8], F32)
            lrow, lrowb = sb(ph, nc, "lrow", [1, 4, 128], F32)
            rb32, rb32b = sb(ph, nc, "rb32", [32, 8], F32)
            oh, ohb = sb(ph, nc, "oh", [32, 384], F32)
            vrow, vrowb = sb(ph, nc, "vrow", [8, 384], F32)
            P.dma("sp", rb32[:, :], A.rel_bias, writes=[rb32b])
            P.dma("sp", oh[:, :], A.c_oh, writes=[ohb])
            P.dma("sp", mrev[:, :], A.c_mrev, writes=[mrevb])
            ps, pb = C.psf.next()
            P.op("pe", lambda e: e.matmul(ps[0:8, 0:384], rb32[:, :], oh[:, :], start=True, stop=True), reads=[rb32b, ohb], writes=[pb])
            P.op("dve", lambda e: e.tensor_copy(vrow[:, :], ps[0:8, 0:384]), reads=[pb], writes=[vrowb])
            P.dma("sp", A.tv[:, 0:384], vrow[:, :], reads=[vrowb], writes=[brevb])
            for hh in range(8):
                src = bass.AP(tensor=A.tv.tensor, offset=hh * 512, ap=[[1, 128], [1, 256]])
                P.dma("sp", brev[:, hh, :], src, reads=[brevb], writes=[brevb], partial=True)
            for hh in range(8):
                t1, t1b = tmpf.next()
                P.op("dve", lambda e: e.tensor_scalar(t1[:, :], brev[:, hh, :], brev[:, hh, 0:1], 1.0 / sc, ALU.subtract, ALU.mult),
                     reads=[brevb], writes=[t1b])
                P.op("dve", lambda e: e.tensor_tensor(t1[:, :], t1[:, :], mrev[:, :], ALU.add), reads=[t1b, mrevb], writes=[t1b])
                P.op("dve", lambda e: e.tensor_copy(bhi[:, hh, :], t1[:, :]), reads=[t1b], writes=[bhib], partial=True)
                t2, t2b = tmpf.next()
                P.op("dve", lambda e: e.tensor_copy(t2[:, :], bhi[:, hh, :]), reads=[bhib], writes=[t2b])
                P.op("dve", lambda e: e.tensor_tensor(blo[:, hh, :], t1[:, :], t2[:, :], ALU.subtract), reads=[t1b, t2b], writes=[blob], partial=True)
            P.dma("sp", lrow[0:1, :, :], A.od_lam.rearrange("(o a) d -> o a d", o=1), writes=[lrowb])
            P.op("dve", lambda e: e.tensor_tensor(lrow[0:1, 0, :], lrow[0:1, 0, :], lrow[0:1, 1, :], ALU.mult), reads=[lrowb], writes=[lrowb])
            P.op("dve", lambda e: e.tensor_tensor(lrow[0:1, 2, :], lrow[0:1, 2, :], lrow[0:1, 3, :], ALU.mult), reads=[lrowb], writes=[lrowb])
            P.op("dve", lambda e: e.reduce_sum(lrow[0:1, 1, 0:1], lrow[0:1, 0, :], AX.X), reads=[lrowb], writes=[lrowb])
            P.op("dve", lambda e: e.reduce_sum(lrow[0:1, 1, 1:2], lrow[0:1, 2, :], AX.X), reads=[lrowb], writes=[lrowb])
            P.op("act", lambda e: e.activation(lrow[0:1, 1, 2:4], lrow[0:1, 1, 0:2], AF.Exp), reads=[lrowb], writes=[lrowb])
            P.op("dve", lambda e: e.tensor_tensor(lrow[0:1, 1, 4:5], lrow[0:1, 1, 3:4], lrow[0:1, 1, 2:3], ALU.subtract), reads=[lrowb], writes=[lrowb])
            P.op("dve", lambda e: e.tensor_scalar(lrow[0:1, 1, 4:5], lrow[0:1, 1, 4:5], -lambda_init, None, ALU.add), reads=[lrowb], writes=[lrowb])
            ps, pb = C.psf.next()
            P.op("pe", lambda e: e.matmul(ps[:, 0:1], C.ones_f[0:1, 0:128], lrow[0:1, 1, 4:5], start=True, stop=True), reads=[lrowb, cb], writes=[pb])
            P.op("dve", lambda e: e.tensor_copy(lam[:, 0:1], ps[:, 0:1]), reads=[pb], writes=[lamb])
            bcast_row(P, C, A.od_subln_g, 256, sgb, sgbb, mul=(1.0 - lambda_init))
            for hh in range(8):
                q_t = [qh.next(), qh.next()]
                k_t = [kh.next(), kh.next()]
                v_t, v_b = vv.next()
                for c in range(2):
                    P.dma("sp", q_t[c][0][:, :], A.qT[(hh * 2 + c) * 128:(hh * 2 + c + 1) * 128, :], writes=[q_t[c][1]])
                    P.dma("sp", k_t[c][0][:, :], A.kT[(hh * 2 + c) * 128:(hh * 2 + c + 1) * 128, :], writes=[k_t[c][1]])
                P.dma("sp", v_t[:, :, :], A.vtm[:, hh * 256:(hh + 1) * 256].rearrange("(kc p) d -> p kc d", p=128), writes=[v_b])
                o0 = {}

                def make_spec(c, qq, qb_, kk, kb_):
                    def s_terms(qt, c0, n):
                        q0 = qt * 128
                        terms = [(qq[:, q0:q0 + 128], kk[:, c0:c0 + n], 0, n)]
                        for (kb0, col0) in ((q0 - 128, 0), (q0, 128)):
                            if kb0 >= c0 and kb0 < c0 + n:
                                for bt in (bhi, blo):
                                    terms.append((C.anti_b[:, :], bt[:, hh, col0:col0 + 128], kb0 - c0, 128))
                        return terms

                    def out_cb(qt, o_ps, o_pb, rinv, rb):
                        if c == 0:
                            t0_, t0b = tmpf.next()
                            P.op("dve", lambda e: e.tensor_scalar(t0_[:, :], o_ps[:, 0:256], rinv, None, ALU.mult), reads=[o_pb, rb], writes=[t0b])
                            o0[qt] = (t0_, t0b)
                        else:
                            t0_, t0b = o0[qt]
                            P.op("dve", lambda e: e.tensor_tensor(lam[:, 1:2], rinv, lam[:, 0:1], ALU.mult), reads=[rb, lamb], writes=[lamb])
                            P.op("dve", lambda e: e.scalar_tensor_tensor(t0_[:, :], o_ps[:, 0:256], lam[:, 1:2], t0_[:, :], ALU.mult, ALU.add),
                                 reads=[o_pb, lamb, t0b], writes=[t0b])
                            jk, jkb = tmpf.next()
                            ss, ssb = C.ssring.next()
                            P.op("act", lambda e: e.activation(jk[:, :], t0_[:, :], AF.Square, accum_out=ss[:, 0:1]), reads=[t0b], writes=[jkb, ssb])
                            rstd_from_ss(P, ss[:, 1:2], ss[:, 0:1], 256, [ssb], [ssb])
                            P.op("dve", lambda e: e.scalar_tensor_tensor(t0_[:, :], t0_[:, :], ss[:, 1:2], sgb[:, :], ALU.mult, ALU.mult),
                                 reads=[t0b, ssb, sgbb], writes=[t0b])
                            out_T_store(P, C, t0_, t0b, 256, A.catT, hh * 256, qt)
                    return s_terms, out_cb, [qb_, kb_, v_b, bhib, blob, cb]
                specs = [make_spec(c, q_t[c][0], q_t[c][1], k_t[c][0], k_t[c][1]) for c in range(2)]
                def gens():
                    for qt in range(NT):
                        for c in range(2):
                            s_terms, out_cb, bufs = specs[c]
                            yield attention_gen(P, C, qt, (qt + 1) * 128, s_terms, lambda kb: v_t[:, kb, :], 256, sc, out_cb, bufs)
                with AttnPools(C):
                    run_gens(gens(), 2)
            P.barrier()

        with ExitStack() as ph:
            catS, catSb = sb(ph, nc, "catS1", [128, 16, S], BF16)
            C.panel = sbring(ph, nc, "p11pan", [128, 16, 512], BF16, 2)
            load_fm(P, C, catS, catSb, A.catT, 0, 16)
            residual_linear(P, C, catS, catSb, 16, A.od_w_out, A.h)
            P.barrier()
            if debug:
                P.dma("sp", A.dbg_h[3], A.h, reads=[])
                P.barrier()
        with ExitStack() as ph:
            C.panel = sbring(ph, nc, "p12pan", [128, 16, 512], BF16, 2)
            cross_attention_layer(P, C, nc, A, 1)
            if debug:
                P.dma("sp", A.dbg_h[4], A.h, reads=[])
                P.barrier()
        experts = [(A.od_moe_wg[e], A.od_moe_wu[e], A.od_moe_wd[e]) for e in range(NEXP)]
        P.barrier()
        cs.close()
        regs = nc.alloc_registers("moe_cnt", engines=list(nc.engines.keys()))
        moe_sparse(P, C, nc, A, experts, regs)
        P.barrier()
    nc._marks = P.marks
    return nc


def _t5_bucket(rel):
    half, max_exact = 16, 8
    ret = (rel > 0).astype(np.int32) * half
    n = np.abs(rel)
    nf = np.maximum(n, 1).astype(np.float32)
    large = max_exact + (np.log(nf / max_exact) / math.log(128 / max_exact) * (half - max_exact)).astype(np.int32)
    large = np.minimum(large, half - 1)
    return ret + np.where(n < max_exact, n, large)


def host_constants():
    c = {}
    c["c_ident"] = np.eye(128, dtype=np.float32)
    c["c_anti"] = np.ascontiguousarray(np.eye(128, dtype=np.float32)[::-1])
    pos = np.arange(S, dtype=np.float32)
    inv = np.power(np.float32(10000.0), -np.arange(0, 64, 2, dtype=np.float32) / np.float32(64)).astype(np.float32)
    ang = (pos[None, :] * inv[:, None]).astype(np.float32)
    cs, sn = np.cos(ang).astype(np.float32), np.sin(ang).astype(np.float32)
    c["c_rope"] = np.concatenate([cs, cs, -sn, sn], axis=0).astype(np.float32)
    rel = np.arange(384, dtype=np.int32) - 255
    bk = _t5_bucket(rel)
    oh = np.zeros((32, 384), np.float32)
    oh[bk, np.arange(384)] = 1.0
    oh[:, 383] = 0.0
    c["c_oh"] = oh
    m = np.zeros((128, 256), np.float32)
    m[64:128, 192:256] = NEGM
    c["c_mrev"] = m
    mr = np.zeros((2, 128), np.float32)
    mr[0, 0:64] = 1.0
    mr[1, 64:128] = NEGM
    c["c_mrow"] = mr
    c["c_iota"] = np.ascontiguousarray(np.broadcast_to(np.arange(512, dtype=np.float32)[None, :], (128, 512)))
    c["c_utri"] = np.triu(np.ones((128, 128), np.float32), k=1)
    return c


def host_layout(inp):
    g = {}
    f = lambda a: np.ascontiguousarray(a, dtype=np.float32)
    g["rel_bias"] = f(inp["rel_bias"])
    g["mem_norm_g"] = f(inp["mem_norm_g"]).reshape(1, D)
    for k in ("norm_mix_g", "norm_cross_g", "norm_ffn_g", "cross_wq", "cross_wkv", "cross_wo"):
        g[k] = f(inp[k])
    w_in = f(inp["ev_w_in"][0])
    g["ev_w_in"] = w_in
    kr = w_in[:, 3072:3136]
    g["ev_w_in_krp"] = f(np.concatenate([kr[:, 32:64], kr[:, 0:32]], axis=1))
    g["ev_conv_w"] = f(inp["ev_conv_w"][0]).reshape(248, 128)
    g["pvec"] = f(np.concatenate([inp["ev_conv_b"][0].reshape(8, 128), inp["ev_ln_g"][0].reshape(8, 128),
                                  inp["ev_ln_b"][0].reshape(8, 128), inp["ev_q_norm_g"][0].reshape(4, 128),
                                  inp["ev_kv_norm_g"][0].reshape(4, 128)], axis=0))
    uq = f(inp["ev_w_uq"][0]).reshape(512, 8, 192)
    g["ev_w_uq_n"] = f(uq[:, :, 0:128].reshape(512, 1024))
    g["ev_w_uq_r"] = f(uq[:, :, 128:192].reshape(512, 512))
    g["ev_w_uq_rp"] = f(np.concatenate([uq[:, :, 160:192], uq[:, :, 128:160]], axis=2).reshape(512, 512))
    ukv = f(inp["ev_w_ukv"][0]).reshape(512, 8, 256)
    g["ev_w_ukv_k"] = f(ukv[:, :, 0:128].reshape(512, 1024))
    g["ev_w_ukv_v"] = f(ukv[:, :, 128:256].reshape(512, 1024))
    g["ev_w_out"] = f(inp["ev_w_out"][0])
    g["ev_ffn_wg"] = f(inp["ev_ffn_wg"][0])
    g["ev_ffn_wu"] = f(inp["ev_ffn_wu"][0])
    g["ev_ffn_wd"] = f(inp["ev_ffn_wd"][0])
    g["od_w_in"] = f(inp["od_w_in"][0])
    g["od_lam"] = f(np.concatenate([inp["od_lambda_q1"], inp["od_lambda_k1"], inp["od_lambda_q2"], inp["od_lambda_k2"]], axis=0))
    g["od_subln_g"] = f(inp["od_subln_g"]).reshape(1, 256)
    g["od_w_out"] = f(inp["od_w_out"][0])
    g["od_router"] = f(inp["od_router"][0])
    g["od_moe_wg"] = f(inp["od_moe_wg"][0])
    g["od_moe_wu"] = f(inp["od_moe_wu"][0])
    g["od_moe_wd"] = f(inp["od_moe_wd"][0])
    g["final_norm_g"] = f(inp["final_norm_g"]).reshape(1, D)
    g.update(host_constants())
    return g


def kernel(**inputs):
    n = 8
    shared = host_layout(inputs)
    x = np.ascontiguousarray(inputs["x"], dtype=np.float32)
    mem = np.ascontiguousarray(inputs["mem"], dtype=np.float32)
    nc = build_program()
    in_maps = []
    for b in range(n):
        m = dict(shared)
        m["x"] = x[b]
        m["mem"] = mem[b]
        in_maps.append(m)
    res = run_bass_kernel_spmd(nc, in_maps, core_ids=list(range(n)))
    return np.stack([np.asarray(r["out"], dtype=np.float32) for r in res.results], axis=0)
```

```python
import math
from contextlib import ExitStack
import numpy as np
import concourse.bass as bass
import concourse.mybir as mybir
from concourse.bass_utils import run_bass_kernel_spmd

F32, BF16 = mybir.dt.float32, mybir.dt.bfloat16
ALU = mybir.AluOpType
AF = mybir.ActivationFunctionType
AX = mybir.AxisListType

S = 2048
D = 2048
NT = S // 128
EPS = 1e-6
NEGM = -30000.0
DFF = 5632
DFE = 7168
NEXP = 8


class Buf:
    __slots__ = ("name", "writes", "reads")

    def __init__(self, name=""):
        self.name = name
        self.writes = {}
        self.reads = {}


def _merge(dst, src):
    for k, (s, v) in src.items():
        if k not in dst or dst[k][1] < v:
            dst[k] = (s, v)


class Prog:
    def __init__(self, nc, es, ndma=12):
        self.nc = nc
        self.eng = {"pe": nc.tensor, "act": nc.scalar, "dve": nc.vector, "pool": nc.gpsimd, "sp": nc.sync}
        self.sem = {}
        self.cnt = {}
        self.pending = {}
        self.waited = {e: {} for e in self.eng}
        for e in self.eng:
            self.sem[e] = es.enter_context(nc.semaphore("c_" + e))
            self.cnt[e] = 0
            self.pending[e] = False
        self.dsem = {"sp": [], "pool": []}
        self.dval = {}
        self.dnext = {"sp": 0, "pool": 0}
        for q in ("sp", "pool"):
            for i in range(ndma):
                key = f"d_{q}{i}"
                self.dsem[q].append((key, es.enter_context(nc.semaphore(key))))
                self.dval[key] = 0
        self.ninst = 0
        self.marks = []

    def _wait(self, e, toks):
        w = self.waited[e]
        for key, (sem, val) in toks.items():
            if val <= 0 or (e == "pe" and key == "pe"):
                continue
            if w.get(key, 0) >= val:
                continue
            if key in self.cnt:
                assert val <= self.cnt[key], f"wait on future signal {key} {val}>{self.cnt[key]}"
            self.eng[e].wait_ge(sem, val)
            w[key] = val

    def _deps(self, reads, writes, partial):
        toks = {}
        for b in reads:
            _merge(toks, b.writes)
        for b in writes:
            _merge(toks, b.reads)
            if not partial:
                _merge(toks, b.writes)
        return toks

    def _commit(self, key, tok, reads, writes, partial):
        for b in reads:
            b.reads[key] = tok
        for b in writes:
            if not partial:
                b.writes = {}
                b.reads = {}
            b.writes[key] = tok

    def op(self, e, fn, reads=(), writes=(), signal=True, partial=False):
        self._wait(e, self._deps(reads, writes, partial))
        ins = fn(self.eng[e])
        self.ninst += 1
        if signal:
            self.cnt[e] += 1
            assert self.cnt[e] < 60000, "semaphore count too large"
            ins.then_inc(self.sem[e], 1)
            val = self.cnt[e]
            self.pending[e] = False
        else:
            val = self.cnt[e] + 1
            self.pending[e] = True
        self._commit(e, (self.sem[e], val), reads, writes, partial)

    def dma(self, q, out, in_, reads=(), writes=(), partial=False):
        toks = self._deps(reads, writes, partial)
        pool = self.dsem[q]
        i = self.dnext[q]
        self.dnext[q] = (i + 1) % len(pool)
        key, sem = pool[i]
        prev = self.dval[key]
        toks[key] = (sem, prev)
        self._wait(q, toks)
        self.eng[q].dma_start(out=out, in_=in_).then_inc(sem, 16)
        self.ninst += 1
        self.dval[key] = prev + 16
        assert prev + 16 < 60000
        self._commit(key, (sem, prev + 16), reads, writes, partial)

    def cond_region(self, regs, cnt_ap, cnt_buf, thresh, body):
        for e in self.eng:
            assert not self.pending[e]
        toks = {}
        _merge(toks, cnt_buf.writes)
        for e in self.eng:
            self._wait(e, toks)
        self.nc.regs_load(regs, cnt_ap)
        before = dict(self.cnt)
        dbefore = dict(self.dval)
        dnext = dict(self.dnext)
        wsnap = {e: dict(w) for e, w in self.waited.items()}
        with self.nc.If_cmp(regs, thresh, "IS_GT"):
            body()
            for e in self.eng:
                assert not self.pending[e]
        after = dict(self.cnt)
        dafter = dict(self.dval)
        with self.nc.Else():
            for e in self.eng:
                if after[e] > before[e]:
                    if before[e] > 0:
                        self.eng[e].wait_ge(self.sem[e], before[e])
                    self.eng[e].sem_inc(self.sem[e], after[e] - before[e])
            for q in self.dsem:
                for key, sem in self.dsem[q]:
                    if dafter[key] > dbefore[key]:
                        if dbefore[key] > 0:
                            self.eng[q].wait_ge(sem, dbefore[key])
                        self.eng[q].sem_inc(sem, dafter[key] - dbefore[key])
        self.waited = wsnap

    def barrier(self):
        import traceback
        fr = traceback.extract_stack(limit=3)[0]
        self.marks.append((f"{fr.name}:{fr.lineno}", self.cnt["pe"]))
        toks = {}
        for e in self.eng:
            assert not self.pending[e], f"pending unsignaled op on {e}"
            if self.cnt[e] > 0:
                toks[e] = (self.sem[e], self.cnt[e])
        for q in self.dsem:
            for key, sem in self.dsem[q]:
                if self.dval[key] > 0:
                    toks[key] = (sem, self.dval[key])
        for e in self.eng:
            self._wait(e, toks)


class Ring:
    def __init__(self, items):
        self.items = items
        self.i = 0

    def next(self):
        it = self.items[self.i]
        self.i = (self.i + 1) % len(self.items)
        return it


class Ctx:
    pass


_uid = [0]
C_EPS = [None]


def sb(es, nc, name, shape, dt):
    _uid[0] += 1
    t = es.enter_context(nc.sbuf_tensor(f"{name}_{_uid[0]}", list(shape), dt))
    return t, Buf(name)


def sbring(es, nc, name, shape, dt, n):
    return Ring([sb(es, nc, f"{name}{i}", shape, dt) for i in range(n)])


def evac_copy(P, C, out_ap, in_ap, reads, writes, partial=True):
    C.flip ^= 1
    if C.flip:
        P.op("act", lambda e: e.copy(out_ap, in_ap), reads=reads, writes=writes, partial=partial)
    else:
        P.op("dve", lambda e: e.tensor_copy(out_ap, in_ap), reads=reads, writes=writes, partial=partial)


def rstd_from_ss(P, out_ap, in_ap, n, reads, writes):
    P.op("act", lambda e: e.activation(out_ap, in_ap, AF.Sqrt, bias=C_EPS[0][0:out_ap.shape[0], 0:1], scale=1.0 / n), reads=reads, writes=writes)
    P.op("dve", lambda e: e.reciprocal(out_ap, out_ap), reads=writes, writes=writes)


def bcast_row(P, C, row_dram, n, dst, dstb, mul=None):
    rt, rb = C.rowring.next()
    P.dma("sp", rt[0:1, 0:n], row_dram, writes=[rb])
    for c0 in range(0, n, 512):
        w = min(512, n - c0)
        ps, pb = C.psf.next()
        P.op("pe", lambda e: e.matmul(ps[:, 0:w], C.ones_f[0:1, 0:128], rt[0:1, c0:c0 + w], start=True, stop=True),
             reads=[rb, C.constb], writes=[pb])
        if mul is None:
            evac_copy(P, C, dst[:, c0:c0 + w], ps[:, 0:w], [pb], [dstb])
        else:
            P.op("dve", lambda e: e.tensor_scalar(dst[:, c0:c0 + w], ps[:, 0:w], mul, None, ALU.mult),
                 reads=[pb], writes=[dstb], partial=True)


def norm_T(P, C, src, T, grow_dram, dstT, dstTb, f32_cb=None):
    gb_t, gb_b = C.gbc
    bcast_row(P, C, grow_dram, D, gb_t, gb_b)
    for tt in range(T // 128):
        xt, xb = C.xring.next()
        P.dma("sp", xt[:, :], src[tt * 128:(tt + 1) * 128, :], writes=[xb])
        ss, ssb = C.ssring.next()
        xs, xsb = C.xsring.next()
        P.op("act", lambda e: e.activation(xs[:, :], xt[:, :], AF.Square, accum_out=ss[:, 0:1]),
             reads=[xb], writes=[xsb, ssb])
        rstd_from_ss(P, ss[:, 1:2], ss[:, 0:1], D, [ssb], [ssb])
        P.op("dve", lambda e: e.scalar_tensor_tensor(xs[:, :], xt[:, :], ss[:, 1:2], gb_t[:, :], ALU.mult, ALU.mult),
             reads=[xb, ssb, gb_b], writes=[xsb])
        if f32_cb is not None:
            f32_cb(tt, xs, xsb)
        for g4 in range(4):
            ps, pb = C.psf.next()
            for j in range(4):
                kc = g4 * 4 + j
                P.op("pe", lambda e: e.transpose(ps[:, j * 128:(j + 1) * 128], xs[:, kc * 128:(kc + 1) * 128], C.ident_f[:, :]),
                     reads=[xsb, C.constb], writes=[pb], partial=True, signal=(j == 3))
            evac_copy(P, C, dstT[:, g4 * 4:(g4 + 1) * 4, tt * 128:(tt + 1) * 128],
                      ps[:, :].rearrange("p (a b) -> p a b", a=4), [pb], [dstTb])


def load_panel(P, C, wdram, k0, K, c0, w, ring):
    wt, wb = ring.next()
    KC = K // 128
    P.dma("pool", wt[:, 0:KC, 0:w], wdram[k0:k0 + K, c0:c0 + w].rearrange("(kc p) n -> p kc n", p=128), writes=[wb])
    return wt, wb


def proj_fm(P, C, xT, xTb, KC, T, wlist, ncols, M, evac, panel_ring):
    PW = 512
    for p0 in range(0, ncols, PW):
        pw = min(PW, ncols - p0)
        pans = [load_panel(P, C, w, 0, KC * 128, p0, pw, panel_ring) for w in wlist]
        for m0 in range(0, pw, M):
            ci = (p0 + m0) // M
            for t0 in range(0, T, 512):
                tw = min(512, T - t0)
                pss = []
                for (wt, wb) in pans:
                    ps, pb = C.psf.next()
                    for kc in range(KC):
                        P.op("pe", lambda e: e.matmul(ps[0:M, 0:tw], wt[:, kc, m0:m0 + M], xT[:, kc, t0:t0 + tw],
                                                      start=(kc == 0), stop=(kc == KC - 1)),
                             reads=[wb, xTb], writes=[pb], partial=True, signal=(kc == KC - 1))
                    pss.append((ps, pb))
                evac(ci, t0, tw, pss)


def proj_tm(P, C, xT, xTb, KC, T, wdram, ncols, evac, panel_ring, col_of=None):
    for p0 in range(0, ncols, 512):
        pw = min(512, ncols - p0)
        wt, wb = load_panel(P, C, wdram, 0, KC * 128, p0, pw, panel_ring)
        for tt in range(T // 128):
            ps, pb = C.psf.next()
            for kc in range(KC):
                P.op("pe", lambda e: e.matmul(ps[:, 0:pw], xT[:, kc, tt * 128:(tt + 1) * 128], wt[:, kc, 0:pw],
                                              start=(kc == 0), stop=(kc == KC - 1)),
                     reads=[wb, xTb], writes=[pb], partial=True, signal=(kc == KC - 1))
            evac(tt, p0, pw, ps, pb)


def store_fm_bf16(P, C, dst_dram, row0):
    def ev(ci, t0, tw, pss, M=128):
        ps, pb = pss[0]
        st, stb = C.stbf.next()
        evac_copy(P, C, st[0:M, 0:tw], ps[0:M, 0:tw], [pb], [stb], partial=False)
        P.dma("sp", dst_dram[row0 + ci * M: row0 + (ci + 1) * M, t0:t0 + tw], st[0:M, 0:tw], reads=[stb])
    return ev


def residual_linear(P, C, xT, xTb, KC, wdram, h):
    pending = []

    def ev(tt, p0, pw, ps, pb):
        ht, hb = C.hpiece.next()
        P.dma("sp", ht[:, 0:pw], h[tt * 128:(tt + 1) * 128, p0:p0 + pw], writes=[hb])
        if len(pending) >= 2:
            pending.pop(0)()
        P.op("dve", lambda e: e.tensor_tensor(ht[:, 0:pw], ps[:, 0:pw], ht[:, 0:pw], ALU.add), reads=[pb, hb], writes=[hb])
        pending.append(lambda: P.dma("sp", h[tt * 128:(tt + 1) * 128, p0:p0 + pw], ht[:, 0:pw], reads=[hb]))
    proj_tm(P, C, xT, xTb, KC, S, wdram, D, ev, C.panel)
    while pending:
        pending.pop(0)()


def attention_gen(P, C, qt, L, s_terms, v_of, dv, scale, out_cb, opnd_bufs):
    chunks = [(c0, min(512, L - c0)) for c0 in range(0, L, 512)]
    st_t, st_b = C.stat.next()

    def scores(c0, n):
        ps, pb = C.psf.next()
        terms = s_terms(qt, c0, n)
        for i, (lhsT, rhs, off, w) in enumerate(terms):
            P.op("pe", lambda e: e.matmul(ps[:, off:off + w], lhsT, rhs, start=(i == 0), stop=(i == len(terms) - 1),
                                          skip_group_check=True),
                 reads=opnd_bufs, writes=[pb], partial=True, signal=(i == len(terms) - 1))
        return ps, pb

    for ci, (c0, n) in enumerate(chunks):
        ps, pb = scores(c0, n)
        P.op("dve", lambda e: e.reduce_max(st_t[:, ci:ci + 1], ps[:, 0:n], AX.X), reads=[pb], writes=[st_b], partial=True)
        yield
    if len(chunks) > 1:
        P.op("dve", lambda e: e.reduce_max(st_t[:, 4:5], st_t[:, 0:len(chunks)], AX.X), reads=[st_b], writes=[st_b], partial=True)
        mcol = st_t[:, 4:5]
    else:
        mcol = st_t[:, 0:1]
    P.op("dve", lambda e: e.tensor_scalar(st_t[:, 5:6], mcol, -scale, None, ALU.mult), reads=[st_b], writes=[st_b], partial=True)
    o_ps, o_pb = C.pso_free.pop()
    nblk = L // 128
    for ci, (c0, n) in enumerate(chunks):
        ps, pb = scores(c0, n)
        et, eb = C.ering.next()
        P.op("act", lambda e: e.activation(et[:, 0:n], ps[:, 0:n], AF.Exp, bias=st_t[:, 5:6], scale=scale,
                                           accum_out=st_t[:, 6 + ci:7 + ci]),
             reads=[pb, st_b], writes=[eb, st_b], partial=True)
        yield
        tp, tpb = C.psb.next()
        nb = n // 128
        for j in range(nb):
            P.op("pe", lambda e: e.transpose(tp[:, j * 128:(j + 1) * 128], et[:, j * 128:(j + 1) * 128], C.ident_b[:, :]),
                 reads=[eb, C.constb], writes=[tpb], partial=True, signal=(j == nb - 1))
        pt, ptb = C.ptring.next()
        P.op("dve", lambda e: e.tensor_copy(pt[:, 0:n], tp[:, 0:n]), reads=[tpb], writes=[ptb])
        yield
        for j in range(nb):
            kb = c0 // 128 + j
            P.op("pe", lambda e: e.matmul(o_ps[:, 0:dv], pt[:, j * 128:(j + 1) * 128], v_of(kb),
                                          start=(kb == 0), stop=(kb == nblk - 1), skip_group_check=True),
                 reads=[ptb] + opnd_bufs, writes=[o_pb], partial=True, signal=(j == nb - 1))
        yield
    nc_ = len(chunks)
    if nc_ > 1:
        P.op("dve", lambda e: e.reduce_sum(st_t[:, 10:11], st_t[:, 6:6 + nc_], AX.X), reads=[st_b], writes=[st_b], partial=True)
        scol = st_t[:, 10:11]
    else:
        scol = st_t[:, 6:7]
    P.op("dve", lambda e: e.reciprocal(st_t[:, 11:12], scol), reads=[st_b], writes=[st_b], partial=True)
    out_cb(qt, o_ps, o_pb, st_t[:, 11:12], st_b)
    C.pso_free.append((o_ps, o_pb))


def run_gens(gens, width):
    active = []
    it = iter(gens)
    done = False
    while True:
        while not done and len(active) < width:
            g = next(it, None)
            if g is None:
                done = True
                break
            active.append(g)
        if not active:
            break
        for g in list(active):
            try:
                next(g)
            except StopIteration:
                active.remove(g)


class AttnPools:
    def __init__(self, C):
        self.C = C

    def __enter__(self):
        C = self.C
        self.saved = C.psf
        items = C.psf.items
        C.psf = Ring(items[0:3])
        C.pso_free = [items[3], items[4], items[5]]

    def __exit__(self, *a):
        self.C.psf = self.saved
        return False


def attention(P, C, nq, L_of, s_terms, v_of, dv, scale, out_cb, opnd_bufs, width=3):
    run_gens((attention_gen(P, C, qt, L_of(qt), s_terms, v_of, dv, scale, out_cb, opnd_bufs) for qt in range(nq)), width)


def out_T_store(P, C, src, srcb, ncol, dst_dram, row0, qt):
    for c0 in range(0, ncol, 128):
        ps, pb = C.psf.next()
        P.op("pe", lambda e: e.transpose(ps[:, 0:128], src[:, c0:c0 + 128], C.ident_f[:, :]), reads=[srcb, C.constb], writes=[pb])
        st, stb = C.stbf.next()
        evac_copy(P, C, st[:, 0:128], ps[:, 0:128], [pb], [stb], partial=False)
        P.dma("sp", dst_dram[row0 + c0: row0 + c0 + 128, qt * 128:(qt + 1) * 128], st[:, 0:128], reads=[stb])


def load_fm(P, C, dst, dstb, src_dram, row0, nch, T=S):
    for c in range(nch):
        P.dma("sp", dst[:, c, 0:T], src_dram[row0 + c * 128: row0 + (c + 1) * 128, 0:T], writes=[dstb], partial=True)


def cross_attention_layer(P, C, nc, A, layer):
    with ExitStack() as ph:
        qT, qTb = sb(ph, nc, "ca_qT", [128, 4, S], BF16)
        with ExitStack() as ph1:
            hnT, hnTb = sb(ph1, nc, "ca_hnT", [128, 16, S], BF16)
            norm_T(P, C, A.h, S, A.norm_cross_g[layer:layer + 1, :], hnT, hnTb)

            def evq(ci, t0, tw, pss):
                ps, pb = pss[0]
                evac_copy(P, C, qT[:, ci, t0:t0 + tw], ps[:, 0:tw], [pb], [qTb])
            proj_fm(P, C, hnT, hnTb, 16, S, [A.cross_wq[layer]], 512, 128, evq, C.panel)
            P.barrier()
        ocT, ocTb = sb(ph, nc, "ca_ocT", [128, 4, S], BF16)
        osb = sbring(ph, nc, "ca_o", [128, 128], F32, 2)
        sc = 128 ** -0.5
        for hh in range(4):
            def s_terms(qt, c0, n):
                return [(qT[:, hh, qt * 128:(qt + 1) * 128], C.cKT[:, layer, hh, c0:c0 + n], 0, n)]

            def v_of(kb):
                return C.cV[:, layer, kb, hh * 128:(hh + 1) * 128]

            def out_cb(qt, o_ps, o_pb, rinv, rb):
                ot, ob = osb.next()
                P.op("dve", lambda e: e.tensor_scalar(ot[:, :], o_ps[:, 0:128], rinv, None, ALU.mult), reads=[o_pb, rb], writes=[ob])
                ps, pb = C.psf.next()
                P.op("pe", lambda e: e.transpose(ps[:, 0:128], ot[:, :], C.ident_f[:, :]), reads=[ob, C.constb], writes=[pb])
                evac_copy(P, C, ocT[:, hh, qt * 128:(qt + 1) * 128], ps[:, 0:128], [pb], [ocTb])
            with AttnPools(C):
                attention(P, C, NT, lambda qt: 256, s_terms, v_of, 128, sc, out_cb, [qTb, C.cKVb])
        residual_linear(P, C, ocT, ocTb, 4, A.cross_wo[layer], A.h)
        P.barrier()


def ffn_dense(P, C, nc, A, g_row, experts, F, router=None, final=None):
    TB = 512
    NH = 2
    FCh = F // 128 // NH
    assert FCh * NH * 128 == F and FCh % 2 == 0
    with ExitStack() as ph:
        hnT, hnTb = sb(ph, nc, "f_hnT", [128, 16, TB], BF16)
        hT, hTb = sb(ph, nc, "f_hT", [128, FCh, TB], BF16)
        yacc, yb = sb(ph, nc, "f_yacc", [128, 4, D], F32)
        wa = sbring(ph, nc, "f_wa", [128, 16, 256], BF16, 4)
        wbr = sbring(ph, nc, "f_wb", [128, FCh, 128], BF16, 2)
        sg = sbring(ph, nc, "f_sg", [128, TB], F32, 2)
        gates, gb = sb(ph, nc, "f_gates", [128, 4, 8], F32)
        rt, rtb = sb(ph, nc, "f_rt", [128, 32], F32)
        wr, wrb = sb(ph, nc, "f_wr", [128, 16, 8], F32)
        xsT, xsTb = sb(ph, nc, "f_xsT", [128, 16, 128], F32)
        if router is not None:
            P.dma("sp", wr[:, :, :], router.rearrange("(kc p) e -> p kc e", p=128), writes=[wrb])

        def f32_cb(tt, xs, xsb):
            if router is None:
                return
            for g4 in range(4):
                ps, pb = C.psf.next()
                for j in range(4):
                    kc = g4 * 4 + j
                    P.op("pe", lambda e: e.transpose(ps[:, j * 128:(j + 1) * 128], xs[:, kc * 128:(kc + 1) * 128], C.ident_f[:, :]),
                         reads=[xsb, C.constb], writes=[pb], partial=True, signal=(j == 3))
                P.op("dve", lambda e: e.tensor_copy(xsT[:, g4 * 4:(g4 + 1) * 4, :], ps[:, :].rearrange("p (a b) -> p a b", a=4)),
                     reads=[pb], writes=[xsTb], partial=True)
            ps, pb = C.psf.next()
            for kc in range(16):
                P.op("pe", lambda e: e.matmul(ps[:, 0:8], xsT[:, kc, :], wr[:, kc, :], start=(kc == 0), stop=(kc == 15)),
                     reads=[xsTb, wrb], writes=[pb], partial=True, signal=(kc == 15))
            lg = rt[:, 0:8]
            P.op("dve", lambda e: e.tensor_copy(lg, ps[:, 0:8]), reads=[pb], writes=[rtb])
            P.op("dve", lambda e: e.max(rt[:, 8:16], lg), reads=[rtb], writes=[rtb])
            P.op("dve", lambda e: e.tensor_scalar(rt[:, 16:17], rt[:, 8:9], -1.0, None, ALU.mult), reads=[rtb], writes=[rtb])
            P.op("act", lambda e: e.activation(rt[:, 24:32], lg, AF.Exp, bias=rt[:, 16:17], scale=1.0), reads=[rtb], writes=[rtb])
            P.op("dve", lambda e: e.tensor_scalar(rt[:, 0:8], lg, rt[:, 9:10], None, ALU.is_ge), reads=[rtb], writes=[rtb])
            P.op("dve", lambda e: e.tensor_tensor(rt[:, 24:32], rt[:, 24:32], rt[:, 0:8], ALU.mult), reads=[rtb], writes=[rtb])
            P.op("dve", lambda e: e.reduce_sum(rt[:, 17:18], rt[:, 24:32], AX.X), reads=[rtb], writes=[rtb])
            P.op("dve", lambda e: e.reciprocal(rt[:, 18:19], rt[:, 17:18]), reads=[rtb], writes=[rtb])
            P.op("dve", lambda e: e.tensor_scalar(gates[:, tt, :], rt[:, 24:32], rt[:, 18:19], None, ALU.mult),
                 reads=[rtb], writes=[gb], partial=True)

        for blk in range(S // TB):
            t0 = blk * TB
            norm_T(P, C, A.h[t0:t0 + TB, :], TB, g_row, hnT, hnTb, f32_cb=f32_cb)
            first = True
            for ei, (wg, wu, wd) in enumerate(experts):
                for half in range(NH):
                    f0 = half * FCh * 128
                    for fp in range(FCh // 2):
                        gt, gbuf = load_panel(P, C, wg, 0, D, f0 + fp * 256, 256, wa)
                        ut, ubuf = load_panel(P, C, wu, 0, D, f0 + fp * 256, 256, wa)
                        for sub in range(2):
                            fc = fp * 2 + sub
                            psg, pgb = C.psf.next()
                            psu, pub = C.psf.next()
                            for (wt_, wb_, ps_, pb_) in ((gt, gbuf, psg, pgb), (ut, ubuf, psu, pub)):
                                for kc in range(16):
                                    P.op("pe", lambda e: e.matmul(ps_[:, 0:TB], wt_[:, kc, sub * 128:(sub + 1) * 128], hnT[:, kc, :],
                                                                  start=(kc == 0), stop=(kc == 15)),
                                         reads=[wb_, hnTb], writes=[pb_], partial=True, signal=(kc == 15))
                            st, stb = sg.next()
                            P.op("act", lambda e: e.activation(st[:, :], psg[:, 0:TB], AF.Silu), reads=[pgb], writes=[stb])
                            P.op("dve", lambda e: e.tensor_tensor(hT[:, fc, :], st[:, :], psu[:, 0:TB], ALU.mult),
                                 reads=[stb, pub], writes=[hTb], partial=True)
                    for npan in range(D // 128):
                        wt, wb = wbr.next()
                        P.dma("pool", wt[:, :, :],
                              wd[f0:f0 + FCh * 128, npan * 128:(npan + 1) * 128].rearrange("(fc p) n -> p fc n", p=128), writes=[wb])
                        for tt in range(4):
                            ps, pb = C.psf.next()
                            for fc in range(FCh):
                                P.op("pe", lambda e: e.matmul(ps[:, 0:128], hT[:, fc, tt * 128:(tt + 1) * 128], wt[:, fc, :],
                                                              start=(fc == 0), stop=(fc == FCh - 1)),
                                     reads=[wb, hTb], writes=[pb], partial=True, signal=(fc == FCh - 1))
                            ya = yacc[:, tt, npan * 128:(npan + 1) * 128]
                            if router is None:
                                if first:
                                    evac_copy(P, C, ya, ps[:, 0:128], [pb], [yb])
                                else:
                                    P.op("dve", lambda e: e.tensor_tensor(ya, ps[:, 0:128], ya, ALU.add), reads=[pb, yb], writes=[yb], partial=True)
                            elif first:
                                P.op("dve", lambda e: e.tensor_scalar(ya, ps[:, 0:128], gates[:, tt, ei:ei + 1], None, ALU.mult),
                                     reads=[pb, gb], writes=[yb], partial=True)
                            else:
                                P.op("dve", lambda e: e.scalar_tensor_tensor(ya, ps[:, 0:128], gates[:, tt, ei:ei + 1], ya, ALU.mult, ALU.add),
                                     reads=[pb, gb, yb], writes=[yb], partial=True)
                    first = False
            if final is not None:
                bcast_row(P, C, final[2], D, C.gbc[0], C.gbc[1])
            for tt in range(4):
                xt, xb = C.xring.next()
                r0 = t0 + tt * 128
                P.dma("sp", xt[:, :], A.h[r0:r0 + 128, :], writes=[xb])
                P.op("dve", lambda e: e.tensor_tensor(xt[:, :], xt[:, :], yacc[:, tt, :], ALU.add), reads=[xb, yb], writes=[xb])
                if final is None:
                    P.dma("sp", A.h[r0:r0 + 128, :], xt[:, :], reads=[xb])
                else:
                    fgb_t, fgb_b = C.gbc
                    out = final[0]
                    ss, ssb = C.ssring.next()
                    xs, xsb = C.xsring.next()
                    P.op("act", lambda e: e.activation(xs[:, :], xt[:, :], AF.Square, accum_out=ss[:, 0:1]), reads=[xb], writes=[xsb, ssb])
                    rstd_from_ss(P, ss[:, 1:2], ss[:, 0:1], D, [ssb], [ssb])
                    P.op("dve", lambda e: e.scalar_tensor_tensor(xs[:, :], xt[:, :], ss[:, 1:2], fgb_t[:, :], ALU.mult, ALU.mult),
                         reads=[xb, ssb, fgb_b], writes=[xsb])
                    P.dma("sp", out[r0:r0 + 128, :], xs[:, :], reads=[xsb])
        P.barrier()


def moe_sparse(P, C, nc, A, experts, regs):
    TB = 512
    F = DFE
    FC = F // 128
    I32 = mybir.dt.int32
    with ExitStack() as mo:
        gates, gb = sb(mo, nc, "m_gates", [128, 16, 8], F32)
        rkm, rkb = sb(mo, nc, "m_rkm", [128, 16, 8], F32)
        cnt_i, cib = sb(mo, nc, "m_cnti", [1, 8], I32)
        iota, iob = sb(mo, nc, "m_iota", [128, 512], F32)
        P.dma("sp", iota[:, :], A.c_iota, writes=[iob])
        with ExitStack() as ph:
            gbt, gbb = sb(ph, nc, "m_gb", [128, D], F32)
            C.gbc = (gbt, gbb)
            C.rowring = sbring(ph, nc, "m_row", [1, D], F32, 1)
            xring = sbring(ph, nc, "m_xt", [128, D], F32, 2)
            xsr = sbring(ph, nc, "m_xs", [128, D], F32, 1)
            ssr = sbring(ph, nc, "m_ss", [128, 2], F32, 4)
            hbr = sbring(ph, nc, "m_hb", [128, D], BF16, 2)
            xsT, xsTb = sb(ph, nc, "m_xsT", [128, 16, 128], F32)
            wr, wrb = sb(ph, nc, "m_wr", [128, 16, 8], F32)
            rt, rtb = sb(ph, nc, "m_rt", [128, 32], F32)
            sel, selb_ = sb(ph, nc, "m_sel", [128, 16, 8], F32)
            selh, selhb = sb(ph, nc, "m_selh", [128, 16, 8], BF16)
            utri, utb = sb(ph, nc, "m_utri", [128, 128], BF16)
            onesb, onb = sb(ph, nc, "m_onesb", [128, 128], BF16)
            cntf, cfb = sb(ph, nc, "m_cntf", [128, 8], F32)
            P.dma("pool", utri[:, :], A.c_utri, writes=[utb])
            P.op("dve", lambda e: e.memset(onesb[:, :], 1.0), writes=[onb])
            P.dma("sp", wr[:, :, :], A.od_router.rearrange("(kc p) e -> p kc e", p=128), writes=[wrb])
            bcast_row(P, C, A.norm_ffn_g[1:2, :], D, gbt, gbb)
            for tt in range(NT):
                xt, xb = xring.next()
                P.dma("sp", xt[:, :], A.h[tt * 128:(tt + 1) * 128, :], writes=[xb])
                ss, ssb = ssr.next()
                xs, xsb = xsr.next()
                P.op("act", lambda e: e.activation(xs[:, :], xt[:, :], AF.Square, accum_out=ss[:, 0:1]), reads=[xb], writes=[xsb, ssb])
                rstd_from_ss(P, ss[:, 1:2], ss[:, 0:1], D, [ssb], [ssb])
                P.op("dve", lambda e: e.scalar_tensor_tensor(xs[:, :], xt[:, :], ss[:, 1:2], gbt[:, :], ALU.mult, ALU.mult),
                     reads=[xb, ssb, gbb], writes=[xsb])
                hb_t, hb_b = hbr.next()
                P.op("act", lambda e: e.copy(hb_t[:, :], xs[:, :]), reads=[xsb], writes=[hb_b])
                P.dma("sp", A.hn_tm[tt * 128:(tt + 1) * 128, :], hb_t[:, :], reads=[hb_b])
                for g4 in range(4):
                    ps, pb = C.psf.next()
                    for j in range(4):
                        kc = g4 * 4 + j
                        P.op("pe", lambda e: e.transpose(ps[:, j * 128:(j + 1) * 128], xs[:, kc * 128:(kc + 1) * 128], C.ident_f[:, :]),
                             reads=[xsb, C.constb], writes=[pb], partial=True, signal=(j == 3))
                    P.op("dve", lambda e: e.tensor_copy(xsT[:, g4 * 4:(g4 + 1) * 4, :], ps[:, :].rearrange("p (a b) -> p a b", a=4)),
                         reads=[pb], writes=[xsTb], partial=True)
                ps, pb = C.psf.next()
                for kc in range(16):
                    P.op("pe", lambda e: e.matmul(ps[:, 0:8], xsT[:, kc, :], wr[:, kc, :], start=(kc == 0), stop=(kc == 15)),
                         reads=[xsTb, wrb], writes=[pb], partial=True, signal=(kc == 15))
                lg = rt[:, 0:8]
                P.op("dve", lambda e: e.tensor_copy(lg, ps[:, 0:8]), reads=[pb], writes=[rtb])
                P.op("dve", lambda e: e.max(rt[:, 8:16], lg), reads=[rtb], writes=[rtb])
                P.op("dve", lambda e: e.tensor_scalar(rt[:, 16:17], rt[:, 8:9], -1.0, None, ALU.mult), reads=[rtb], writes=[rtb])
                P.op("act", lambda e: e.activation(rt[:, 24:32], lg, AF.Exp, bias=rt[:, 16:17], scale=1.0), reads=[rtb], writes=[rtb])
                P.op("dve", lambda e: e.tensor_scalar(sel[:, tt, :], lg, rt[:, 9:10], None, ALU.is_ge), reads=[rtb], writes=[selb_], partial=True)
                P.op("dve", lambda e: e.tensor_copy(selh[:, tt, :], sel[:, tt, :]), reads=[selb_], writes=[selhb], partial=True)
                P.op("dve", lambda e: e.tensor_tensor(rt[:, 24:32], rt[:, 24:32], sel[:, tt, :], ALU.mult), reads=[rtb, selb_], writes=[rtb])
                P.op("dve", lambda e: e.reduce_sum(rt[:, 17:18], rt[:, 24:32], AX.X), reads=[rtb], writes=[rtb])
                P.op("dve", lambda e: e.reciprocal(rt[:, 18:19], rt[:, 17:18]), reads=[rtb], writes=[rtb])
                P.op("dve", lambda e: e.tensor_scalar(gates[:, tt, :], rt[:, 24:32], rt[:, 18:19], None, ALU.mult),
                     reads=[rtb], writes=[gb], partial=True)
            for tt in range(NT):
                ps, pb = C.psf.next()
                for t2 in range(tt + 1):
                    lhs = utri if t2 == tt else onesb
                    P.op("pe", lambda e: e.matmul(ps[:, 0:8], lhs[:, :], selh[:, t2, :], start=(t2 == 0), stop=(t2 == tt)),
                         reads=[selhb, utb, onb], writes=[pb], partial=True, signal=(t2 == tt))
                P.op("dve", lambda e: e.scalar_tensor_tensor(rkm[:, tt, :], ps[:, 0:8], 1.0, sel[:, tt, :], ALU.add, ALU.mult),
                     reads=[pb, selb_], writes=[rkb], partial=True)
            P.op("dve", lambda e: e.tensor_scalar(rkm[:, :, :], rkm[:, :, :], -1.0, None, ALU.add), reads=[rkb], writes=[rkb])
            ps, pb = C.psf.next()
            for t2 in range(NT):
                P.op("pe", lambda e: e.matmul(ps[:, 0:8], onesb[:, :], selh[:, t2, :], start=(t2 == 0), stop=(t2 == NT - 1)),
                     reads=[selhb, onb], writes=[pb], partial=True, signal=(t2 == NT - 1))
            P.op("dve", lambda e: e.tensor_copy(cntf[:, :], ps[:, 0:8]), reads=[pb], writes=[cfb])
            P.op("dve", lambda e: e.tensor_copy(cnt_i[0:1, :], cntf[0:1, :]), reads=[cfb], writes=[cib])
            P.barrier()
        with ExitStack() as ph:
            OH, ohb = sb(ph, nc, "m_OH", [128, 16, TB], BF16)
            OHT, ohtb = sb(ph, nc, "m_OHT", [128, 4, S], BF16)
            hnT, hnTb = sb(ph, nc, "m_hnT", [128, 16, TB], BF16)
            hT, hTb = sb(ph, nc, "m_hT", [128, FC, TB], BF16)
            ysl, yslb = sb(ph, nc, "m_ysl", [128, 4, D], BF16)
            wa = sbring(ph, nc, "m_wa", [128, 16, 256], BF16, 4)
            wbr = sbring(ph, nc, "m_wb", [128, FC, 128], BF16, 2)
            sg = sbring(ph, nc, "m_sg", [128, TB], F32, 2)
            hnr = sbring(ph, nc, "m_hnr", [128, 512], BF16, 3)
            hn32 = hnT[:, :, :].bitcast(F32)
            rmw = Ring([(hn32[:, 2 * i:2 * i + 2, :], Buf(f"rmw{i}")) for i in range(8)])
            rmw_bufs = [b for (_, b) in rmw.items]
            rka, rkab = sb(ph, nc, "m_rka", [128, 16], F32)
            hdram = [Buf(f"hrow{tt}") for tt in range(NT)]
            for ei, (wg, wu, wd) in enumerate(experts):
                for g in range(S // TB):
                    def body(ei=ei, g=g, wg=wg, wu=wu, wd=wd):
                        P.op("dve", lambda e: e.tensor_scalar(rka[:, :], rkm[:, :, ei], float(-g * TB), None, ALU.add), reads=[rkb], writes=[rkab])
                        for tt in range(NT):
                            P.op("dve", lambda e: e.tensor_scalar(OH[:, tt, :], iota[:, :], rka[:, tt:tt + 1], None, ALU.is_equal),
                                 reads=[iob, rkab], writes=[ohb], partial=True)
                        for q in range(4):
                            acc = [C.psf.next() for _ in range(4)]
                            for tt in range(NT):
                                ht_, hb_ = hnr.next()
                                P.dma("sp", ht_[:, :], A.hn_tm[tt * 128:(tt + 1) * 128, q * 512:(q + 1) * 512], writes=[hb_])
                                for j in range(4):
                                    P.op("pe", lambda e: e.matmul(acc[j][0][:, 0:TB], ht_[:, j * 128:(j + 1) * 128], OH[:, tt, :],
                                                                  start=(tt == 0), stop=(tt == NT - 1)),
                                         reads=[hb_, ohb], writes=[acc[j][1]], partial=True, signal=(j == 3))
                            for j in range(4):
                                evac_copy(P, C, hnT[:, q * 4 + j, :], acc[j][0][:, 0:TB], [acc[j][1]], [hnTb] + rmw_bufs)
                        for fp in range(FC // 2):
                            gt, gbuf = load_panel(P, C, wg, 0, D, fp * 256, 256, wa)
                            ut, ubuf = load_panel(P, C, wu, 0, D, fp * 256, 256, wa)
                            for sub in range(2):
                                fc = fp * 2 + sub
                                psg, pgb = C.psf.next()
                                psu, pub = C.psf.next()
                                for (wt_, wb_, ps_, pb_) in ((gt, gbuf, psg, pgb), (ut, ubuf, psu, pub)):
                                    for kc in range(16):
                                        P.op("pe", lambda e: e.matmul(ps_[:, 0:TB], wt_[:, kc, sub * 128:(sub + 1) * 128], hnT[:, kc, :],
                                                                      start=(kc == 0), stop=(kc == 15)),
                                             reads=[wb_, hnTb], writes=[pb_], partial=True, signal=(kc == 15))
                                st, stb = sg.next()
                                P.op("act", lambda e: e.activation(st[:, :], psg[:, 0:TB], AF.Silu), reads=[pgb], writes=[stb])
                                P.op("dve", lambda e: e.tensor_tensor(hT[:, fc, :], st[:, :], psu[:, 0:TB], ALU.mult),
                                     reads=[stb, pub], writes=[hTb], partial=True)
                        for npan in range(D // 128):
                            wt, wb = wbr.next()
                            P.dma("pool", wt[:, :, :], wd[:, npan * 128:(npan + 1) * 128].rearrange("(fc p) n -> p fc n", p=128), writes=[wb])
                            for blk in range(4):
                                ps, pb = C.psf.next()
                                for fc in range(FC):
                                    P.op("pe", lambda e: e.matmul(ps[:, 0:128], hT[:, fc, blk * 128:(blk + 1) * 128], wt[:, fc, :],
                                                                  start=(fc == 0), stop=(fc == FC - 1)),
                                         reads=[wb, hTb], writes=[pb], partial=True, signal=(fc == FC - 1))
                                evac_copy(P, C, ysl[:, blk, npan * 128:(npan + 1) * 128], ps[:, 0:128], [pb], [yslb])
                        for blk in range(4):
                            for half in range(2):
                                tp, tpb = C.psb.next()
                                for t8 in range(8):
                                    tt = half * 8 + t8
                                    P.op("pe", lambda e: e.transpose(tp[:, t8 * 128:(t8 + 1) * 128], OH[:, tt, blk * 128:(blk + 1) * 128], C.ident_b[:, :]),
                                         reads=[ohb, C.constb], writes=[tpb], partial=True, signal=(t8 == 7))
                                evac_copy(P, C, OHT[:, blk, half * 1024:(half + 1) * 1024], tp[:, 0:1024], [tpb], [ohtb])
                        def hpiece(tt, nt_):
                            return A.h[tt * 128:(tt + 1) * 128, nt_ * 512:(nt_ + 1) * 512].rearrange("p (a b) -> p a b", a=2)

                        def issue_loads(tt):
                            bufs = []
                            for nt_ in range(4):
                                rt_, rb_ = rmw.next()
                                P.dma("sp", rt_, hpiece(tt, nt_), reads=[hdram[tt]], writes=[rb_, hnTb], partial=True)
                                bufs.append((rt_, rb_))
                            return bufs
                        nxt = issue_loads(0)
                        for tt in range(NT):
                            cur = nxt
                            if tt + 1 < NT:
                                nxt = issue_loads(tt + 1)
                            for nt_ in range(4):
                                ps, pb = C.psf.next()
                                for blk in range(4):
                                    P.op("pe", lambda e: e.matmul(ps[:, 0:512], OHT[:, blk, tt * 128:(tt + 1) * 128], ysl[:, blk, nt_ * 512:(nt_ + 1) * 512],
                                                                  start=(blk == 0), stop=(blk == 3)),
                                         reads=[ohtb, yslb], writes=[pb], partial=True, signal=(blk == 3))
                                rt_, rb_ = cur[nt_]
                                P.op("dve", lambda e: e.scalar_tensor_tensor(rt_, ps[:, 0:512].rearrange("p (a b) -> p a b", a=2),
                                                                             gates[:, tt, ei:ei + 1], rt_, ALU.mult, ALU.add),
                                     reads=[pb, gb, rb_], writes=[rb_], partial=True)
                            for nt_ in range(4):
                                rt_, rb_ = cur[nt_]
                                P.dma("sp", hpiece(tt, nt_), rt_, reads=[rb_], writes=[hdram[tt]], partial=True)
                    P.cond_region(regs, cnt_i[0:1, ei:ei + 1], cib, g * TB, body)
            P.barrier()
        with ExitStack() as ph:
            gbt, gbb = sb(ph, nc, "m_fgb", [128, D], F32)
            C.rowring = sbring(ph, nc, "m_row2", [1, D], F32, 1)
            xring = sbring(ph, nc, "m_xt2", [128, D], F32, 2)
            xsr = sbring(ph, nc, "m_xs2", [128, D], F32, 2)
            ssr = sbring(ph, nc, "m_ss2", [128, 2], F32, 4)
            bcast_row(P, C, A.final_norm_g, D, gbt, gbb)
            for tt in range(NT):
                xt, xb = xring.next()
                P.dma("sp", xt[:, :], A.h[tt * 128:(tt + 1) * 128, :], writes=[xb])
                ss, ssb = ssr.next()
                xs, xsb = xsr.next()
                P.op("act", lambda e: e.activation(xs[:, :], xt[:, :], AF.Square, accum_out=ss[:, 0:1]), reads=[xb], writes=[xsb, ssb])
                rstd_from_ss(P, ss[:, 1:2], ss[:, 0:1], D, [ssb], [ssb])
                P.op("dve", lambda e: e.scalar_tensor_tensor(xs[:, :], xt[:, :], ss[:, 1:2], gbt[:, :], ALU.mult, ALU.mult),
                     reads=[xb, ssb, gbb], writes=[xsb])
                P.dma("sp", A.out[tt * 128:(tt + 1) * 128, :], xs[:, :], reads=[xsb])
            P.barrier()


def build_program(debug=False):
    nc = bass.Bass("TRN2", target_bir_lowering=False)
    A = Ctx()

    def din(name, shape, dt=F32):
        return nc.dram_tensor(name, list(shape), dt, kind="ExternalInput").ap()

    A.x = din("x", [S, D])
    A.mem = din("mem", [256, D])
    A.rel_bias = din("rel_bias", [32, 8])
    A.mem_norm_g = din("mem_norm_g", [1, D])
    A.norm_mix_g = din("norm_mix_g", [2, D])
    A.norm_cross_g = din("norm_cross_g", [2, D])
    A.norm_ffn_g = din("norm_ffn_g", [2, D])
    A.cross_wq = din("cross_wq", [2, D, 512])
    A.cross_wkv = din("cross_wkv", [2, D, 1024])
    A.cross_wo = din("cross_wo", [2, 512, D])
    A.ev_w_in = din("ev_w_in", [D, 3136])
    A.ev_w_in_krp = din("ev_w_in_krp", [D, 64])
    A.ev_conv_w = din("ev_conv_w", [248, 128])
    A.pvec = din("pvec", [32, 128])
    A.ev_w_uq_n = din("ev_w_uq_n", [512, 1024])
    A.ev_w_uq_r = din("ev_w_uq_r", [512, 512])
    A.ev_w_uq_rp = din("ev_w_uq_rp", [512, 512])
    A.ev_w_ukv_k = din("ev_w_ukv_k", [512, 1024])
    A.ev_w_ukv_v = din("ev_w_ukv_v", [512, 1024])
    A.ev_w_out = din("ev_w_out", [D, D])
    A.ev_ffn_wg = din("ev_ffn_wg", [D, DFF])
    A.ev_ffn_wu = din("ev_ffn_wu", [D, DFF])
    A.ev_ffn_wd = din("ev_ffn_wd", [DFF, D])
    A.od_w_in = din("od_w_in", [D, 3 * D])
    A.od_lam = din("od_lam", [4, 128])
    A.od_subln_g = din("od_subln_g", [1, 256])
    A.od_w_out = din("od_w_out", [D, D])
    A.od_router = din("od_router", [D, 8])
    A.od_moe_wg = din("od_moe_wg", [NEXP, D, DFE])
    A.od_moe_wu = din("od_moe_wu", [NEXP, D, DFE])
    A.od_moe_wd = din("od_moe_wd", [NEXP, DFE, D])
    A.final_norm_g = din("final_norm_g", [1, D])
    A.c_ident = din("c_ident", [128, 128])
    A.c_anti = din("c_anti", [128, 128])
    A.c_rope = din("c_rope", [128, S])
    A.c_oh = din("c_oh", [32, 384])
    A.c_mrev = din("c_mrev", [128, 256])
    A.c_mrow = din("c_mrow", [2, 128])
    A.c_iota = din("c_iota", [128, 512])
    A.c_utri = din("c_utri", [128, 128])
    A.out = nc.dram_tensor("out", [S, D], F32, kind="ExternalOutput").ap()
    A.h = nc.dram_tensor("h_res", [S, D], F32).ap()
    A.zT = nc.dram_tensor("zT", [3200, S], F32).ap()
    A.catT = nc.dram_tensor("catT", [D, S], BF16).ap()
    A.qT = nc.dram_tensor("qT", [2560, S], BF16).ap()
    A.kT = nc.dram_tensor("kT", [2176, S], BF16).ap()
    A.vtm = nc.dram_tensor("vtm", [S, D], BF16).ap()
    A.tv = nc.dram_tensor("tv", [8, 512], F32).ap()
    A.hn_tm = nc.dram_tensor("hn_tm", [S, D], BF16).ap()
    if debug:
        A.dbg_h = [nc.dram_tensor(f"dbg_h{i}", [S, D], F32, kind="ExternalOutput").ap() for i in range(5)]
        A.dbg_cat = nc.dram_tensor("dbg_cat", [D, S], BF16, kind="ExternalOutput").ap()

    with ExitStack() as es:
        P = Prog(nc, es)
        C = Ctx()
        C.flip = 0
        C.psf = Ring([])
        for i in range(6):
            t = es.enter_context(nc.psum_tensor(f"psf{i}", [128, 512], F32))
            C.psf.items.append((t, Buf(f"psf{i}")))
        C.psb = Ring([])
        for i in range(2):
            t = es.enter_context(nc.psum_tensor(f"psb{i}", [128, 1024], BF16))
            C.psb.items.append((t, Buf(f"psb{i}")))
        C.constb = Buf("const")
        C.ident_f, _ = sb(es, nc, "ident_f", [128, 128], F32)
        C.ident_b, _ = sb(es, nc, "ident_b", [128, 128], BF16)
        C.anti_b, _ = sb(es, nc, "anti_b", [128, 128], BF16)
        C.ones_f, _ = sb(es, nc, "ones_f", [128, 128], F32)
        C.mrow, _ = sb(es, nc, "mrow", [1, 256], BF16)
        C.pcol, _ = sb(es, nc, "pcol", [128, 32], F32)
        C.cwcol, _ = sb(es, nc, "cwcol", [128, 248], F32)
        C.cKT, C.cKVb = sb(es, nc, "cKT", [128, 2, 4, 256], BF16)
        C.cV, _ = sb(es, nc, "cV", [128, 2, 2, 512], BF16)
        C.epsc, _ = sb(es, nc, "epsc", [128, 2], F32)
        cs = ExitStack()
        C.gbc = sb(cs, nc, "gbc", [128, D], F32)
        C.rowring = sbring(cs, nc, "row", [1, D], F32, 1)
        C.xring = sbring(cs, nc, "xt", [128, D], F32, 2)
        C.xsring = sbring(cs, nc, "xs", [128, D], F32, 1)
        C.ssring = sbring(cs, nc, "ss", [128, 2], F32, 4)
        C.stbf = sbring(cs, nc, "stbf", [128, 512], BF16, 4)
        C.stf = sbring(cs, nc, "stf", [128, 512], F32, 2)
        C.hpiece = sbring(cs, nc, "hpc", [128, 512], F32, 4)
        C.stat = sbring(cs, nc, "stat", [128, 16], F32, 6)
        C.ering = sbring(cs, nc, "er", [128, 512], BF16, 4)
        C.ptring = sbring(cs, nc, "ptr", [128, 512], BF16, 4)

        cb = C.constb
        P.dma("sp", C.ident_f[:, :], A.c_ident, writes=[cb], partial=True)
        P.dma("pool", C.ident_b[:, :], A.c_ident, writes=[cb], partial=True)
        P.dma("pool", C.anti_b[:, :], A.c_anti, writes=[cb], partial=True)
        P.dma("pool", C.mrow[0:1, 0:128], A.c_mrow[0:1, :], writes=[cb], partial=True)
        P.dma("pool", C.mrow[0:1, 128:256], A.c_mrow[1:2, :], writes=[cb], partial=True)
        P.op("dve", lambda e: e.memset(C.ones_f[:, :], 1.0), writes=[cb], partial=True)
        P.op("dve", lambda e: e.memset(C.epsc[:, :], EPS), writes=[cb], partial=True)
        C_EPS[0] = C.epsc
        with ExitStack() as ph:
            stg, stgb = sb(ph, nc, "p0stg", [128, 128], F32)
            P.dma("sp", stg[0:32, :], A.pvec, writes=[stgb])
            ps, pb = C.psf.next()
            P.op("pe", lambda e: e.transpose(ps[:, 0:32], stg[0:32, :], C.ident_f[0:32, 0:32]), reads=[stgb, cb], writes=[pb])
            P.op("dve", lambda e: e.tensor_copy(C.pcol[:, :], ps[:, 0:32]), reads=[pb], writes=[cb], partial=True)
            for (r0, nr) in ((0, 128), (128, 120)):
                stg2, stg2b = sb(ph, nc, f"p0stg{r0}", [128, 128], F32)
                P.dma("sp", stg2[0:nr, :], A.ev_conv_w[r0:r0 + nr, :], writes=[stg2b])
                ps, pb = C.psf.next()
                P.op("pe", lambda e: e.transpose(ps[:, 0:nr], stg2[0:nr, :], C.ident_f[0:nr, 0:nr]), reads=[stg2b, cb], writes=[pb])
                P.op("dve", lambda e: e.tensor_copy(C.cwcol[:, r0:r0 + nr], ps[:, 0:nr]), reads=[pb], writes=[cb], partial=True)
            P.barrier()

        with ExitStack() as ph:
            mT, mTb = sb(ph, nc, "memT", [128, 16, 256], BF16)
            panel = sbring(ph, nc, "p1pan", [128, 16, 512], BF16, 2)
            C.panel = panel
            norm_T(P, C, A.mem, 256, A.mem_norm_g, mT, mTb)
            for layer in range(2):
                def evk(ci, t0, tw, pss):
                    ps, pb = pss[0]
                    evac_copy(P, C, C.cKT[:, layer, ci, 0:256], ps[:, 0:256], [pb], [C.cKVb])
                proj_fm(P, C, mT, mTb, 16, 256, [A.cross_wkv[layer][:, 0:512]], 512, 128, evk, panel)

                def evv(tt, p0, pw, ps, pb):
                    evac_copy(P, C, C.cV[:, layer, tt, 0:512], ps[:, 0:512], [pb], [C.cKVb])
                proj_tm(P, C, mT, mTb, 16, 256, A.cross_wkv[layer][:, 512:1024], 512, evv, panel)
            P.barrier()

        hb_ = Buf("hcopy")
        for i in range(4):
            P.dma("sp", A.h[i * 512:(i + 1) * 512, :], A.x[i * 512:(i + 1) * 512, :], writes=[hb_], partial=True)
        P.barrier()

        with ExitStack() as ph:
            hnT, hnTb = sb(ph, nc, "l0_hnT", [128, 16, S], BF16)
            C.panel = sbring(ph, nc, "l0pan", [128, 16, 512], BF16, 2)
            norm_T(P, C, A.h, S, A.norm_mix_g[0:1, :], hnT, hnTb)

            def mk_ev(row0, M):
                def ev(ci, t0, tw, pss):
                    ps, pb = pss[0]
                    st, stb = C.stf.next()
                    evac_copy(P, C, st[0:M, 0:tw], ps[0:M, 0:tw], [pb], [stb], partial=False)
                    P.dma("sp", A.zT[row0 + ci * M: row0 + (ci + 1) * M, t0:t0 + tw], st[0:M, 0:tw], reads=[stb])
                return ev
            proj_fm(P, C, hnT, hnTb, 16, S, [A.ev_w_in[:, 0:3072]], 3072, 128, mk_ev(0, 128), C.panel)
            proj_fm(P, C, hnT, hnTb, 16, S, [A.ev_w_in[:, 3072:3136]], 64, 64, mk_ev(3072, 64), C.panel)
            proj_fm(P, C, hnT, hnTb, 16, S, [A.ev_w_in_krp], 64, 64, mk_ev(3136, 64), C.panel)
            P.barrier()

        with ExitStack() as ph:
            cv, cvb = sb(ph, nc, "cv", [128, 8, S], F32)
            vg = sbring(ph, nc, "vg", [128, S], F32, 4)
            up = sbring(ph, nc, "upad", [128, S + 32], F32, 2)
            sq = sbring(ph, nc, "sq", [128, 512], F32, 2)
            mr, mrb = sb(ph, nc, "mr", [128, 3, 512], F32)
            for cc in range(8):
                vt, vb = vg.next()
                gt, gtb = vg.next()
                P.dma("sp", vt[:, :], A.zT[cc * 128:(cc + 1) * 128, :], writes=[vb])
                P.dma("sp", gt[:, :], A.zT[1024 + cc * 128:1024 + (cc + 1) * 128, :], writes=[gtb])
                P.op("act", lambda e: e.activation(gt[:, :], gt[:, :], AF.Sigmoid), reads=[gtb], writes=[gtb])
                ut, ub = up.next()
                P.op("dve", lambda e: e.memset(ut[:, 0:32], 0.0), writes=[ub])
                P.op("dve", lambda e: e.tensor_tensor(ut[:, 32:32 + S], vt[:, :], gt[:, :], ALU.mult), reads=[vb, gtb, ub], writes=[ub], partial=True)
                for j in range(31):
                    wcol = C.cwcol[:, j * 8 + cc: j * 8 + cc + 1]
                    if j == 0:
                        P.op("dve", lambda e: e.tensor_scalar(cv[:, cc, :], ut[:, 2:2 + S], wcol, C.pcol[:, cc:cc + 1], ALU.mult, ALU.add),
                             reads=[ub, cb], writes=[cvb], partial=True)
                    else:
                        P.op("dve", lambda e: e.scalar_tensor_tensor(cv[:, cc, :], ut[:, 2 + j:2 + j + S], wcol, cv[:, cc, :], ALU.mult, ALU.add),
                             reads=[ub, cb, cvb], writes=[cvb], partial=True)
            for t0 in range(0, S, 512):
                psm, pmb = C.psf.next()
                pss_, psb_ = C.psf.next()
                for cc in range(8):
                    P.op("pe", lambda e: e.matmul(psm[:, :], C.ones_f[:, :], cv[:, cc, t0:t0 + 512], start=(cc == 0), stop=(cc == 7)),
                         reads=[cvb, cb], writes=[pmb], partial=True, signal=(cc == 7))
                for cc in range(8):
                    st, stb = sq.next()
                    P.op("act", lambda e: e.activation(st[:, :], cv[:, cc, t0:t0 + 512], AF.Square), reads=[cvb], writes=[stb])
                    P.op("pe", lambda e: e.matmul(pss_[:, :], C.ones_f[:, :], st[:, :], start=(cc == 0), stop=(cc == 7)),
                         reads=[stb, cb], writes=[psb_], partial=True, signal=True)
                P.op("dve", lambda e: e.tensor_scalar(mr[:, 0, :], psm[:, :], 1.0 / 1024, None, ALU.mult), reads=[pmb], writes=[mrb])
                P.op("dve", lambda e: e.tensor_tensor(mr[:, 1, :], mr[:, 0, :], mr[:, 0, :], ALU.mult), reads=[mrb], writes=[mrb])
                P.op("dve", lambda e: e.scalar_tensor_tensor(mr[:, 1, :], pss_[:, :], 1.0 / 1024, mr[:, 1, :], ALU.mult, ALU.subtract),
                     reads=[psb_, mrb], writes=[mrb])
                P.op("act", lambda e: e.activation(mr[:, 1, :], mr[:, 1, :], AF.Sqrt, bias=C_EPS[0][:, 0:1], scale=1.0), reads=[mrb], writes=[mrb])
                P.op("dve", lambda e: e.reciprocal(mr[:, 1, :], mr[:, 1, :]), reads=[mrb], writes=[mrb])
                for cc in range(8):
                    st, stb = sq.next()
                    P.op("dve", lambda e: e.tensor_tensor(st[:, :], cv[:, cc, t0:t0 + 512], mr[:, 0, :], ALU.subtract), reads=[cvb, mrb], writes=[stb])
                    P.op("dve", lambda e: e.tensor_tensor(st[:, :], st[:, :], mr[:, 1, :], ALU.mult), reads=[stb, mrb], writes=[stb])
                    so, sob = C.stbf.next()
                    P.op("act", lambda e: e.activation(so[:, :], st[:, :], AF.Silu, bias=C.pcol[:, 16 + cc:17 + cc], scale=C.pcol[:, 8 + cc:9 + cc]),
                         reads=[stb, cb], writes=[sob])
                    P.dma("sp", A.catT[cc * 128:(cc + 1) * 128, t0:t0 + 512], so[:, :], reads=[sob])
            P.barrier()

        with ExitStack() as ph:
            cqn, cqnb = sb(ph, nc, "cqn", [128, 4, S], BF16)
            ckvn, ckvnb = sb(ph, nc, "ckvn", [128, 4, S], BF16)
            rope, ropeb = sb(ph, nc, "rope", [64, 2, S], F32)
            sq = sbring(ph, nc, "sq4", [128, 512], F32, 3)
            C.panel = sbring(ph, nc, "p4pan", [128, 4, 512], BF16, 3)
            P.dma("sp", rope[:, 0, :], A.c_rope[0:64, :], writes=[ropeb], partial=True)
            P.dma("sp", rope[:, 1, :], A.c_rope[64:128, :], writes=[ropeb], partial=True)
            for (zrow, dst, dstb, gc0) in ((2048, cqn, cqnb, 24), (2560, ckvn, ckvnb, 28)):
              with ExitStack() as ph2:
                src, srcb = sb(ph2, nc, f"csrc{zrow}", [128, 4, S], F32)
                load_fm(P, C, src, srcb, A.zT, zrow, 4)
                for t0 in range(0, S, 512):
                    ps, pb = C.psf.next()
                    for c in range(4):
                        st, stb = sq.next()
                        P.op("act", lambda e: e.activation(st[:, :], src[:, c, t0:t0 + 512], AF.Square), reads=[srcb], writes=[stb])
                        P.op("pe", lambda e: e.matmul(ps[:, :], C.ones_f[:, :], st[:, :], start=(c == 0), stop=(c == 3)),
                             reads=[stb, cb], writes=[pb], partial=True)
                    rs, rsb = sq.next()
                    rstd_from_ss(P, rs[:, :], ps[:, :], 512, [pb], [rsb])
                    for c in range(4):
                        P.op("dve", lambda e: e.scalar_tensor_tensor(dst[:, c, t0:t0 + 512], src[:, c, t0:t0 + 512], C.pcol[:, gc0 + c:gc0 + c + 1],
                                                                     rs[:, :], ALU.mult, ALU.mult),
                             reads=[srcb, rsb, cb], writes=[dstb], partial=True)
                P.barrier()
            def rope_evac(x_ap, xp_ap, rd, t0, tw, dst_rows):
                a, ab = sq.next()
                b, bb = sq.next()
                P.op("dve", lambda e: e.tensor_tensor(a[0:64, 0:tw], x_ap, rope[:, 0, t0:t0 + tw], ALU.mult), reads=rd + [ropeb], writes=[ab])
                P.op("dve", lambda e: e.tensor_tensor(b[0:64, 0:tw], xp_ap, rope[:, 1, t0:t0 + tw], ALU.mult), reads=rd + [ropeb], writes=[bb])
                so, sob = C.stbf.next()
                P.op("dve", lambda e: e.tensor_tensor(so[0:64, 0:tw], a[0:64, 0:tw], b[0:64, 0:tw], ALU.add), reads=[ab, bb], writes=[sob])
                P.dma("sp", dst_rows[:, t0:t0 + tw], so[0:64, 0:tw], reads=[sob])
            with ExitStack() as ph2:
                kr, krb = sb(ph2, nc, "kr", [64, 2, S], F32)
                P.dma("sp", kr[:, 0, :], A.zT[3072:3136, :], writes=[krb], partial=True)
                P.dma("sp", kr[:, 1, :], A.zT[3136:3200, :], writes=[krb], partial=True)
                for t0 in range(0, S, 512):
                    rope_evac(kr[:, 0, t0:t0 + 512], kr[:, 1, t0:t0 + 512], [krb], t0, 512, A.kT[1024:1088, :])
                P.barrier()
            proj_fm(P, C, cqn, cqnb, 4, S, [A.ev_w_uq_n], 1024, 128, store_fm_bf16(P, C, A.qT, 0), C.panel)

            def ev_qr(ci, t0, tw, pss):
                (p1, b1), (p2, b2) = pss
                rope_evac(p1[0:64, 0:tw], p2[0:64, 0:tw], [b1, b2], t0, tw, A.qT[1024 + ci * 64:1024 + (ci + 1) * 64, :])
            proj_fm(P, C, cqn, cqnb, 4, S, [A.ev_w_uq_r, A.ev_w_uq_rp], 512, 64, ev_qr, C.panel)
            proj_fm(P, C, ckvn, ckvnb, 4, S, [A.ev_w_ukv_k], 1024, 128, store_fm_bf16(P, C, A.kT, 0), C.panel)

            def ev_v(tt, p0, pw, ps, pb):
                st, stb = C.stbf.next()
                evac_copy(P, C, st[:, 0:pw], ps[:, 0:pw], [pb], [stb], partial=False)
                P.dma("sp", A.vtm[tt * 128:(tt + 1) * 128, p0:p0 + pw], st[:, 0:pw], reads=[stb])
            proj_tm(P, C, ckvn, ckvnb, 4, S, A.ev_w_ukv_v, 1024, ev_v, C.panel)
            P.barrier()

        with ExitStack() as ph:
            qn = sbring(ph, nc, "a_qn", [128, S], BF16, 2)
            qr = sbring(ph, nc, "a_qr", [64, S], BF16, 2)
            kn = sbring(ph, nc, "a_kn", [128, S], BF16, 2)
            vv = sbring(ph, nc, "a_v", [128, 16, 128], BF16, 2)
            krt, krtb = sb(ph, nc, "a_kr", [64, S], BF16)
            osb = sbring(ph, nc, "a_o", [128, 128], F32, 2)
            P.dma("sp", krt[:, :], A.kT[1024:1088, :], writes=[krtb])
            sc = 192 ** -0.5
            for hh in range(8):
                qn_t, qn_b = qn.next()
                qr_t, qr_b = qr.next()
                kn_t, kn_b = kn.next()
                v_t, v_b = vv.next()
                P.dma("sp", qn_t[:, :], A.qT[hh * 128:(hh + 1) * 128, :], writes=[qn_b])
                P.dma("sp", qr_t[:, :], A.qT[1024 + hh * 64:1024 + (hh + 1) * 64, :], writes=[qr_b])
                P.dma("sp", kn_t[:, :], A.kT[hh * 128:(hh + 1) * 128, :], writes=[kn_b])
                P.dma("sp", v_t[:, :, :], A.vtm[:, hh * 128:(hh + 1) * 128].rearrange("(kc p) d -> p kc d", p=128), writes=[v_b])

                def s_terms(qt, c0, n):
                    q0 = qt * 128
                    terms = [(qn_t[:, q0:q0 + 128], kn_t[:, c0:c0 + n], 0, n),
                             (qr_t[:, q0:q0 + 128], krt[:, c0:c0 + n], 0, n)]
                    if c0 + n == q0 + 128:
                        terms.append((C.mrow[0:1, 0:128], C.mrow[0:1, 128:256], n - 128, 128))
                    return terms

                def out_cb(qt, o_ps, o_pb, rinv, rb):
                    ot, ob = osb.next()
                    P.op("dve", lambda e: e.tensor_scalar(ot[:, :], o_ps[:, 0:128], rinv, None, ALU.mult), reads=[o_pb, rb], writes=[ob])
                    out_T_store(P, C, ot, ob, 128, A.catT, 1024 + hh * 128, qt)
                with AttnPools(C):
                    attention(P, C, NT, lambda qt: (qt + 1) * 128, s_terms, lambda kb: v_t[:, kb, :], 128, sc, out_cb,
                              [qn_b, qr_b, kn_b, v_b, krtb, cb])
            P.barrier()

        with ExitStack() as ph:
            catS, catSb = sb(ph, nc, "catS", [128, 16, S], BF16)
            C.panel = sbring(ph, nc, "p6pan", [128, 16, 512], BF16, 2)
            load_fm(P, C, catS, catSb, A.catT, 0, 16)
            residual_linear(P, C, catS, catSb, 16, A.ev_w_out, A.h)
            P.barrier()
            if debug:
                P.dma("sp", A.dbg_h[0], A.h, reads=[])
                P.dma("sp", A.dbg_cat, A.catT, reads=[])
                P.barrier()
        with ExitStack() as ph:
            C.panel = sbring(ph, nc, "p7pan", [128, 16, 512], BF16, 2)
            cross_attention_layer(P, C, nc, A, 0)
            if debug:
                P.dma("sp", A.dbg_h[1], A.h, reads=[])
                P.barrier()
        ffn_dense(P, C, nc, A, A.norm_ffn_g[0:1, :], [(A.ev_ffn_wg, A.ev_ffn_wu, A.ev_ffn_wd)], DFF)
        if debug:
            P.dma("sp", A.dbg_h[2], A.h, reads=[])
            P.barrier()

        lambda_init = 0.8 - 0.6 * math.exp(-0.3 * 1)
        with ExitStack() as ph:
            hnT, hnTb = sb(ph, nc, "l1_hnT", [128, 16, S], BF16)
            C.panel = sbring(ph, nc, "l1pan", [128, 16, 512], BF16, 2)
            norm_T(P, C, A.h, S, A.norm_mix_g[1:2, :], hnT, hnTb)
            proj_fm(P, C, hnT, hnTb, 16, S, [A.od_w_in[:, 0:D]], D, 128, store_fm_bf16(P, C, A.qT, 0), C.panel)
            proj_fm(P, C, hnT, hnTb, 16, S, [A.od_w_in[:, D:2 * D]], D, 128, store_fm_bf16(P, C, A.kT, 0), C.panel)

            def ev_v1(tt, p0, pw, ps, pb):
                st, stb = C.stbf.next()
                evac_copy(P, C, st[:, 0:pw], ps[:, 0:pw], [pb], [stb], partial=False)
                P.dma("sp", A.vtm[tt * 128:(tt + 1) * 128, p0:p0 + pw], st[:, 0:pw], reads=[stb])
            proj_tm(P, C, hnT, hnTb, 16, S, A.od_w_in[:, 2 * D:3 * D], D, ev_v1, C.panel)
            P.barrier()

        with ExitStack() as ph:
            sc = 128 ** -0.5
            qh = sbring(ph, nc, "d_q", [128, S], BF16, 4)
            kh = sbring(ph, nc, "d_k", [128, S], BF16, 4)
            vv = sbring(ph, nc, "d_v", [128, 16, 256], BF16, 2)
            brev, brevb = sb(ph, nc, "brev", [128, 8, 256], F32)
            bhi, bhib = sb(ph, nc, "bhi", [128, 8, 256], BF16)
            blo, blob = sb(ph, nc, "blo", [128, 8, 256], BF16)
            mrev, mrevb = sb(ph, nc, "mrev", [128, 256], F32)
            tmpf = sbring(ph, nc, "d_tmp", [128, 256], F32, 8)
            sgb, sgbb = sb(ph, nc, "sgb", [128, 256], F32)
            lam, lamb = sb(ph, nc, "lam", [128, 8], F32)
            lrow, lrowb = sb(ph, nc, "lrow", [1, 4, 128], F32)
            rb32, rb32b = sb(ph, nc, "rb32", [32, 8], F32)
            oh, ohb = sb(ph, nc, "oh", [32, 384], F32)
            vrow, vrowb = sb(ph, nc, "vrow", [8, 384], F32)
            P.dma("sp", rb32[:, :], A.rel_bias, writes=[rb32b])
            P.dma("sp", oh[:, :], A.c_oh, writes=[ohb])
            P.dma("sp", mrev[:, :], A.c_mrev, writes=[mrevb])
            ps, pb = C.psf.next()
            P.op("pe", lambda e: e.matmul(ps[0:8, 0:384], rb32[:, :], oh[:, :], start=True, stop=True), reads=[rb32b, ohb], writes=[pb])
            P.op("dve", lambda e: e.tensor_copy(vrow[:, :], ps[0:8, 0:384]), reads=[pb], writes=[vrowb])
            P.dma("sp", A.tv[:, 0:384], vrow[:, :], reads=[vrowb], writes=[brevb])
            for hh in range(8):
                src = bass.AP(tensor=A.tv.tensor, offset=hh * 512, ap=[[1, 128], [1, 256]])
                P.dma("sp", brev[:, hh, :], src, reads=[brevb], writes=[brevb], partial=True)
            for hh in range(8):
                t1, t1b = tmpf.next()
                P.op("dve", lambda e: e.tensor_scalar(t1[:, :], brev[:, hh, :], brev[:, hh, 0:1], 1.0 / sc, ALU.subtract, ALU.mult),
                     reads=[brevb], writes=[t1b])
                P.op("dve", lambda e: e.tensor_tensor(t1[:, :], t1[:, :], mrev[:, :], ALU.add), reads=[t1b, mrevb], writes=[t1b])
                P.op("dve", lambda e: e.tensor_copy(bhi[:, hh, :], t1[:, :]), reads=[t1b], writes=[bhib], partial=True)
                t2, t2b = tmpf.next()
                P.op("dve", lambda e: e.tensor_copy(t2[:, :], bhi[:, hh, :]), reads=[bhib], writes=[t2b])
                P.op("dve", lambda e: e.tensor_tensor(blo[:, hh, :], t1[:, :], t2[:, :], ALU.subtract), reads=[t1b, t2b], writes=[blob], partial=True)
            P.dma("sp", lrow[0:1, :, :], A.od_lam.rearrange("(o a) d -> o a d", o=1), writes=[lrowb])
            P.op("dve", lambda e: e.tensor_tensor(lrow[0:1, 0, :], lrow[0:1, 0, :], lrow[0:1, 1, :], ALU.mult), reads=[lrowb], writes=[lrowb])
            P.op("dve", lambda e: e.tensor_tensor(lrow[0:1, 2, :], lrow[0:1, 2, :], lrow[0:1, 3, :], ALU.mult), reads=[lrowb], writes=[lrowb])
            P.op("dve", lambda e: e.reduce_sum(lrow[0:1, 1, 0:1], lrow[0:1, 0, :], AX.X), reads=[lrowb], writes=[lrowb])
            P.op("dve", lambda e: e.reduce_sum(lrow[0:1, 1, 1:2], lrow[0:1, 2, :], AX.X), reads=[lrowb], writes=[lrowb])
            P.op("act", lambda e: e.activation(lrow[0:1, 1, 2:4], lrow[0:1, 1, 0:2], AF.Exp), reads=[lrowb], writes=[lrowb])
            P.op("dve", lambda e: e.tensor_tensor(lrow[0:1, 1, 4:5], lrow[0:1, 1, 3:4], lrow[0:1, 1, 2:3], ALU.subtract), reads=[lrowb], writes=[lrowb])
            P.op("dve", lambda e: e.tensor_scalar(lrow[0:1, 1, 4:5], lrow[0:1, 1, 4:5], -lambda_init, None, ALU.add), reads=[lrowb], writes=[lrowb])
            ps, pb = C.psf.next()
            P.op("pe", lambda e: e.matmul(ps[:, 0:1], C.ones_f[0:1, 0:128], lrow[0:1, 1, 4:5], start=True, stop=True), reads=[lrowb, cb], writes=[pb])
            P.op("dve", lambda e: e.tensor_copy(lam[:, 0:1], ps[:, 0:1]), reads=[pb], writes=[lamb])
            bcast_row(P, C, A.od_subln_g, 256, sgb, sgbb, mul=(1.0 - lambda_init))
            for hh in range(8):
                q_t = [qh.next(), qh.next()]
                k_t = [kh.next(), kh.next()]
                v_t, v_b = vv.next()
                for c in range(2):
                    P.dma("sp", q_t[c][0][:, :], A.qT[(hh * 2 + c) * 128:(hh * 2 + c + 1) * 128, :], writes=[q_t[c][1]])
                    P.dma("sp", k_t[c][0][:, :], A.kT[(hh * 2 + c) * 128:(hh * 2 + c + 1) * 128, :], writes=[k_t[c][1]])
                P.dma("sp", v_t[:, :, :], A.vtm[:, hh * 256:(hh + 1) * 256].rearrange("(kc p) d -> p kc d", p=128), writes=[v_b])
                o0 = {}

                def make_spec(c, qq, qb_, kk, kb_):
                    def s_terms(qt, c0, n):
                        q0 = qt * 128
                        terms = [(qq[:, q0:q0 + 128], kk[:, c0:c0 + n], 0, n)]
                        for (kb0, col0) in ((q0 - 128, 0), (q0, 128)):
                            if kb0 >= c0 and kb0 < c0 + n:
                                for bt in (bhi, blo):
                                    terms.append((C.anti_b[:, :], bt[:, hh, col0:col0 + 128], kb0 - c0, 128))
                        return terms

                    def out_cb(qt, o_ps, o_pb, rinv, rb):
                        if c == 0:
                            t0_, t0b = tmpf.next()
                            P.op("dve", lambda e: e.tensor_scalar(t0_[:, :], o_ps[:, 0:256], rinv, None, ALU.mult), reads=[o_pb, rb], writes=[t0b])
                            o0[qt] = (t0_, t0b)
                        else:
                            t0_, t0b = o0[qt]
                            P.op("dve", lambda e: e.tensor_tensor(lam[:, 1:2], rinv, lam[:, 0:1], ALU.mult), reads=[rb, lamb], writes=[lamb])
                            P.op("dve", lambda e: e.scalar_tensor_tensor(t0_[:, :], o_ps[:, 0:256], lam[:, 1:2], t0_[:, :], ALU.mult, ALU.add),
                                 reads=[o_pb, lamb, t0b], writes=[t0b])
                            jk, jkb = tmpf.next()
                            ss, ssb = C.ssring.next()
                            P.op("act", lambda e: e.activation(jk[:, :], t0_[:, :], AF.Square, accum_out=ss[:, 0:1]), reads=[t0b], writes=[jkb, ssb])
                            rstd_from_ss(P, ss[:, 1:2], ss[:, 0:1], 256, [ssb], [ssb])
                            P.op("dve", lambda e: e.scalar_tensor_tensor(t0_[:, :], t0_[:, :], ss[:, 1:2], sgb[:, :], ALU.mult, ALU.mult),
                                 reads=[t0b, ssb, sgbb], writes=[t0b])
                            out_T_store(P, C, t0_, t0b, 256, A.catT, hh * 256, qt)
                    return s_terms, out_cb, [qb_, kb_, v_b, bhib, blob, cb]
                specs = [make_spec(c, q_t[c][0], q_t[c][1], k_t[c][0], k_t[c][1]) for c in range(2)]
                def gens():
                    for qt in range(NT):
                        for c in range(2):
                            s_terms, out_cb, bufs = specs[c]
                            yield attention_gen(P, C, qt, (qt + 1) * 128, s_terms, lambda kb: v_t[:, kb, :], 256, sc, out_cb, bufs)
                with AttnPools(C):
                    run_gens(gens(), 3)
            P.barrier()

        with ExitStack() as ph:
            catS, catSb = sb(ph, nc, "catS1", [128, 16, S], BF16)
            C.panel = sbring(ph, nc, "p11pan", [128, 16, 512], BF16, 2)
            load_fm(P, C, catS, catSb, A.catT, 0, 16)
            residual_linear(P, C, catS, catSb, 16, A.od_w_out, A.h)
            P.barrier()
            if debug:
                P.dma("sp", A.dbg_h[3], A.h, reads=[])
                P.barrier()
        with ExitStack() as ph:
            C.panel = sbring(ph, nc, "p12pan", [128, 16, 512], BF16, 2)
            cross_attention_layer(P, C, nc, A, 1)
            if debug:
                P.dma("sp", A.dbg_h[4], A.h, reads=[])
                P.barrier()
        experts = [(A.od_moe_wg[e], A.od_moe_wu[e], A.od_moe_wd[e]) for e in range(NEXP)]
        P.barrier()
        cs.close()
        regs = nc.alloc_registers("moe_cnt", engines=list(nc.engines.keys()))
        moe_sparse(P, C, nc, A, experts, regs)
        P.barrier()
    nc._marks = P.marks
    return nc


def _t5_bucket(rel):
    half, max_exact = 16, 8
    ret = (rel > 0).astype(np.int32) * half
    n = np.abs(rel)
    nf = np.maximum(n, 1).astype(np.float32)
    large = max_exact + (np.log(nf / max_exact) / math.log(128 / max_exact) * (half - max_exact)).astype(np.int32)
    large = np.minimum(large, half - 1)
    return ret + np.where(n < max_exact, n, large)


def host_constants():
    c = {}
    c["c_ident"] = np.eye(128, dtype=np.float32)
    c["c_anti"] = np.ascontiguousarray(np.eye(128, dtype=np.float32)[::-1])
    pos = np.arange(S, dtype=np.float32)
    inv = np.power(np.float32(10000.0), -np.arange(0, 64, 2, dtype=np.float32) / np.float32(64)).astype(np.float32)
    ang = (pos[None, :] * inv[:, None]).astype(np.float32)
    cs, sn = np.cos(ang).astype(np.float32), np.sin(ang).astype(np.float32)
    c["c_rope"] = np.concatenate([cs, cs, -sn, sn], axis=0).astype(np.float32)
    rel = np.arange(384, dtype=np.int32) - 255
    bk = _t5_bucket(rel)
    oh = np.zeros((32, 384), np.float32)
    oh[bk, np.arange(384)] = 1.0
    oh[:, 383] = 0.0
    c["c_oh"] = oh
    m = np.zeros((128, 256), np.float32)
    m[64:128, 192:256] = NEGM
    c["c_mrev"] = m
    mr = np.zeros((2, 128), np.float32)
    mr[0, 0:64] = 1.0
    mr[1, 64:128] = NEGM
    c["c_mrow"] = mr
    c["c_iota"] = np.ascontiguousarray(np.broadcast_to(np.arange(512, dtype=np.float32)[None, :], (128, 512)))
    c["c_utri"] = np.triu(np.ones((128, 128), np.float32), k=1)
    return c


def host_layout(inp):
    g = {}
    f = lambda a: np.ascontiguousarray(a, dtype=np.float32)
    g["rel_bias"] = f(inp["rel_bias"])
    g["mem_norm_g"] = f(inp["mem_norm_g"]).reshape(1, D)
    for k in ("norm_mix_g", "norm_cross_g", "norm_ffn_g", "cross_wq", "cross_wkv", "cross_wo"):
        g[k] = f(inp[k])
    w_in = f(inp["ev_w_in"][0])
    g["ev_w_in"] = w_in
    kr = w_in[:, 3072:3136]
    g["ev_w_in_krp"] = f(np.concatenate([kr[:, 32:64], kr[:, 0:32]], axis=1))
    g["ev_conv_w"] = f(inp["ev_conv_w"][0]).reshape(248, 128)
    g["pvec"] = f(np.concatenate([inp["ev_conv_b"][0].reshape(8, 128), inp["ev_ln_g"][0].reshape(8, 128),
                                  inp["ev_ln_b"][0].reshape(8, 128), inp["ev_q_norm_g"][0].reshape(4, 128),
                                  inp["ev_kv_norm_g"][0].reshape(4, 128)], axis=0))
    uq = f(inp["ev_w_uq"][0]).reshape(512, 8, 192)
    g["ev_w_uq_n"] = f(uq[:, :, 0:128].reshape(512, 1024))
    g["ev_w_uq_r"] = f(uq[:, :, 128:192].reshape(512, 512))
    g["ev_w_uq_rp"] = f(np.concatenate([uq[:, :, 160:192], uq[:, :, 128:160]], axis=2).reshape(512, 512))
    ukv = f(inp["ev_w_ukv"][0]).reshape(512, 8, 256)
    g["ev_w_ukv_k"] = f(ukv[:, :, 0:128].reshape(512, 1024))
    g["ev_w_ukv_v"] = f(ukv[:, :, 128:256].reshape(512, 1024))
    g["ev_w_out"] = f(inp["ev_w_out"][0])
    g["ev_ffn_wg"] = f(inp["ev_ffn_wg"][0])
    g["ev_ffn_wu"] = f(inp["ev_ffn_wu"][0])
    g["ev_ffn_wd"] = f(inp["ev_ffn_wd"][0])
    g["od_w_in"] = f(inp["od_w_in"][0])
    g["od_lam"] = f(np.concatenate([inp["od_lambda_q1"], inp["od_lambda_k1"], inp["od_lambda_q2"], inp["od_lambda_k2"]], axis=0))
    g["od_subln_g"] = f(inp["od_subln_g"]).reshape(1, 256)
    g["od_w_out"] = f(inp["od_w_out"][0])
    g["od_router"] = f(inp["od_router"][0])
    g["od_moe_wg"] = f(inp["od_moe_wg"][0])
    g["od_moe_wu"] = f(inp["od_moe_wu"][0])
    g["od_moe_wd"] = f(inp["od_moe_wd"][0])
    g["final_norm_g"] = f(inp["final_norm_g"]).reshape(1, D)
    g.update(host_constants())
    return g


def kernel(**inputs):
    n = 8
    shared = host_layout(inputs)
    x = np.ascontiguousarray(inputs["x"], dtype=np.float32)
    mem = np.ascontiguousarray(inputs["mem"], dtype=np.float32)
    nc = build_program()
    in_maps = []
    for b in range(n):
        m = dict(shared)
        m["x"] = x[b]
        m["mem"] = mem[b]
        in_maps.append(m)
    res = run_bass_kernel_spmd(nc, in_maps, core_ids=list(range(n)))
    return np.stack([np.asarray(r["out"], dtype=np.float32) for r in res.results], axis=0)
```

```python
import math
from contextlib import ExitStack
import numpy as np
import concourse.bass as bass
import concourse.mybir as mybir
from concourse.bass_utils import run_bass_kernel_spmd

F32, BF16 = mybir.dt.float32, mybir.dt.bfloat16
ALU = mybir.AluOpType
AF = mybir.ActivationFunctionType
AX = mybir.AxisListType

S = 2048
D = 2048
NT = S // 128
EPS = 1e-6
NEGM = -30000.0
DFF = 5632
DFE = 7168
NEXP = 8


class Buf:
    __slots__ = ("name", "writes", "reads")

    def __init__(self, name=""):
        self.name = name
        self.writes = {}
        self.reads = {}


def _merge(dst, src):
    for k, (s, v) in src.items():
        if k not in dst or dst[k][1] < v:
            dst[k] = (s, v)


class Prog:
    def __init__(self, nc, es, ndma=12):
        self.nc = nc
        self.eng = {"pe": nc.tensor, "act": nc.scalar, "dve": nc.vector, "pool": nc.gpsimd, "sp": nc.sync}
        self.sem = {}
        self.cnt = {}
        self.pending = {}
        self.waited = {e: {} for e in self.eng}
        for e in self.eng:
            self.sem[e] = es.enter_context(nc.semaphore("c_" + e))
            self.cnt[e] = 0
            self.pending[e] = False
        self.dsem = {"sp": [], "pool": []}
        self.dval = {}
        self.dnext = {"sp": 0, "pool": 0}
        for q in ("sp", "pool"):
            for i in range(ndma):
                key = f"d_{q}{i}"
                self.dsem[q].append((key, es.enter_context(nc.semaphore(key))))
                self.dval[key] = 0
        self.ninst = 0
        self.marks = []

    def _wait(self, e, toks):
        w = self.waited[e]
        for key, (sem, val) in toks.items():
            if val <= 0 or (e == "pe" and key == "pe"):
                continue
            if w.get(key, 0) >= val:
                continue
            if key in self.cnt:
                assert val <= self.cnt[key], f"wait on future signal {key} {val}>{self.cnt[key]}"
            self.eng[e].wait_ge(sem, val)
            w[key] = val

    def _deps(self, reads, writes, partial):
        toks = {}
        for b in reads:
            _merge(toks, b.writes)
        for b in writes:
            _merge(toks, b.reads)
            if not partial:
                _merge(toks, b.writes)
        return toks

    def _commit(self, key, tok, reads, writes, partial):
        for b in reads:
            b.reads[key] = tok
        for b in writes:
            if not partial:
                b.writes = {}
                b.reads = {}
            b.writes[key] = tok

    def op(self, e, fn, reads=(), writes=(), signal=True, partial=False):
        self._wait(e, self._deps(reads, writes, partial))
        ins = fn(self.eng[e])
        self.ninst += 1
        if signal:
            self.cnt[e] += 1
            assert self.cnt[e] < 60000, "semaphore count too large"
            ins.then_inc(self.sem[e], 1)
            val = self.cnt[e]
            self.pending[e] = False
        else:
            val = self.cnt[e] + 1
            self.pending[e] = True
        self._commit(e, (self.sem[e], val), reads, writes, partial)

    def dma(self, q, out, in_, reads=(), writes=(), partial=False):
        toks = self._deps(reads, writes, partial)
        pool = self.dsem[q]
        i = self.dnext[q]
        self.dnext[q] = (i + 1) % len(pool)
        key, sem = pool[i]
        prev = self.dval[key]
        toks[key] = (sem, prev)
        self._wait(q, toks)
        self.eng[q].dma_start(out=out, in_=in_).then_inc(sem, 16)
        self.ninst += 1
        self.dval[key] = prev + 16
        assert prev + 16 < 60000
        self._commit(key, (sem, prev + 16), reads, writes, partial)

    def cond_region(self, regs, cnt_ap, cnt_buf, thresh, body):
        for e in self.eng:
            assert not self.pending[e]
        toks = {}
        _merge(toks, cnt_buf.writes)
        for e in self.eng:
            self._wait(e, toks)
        self.nc.regs_load(regs, cnt_ap)
        before = dict(self.cnt)
        dbefore = dict(self.dval)
        dnext = dict(self.dnext)
        wsnap = {e: dict(w) for e, w in self.waited.items()}
        with self.nc.If_cmp(regs, thresh, "IS_GT"):
            body()
            for e in self.eng:
                assert not self.pending[e]
        after = dict(self.cnt)
        dafter = dict(self.dval)
        with self.nc.Else():
            for e in self.eng:
                if after[e] > before[e]:
                    if before[e] > 0:
                        self.eng[e].wait_ge(self.sem[e], before[e])
                    self.eng[e].sem_inc(self.sem[e], after[e] - before[e])
            for q in self.dsem:
                for key, sem in self.dsem[q]:
                    if dafter[key] > dbefore[key]:
                        if dbefore[key] > 0:
                            self.eng[q].wait_ge(sem, dbefore[key])
                        self.eng[q].sem_inc(sem, dafter[key] - dbefore[key])
        self.waited = wsnap

    def barrier(self):
        import traceback
        fr = traceback.extract_stack(limit=3)[0]
        self.marks.append((f"{fr.name}:{fr.lineno}", self.cnt["pe"]))
        toks = {}
        for e in self.eng:
            assert not self.pending[e], f"pending unsignaled op on {e}"
            if self.cnt[e] > 0:
                toks[e] = (self.sem[e], self.cnt[e])
        for q in self.dsem:
            for key, sem in self.dsem[q]:
                if self.dval[key] > 0:
                    toks[key] = (sem, self.dval[key])
        for e in self.eng:
            self._wait(e, toks)


class Ring:
    def __init__(self, items):
        self.items = items
        self.i = 0

    def next(self):
        it = self.items[self.i]
        self.i = (self.i + 1) % len(self.items)
        return it


class Ctx:
    pass


_uid = [0]
C_EPS = [None]


def sb(es, nc, name, shape, dt):
    _uid[0] += 1
    t = es.enter_context(nc.sbuf_tensor(f"{name}_{_uid[0]}", list(shape), dt))
    return t, Buf(name)


def sbring(es, nc, name, shape, dt, n):
    return Ring([sb(es, nc, f"{name}{i}", shape, dt) for i in range(n)])


def evac_copy(P, C, out_ap, in_ap, reads, writes, partial=True):
    C.flip ^= 1
    if C.flip:
        P.op("act", lambda e: e.copy(out_ap, in_ap), reads=reads, writes=writes, partial=partial)
    else:
        P.op("dve", lambda e: e.tensor_copy(out_ap, in_ap), reads=reads, writes=writes, partial=partial)


def rstd_from_ss(P, out_ap, in_ap, n, reads, writes):
    P.op("act", lambda e: e.activation(out_ap, in_ap, AF.Sqrt, bias=C_EPS[0][0:out_ap.shape[0], 0:1], scale=1.0 / n), reads=reads, writes=writes)
    P.op("dve", lambda e: e.reciprocal(out_ap, out_ap), reads=writes, writes=writes)


def bcast_row(P, C, row_dram, n, dst, dstb, mul=None):
    rt, rb = C.rowring.next()
    P.dma("sp", rt[0:1, 0:n], row_dram, writes=[rb])
    for c0 in range(0, n, 512):
        w = min(512, n - c0)
        ps, pb = C.psf.next()
        P.op("pe", lambda e: e.matmul(ps[:, 0:w], C.ones_f[0:1, 0:128], rt[0:1, c0:c0 + w], start=True, stop=True),
             reads=[rb, C.constb], writes=[pb])
        if mul is None:
            evac_copy(P, C, dst[:, c0:c0 + w], ps[:, 0:w], [pb], [dstb])
        else:
            P.op("dve", lambda e: e.tensor_scalar(dst[:, c0:c0 + w], ps[:, 0:w], mul, None, ALU.mult),
                 reads=[pb], writes=[dstb], partial=True)


def norm_T(P, C, src, T, grow_dram, dstT, dstTb, f32_cb=None):
    gb_t, gb_b = C.gbc
    bcast_row(P, C, grow_dram, D, gb_t, gb_b)
    for tt in range(T // 128):
        xt, xb = C.xring.next()
        P.dma("sp", xt[:, :], src[tt * 128:(tt + 1) * 128, :], writes=[xb])
        ss, ssb = C.ssring.next()
        xs, xsb = C.xsring.next()
        P.op("act", lambda e: e.activation(xs[:, :], xt[:, :], AF.Square, accum_out=ss[:, 0:1]),
             reads=[xb], writes=[xsb, ssb])
        rstd_from_ss(P, ss[:, 1:2], ss[:, 0:1], D, [ssb], [ssb])
        P.op("dve", lambda e: e.scalar_tensor_tensor(xs[:, :], xt[:, :], ss[:, 1:2], gb_t[:, :], ALU.mult, ALU.mult),
             reads=[xb, ssb, gb_b], writes=[xsb])
        if f32_cb is not None:
            f32_cb(tt, xs, xsb)
        for g4 in range(4):
            ps, pb = C.psf.next()
            for j in range(4):
                kc = g4 * 4 + j
                P.op("pe", lambda e: e.transpose(ps[:, j * 128:(j + 1) * 128], xs[:, kc * 128:(kc + 1) * 128], C.ident_f[:, :]),
                     reads=[xsb, C.constb], writes=[pb], partial=True, signal=(j == 3))
            evac_copy(P, C, dstT[:, g4 * 4:(g4 + 1) * 4, tt * 128:(tt + 1) * 128],
                      ps[:, :].rearrange("p (a b) -> p a b", a=4), [pb], [dstTb])


def load_panel(P, C, wdram, k0, K, c0, w, ring):
    wt, wb = ring.next()
    KC = K // 128
    P.dma("pool", wt[:, 0:KC, 0:w], wdram[k0:k0 + K, c0:c0 + w].rearrange("(kc p) n -> p kc n", p=128), writes=[wb])
    return wt, wb


def proj_fm(P, C, xT, xTb, KC, T, wlist, ncols, M, evac, panel_ring):
    PW = 512
    for p0 in range(0, ncols, PW):
        pw = min(PW, ncols - p0)
        pans = [load_panel(P, C, w, 0, KC * 128, p0, pw, panel_ring) for w in wlist]
        for m0 in range(0, pw, M):
            ci = (p0 + m0) // M
            for t0 in range(0, T, 512):
                tw = min(512, T - t0)
                pss = []
                for (wt, wb) in pans:
                    ps, pb = C.psf.next()
                    for kc in range(KC):
                        P.op("pe", lambda e: e.matmul(ps[0:M, 0:tw], wt[:, kc, m0:m0 + M], xT[:, kc, t0:t0 + tw],
                                                      start=(kc == 0), stop=(kc == KC - 1)),
                             reads=[wb, xTb], writes=[pb], partial=True, signal=(kc == KC - 1))
                    pss.append((ps, pb))
                evac(ci, t0, tw, pss)


def proj_tm(P, C, xT, xTb, KC, T, wdram, ncols, evac, panel_ring, col_of=None):
    for p0 in range(0, ncols, 512):
        pw = min(512, ncols - p0)
        wt, wb = load_panel(P, C, wdram, 0, KC * 128, p0, pw, panel_ring)
        for tt in range(T // 128):
            ps, pb = C.psf.next()
            for kc in range(KC):
                P.op("pe", lambda e: e.matmul(ps[:, 0:pw], xT[:, kc, tt * 128:(tt + 1) * 128], wt[:, kc, 0:pw],
                                              start=(kc == 0), stop=(kc == KC - 1)),
                     reads=[wb, xTb], writes=[pb], partial=True, signal=(kc == KC - 1))
            evac(tt, p0, pw, ps, pb)


def store_fm_bf16(P, C, dst_dram, row0):
    def ev(ci, t0, tw, pss, M=128):
        ps, pb = pss[0]
        st, stb = C.stbf.next()
        evac_copy(P, C, st[0:M, 0:tw], ps[0:M, 0:tw], [pb], [stb], partial=False)
        P.dma("sp", dst_dram[row0 + ci * M: row0 + (ci + 1) * M, t0:t0 + tw], st[0:M, 0:tw], reads=[stb])
    return ev


def residual_linear(P, C, xT, xTb, KC, wdram, h):
    pending = []

    def ev(tt, p0, pw, ps, pb):
        ht, hb = C.hpiece.next()
        P.dma("sp", ht[:, 0:pw], h[tt * 128:(tt + 1) * 128, p0:p0 + pw], writes=[hb])
        if len(pending) >= 2:
            pending.pop(0)()
        P.op("dve", lambda e: e.tensor_tensor(ht[:, 0:pw], ps[:, 0:pw], ht[:, 0:pw], ALU.add), reads=[pb, hb], writes=[hb])
        pending.append(lambda: P.dma("sp", h[tt * 128:(tt + 1) * 128, p0:p0 + pw], ht[:, 0:pw], reads=[hb]))
    proj_tm(P, C, xT, xTb, KC, S, wdram, D, ev, C.panel)
    while pending:
        pending.pop(0)()


def attention_gen(P, C, qt, L, s_terms, v_of, dv, scale, out_cb, opnd_bufs):
    chunks = [(c0, min(512, L - c0)) for c0 in range(0, L, 512)]
    st_t, st_b = C.stat.next()

    def scores(c0, n):
        ps, pb = C.psf.next()
        terms = s_terms(qt, c0, n)
        for i, (lhsT, rhs, off, w) in enumerate(terms):
            P.op("pe", lambda e: e.matmul(ps[:, off:off + w], lhsT, rhs, start=(i == 0), stop=(i == len(terms) - 1),
                                          skip_group_check=True),
                 reads=opnd_bufs, writes=[pb], partial=True, signal=(i == len(terms) - 1))
        return ps, pb

    for ci, (c0, n) in enumerate(chunks):
        ps, pb = scores(c0, n)
        P.op("dve", lambda e: e.reduce_max(st_t[:, ci:ci + 1], ps[:, 0:n], AX.X), reads=[pb], writes=[st_b], partial=True)
        yield
    if len(chunks) > 1:
        P.op("dve", lambda e: e.reduce_max(st_t[:, 4:5], st_t[:, 0:len(chunks)], AX.X), reads=[st_b], writes=[st_b], partial=True)
        mcol = st_t[:, 4:5]
    else:
        mcol = st_t[:, 0:1]
    P.op("dve", lambda e: e.tensor_scalar(st_t[:, 5:6], mcol, -scale, None, ALU.mult), reads=[st_b], writes=[st_b], partial=True)
    o_ps, o_pb = C.pso_free.pop()
    nblk = L // 128
    for ci, (c0, n) in enumerate(chunks):
        ps, pb = scores(c0, n)
        et, eb = C.ering.next()
        P.op("act", lambda e: e.activation(et[:, 0:n], ps[:, 0:n], AF.Exp, bias=st_t[:, 5:6], scale=scale,
                                           accum_out=st_t[:, 6 + ci:7 + ci]),
             reads=[pb, st_b], writes=[eb, st_b], partial=True)
        yield
        tp, tpb = C.psb.next()
        nb = n // 128
        for j in range(nb):
            P.op("pe", lambda e: e.transpose(tp[:, j * 128:(j + 1) * 128], et[:, j * 128:(j + 1) * 128], C.ident_b[:, :]),
                 reads=[eb, C.constb], writes=[tpb], partial=True, signal=(j == nb - 1))
        pt, ptb = C.ptring.next()
        P.op("dve", lambda e: e.tensor_copy(pt[:, 0:n], tp[:, 0:n]), reads=[tpb], writes=[ptb])
        yield
        for j in range(nb):
            kb = c0 // 128 + j
            P.op("pe", lambda e: e.matmul(o_ps[:, 0:dv], pt[:, j * 128:(j + 1) * 128], v_of(kb),
                                          start=(kb == 0), stop=(kb == nblk - 1), skip_group_check=True),
                 reads=[ptb] + opnd_bufs, writes=[o_pb], partial=True, signal=(j == nb - 1))
        yield
    nc_ = len(chunks)
    if nc_ > 1:
        P.op("dve", lambda e: e.reduce_sum(st_t[:, 10:11], st_t[:, 6:6 + nc_], AX.X), reads=[st_b], writes=[st_b], partial=True)
        scol = st_t[:, 10:11]
    else:
        scol = st_t[:, 6:7]
    P.op("dve", lambda e: e.reciprocal(st_t[:, 11:12], scol), reads=[st_b], writes=[st_b], partial=True)
    out_cb(qt, o_ps, o_pb, st_t[:, 11:12], st_b)
    C.pso_free.append((o_ps, o_pb))


def run_gens(gens, width):
    active = []
    it = iter(gens)
    done = False
    while True:
        while not done and len(active) < width:
            g = next(it, None)
            if g is None:
                done = True
                break
            active.append(g)
        if not active:
            break
        for g in list(active):
            try:
                next(g)
            except StopIteration:
                active.remove(g)


class AttnPools:
    def __init__(self, C):
        self.C = C

    def __enter__(self):
        C = self.C
        self.saved = C.psf
        items = C.psf.items
        C.psf = Ring(items[0:4])
        C.pso_free = [items[4], items[5]]

    def __exit__(self, *a):
        self.C.psf = self.saved
        return False


def attention(P, C, nq, L_of, s_terms, v_of, dv, scale, out_cb, opnd_bufs, width=2):
    run_gens((attention_gen(P, C, qt, L_of(qt), s_terms, v_of, dv, scale, out_cb, opnd_bufs) for qt in range(nq)), width)


def out_T_store(P, C, src, srcb, ncol, dst_dram, row0, qt):
    for c0 in range(0, ncol, 128):
        ps, pb = C.psf.next()
        P.op("pe", lambda e: e.transpose(ps[:, 0:128], src[:, c0:c0 + 128], C.ident_f[:, :]), reads=[srcb, C.constb], writes=[pb])
        st, stb = C.stbf.next()
        evac_copy(P, C, st[:, 0:128], ps[:, 0:128], [pb], [stb], partial=False)
        P.dma("sp", dst_dram[row0 + c0: row0 + c0 + 128, qt * 128:(qt + 1) * 128], st[:, 0:128], reads=[stb])


def load_fm(P, C, dst, dstb, src_dram, row0, nch, T=S):
    for c in range(nch):
        P.dma("sp", dst[:, c, 0:T], src_dram[row0 + c * 128: row0 + (c + 1) * 128, 0:T], writes=[dstb], partial=True)


def cross_attention_layer(P, C, nc, A, layer):
    with ExitStack() as ph:
        qT, qTb = sb(ph, nc, "ca_qT", [128, 4, S], BF16)
        with ExitStack() as ph1:
            hnT, hnTb = sb(ph1, nc, "ca_hnT", [128, 16, S], BF16)
            norm_T(P, C, A.h, S, A.norm_cross_g[layer:layer + 1, :], hnT, hnTb)

            def evq(ci, t0, tw, pss):
                ps, pb = pss[0]
                evac_copy(P, C, qT[:, ci, t0:t0 + tw], ps[:, 0:tw], [pb], [qTb])
            proj_fm(P, C, hnT, hnTb, 16, S, [A.cross_wq[layer]], 512, 128, evq, C.panel)
            P.barrier()
        ocT, ocTb = sb(ph, nc, "ca_ocT", [128, 4, S], BF16)
        osb = sbring(ph, nc, "ca_o", [128, 128], F32, 2)
        sc = 128 ** -0.5
        for hh in range(4):
            def s_terms(qt, c0, n):
                return [(qT[:, hh, qt * 128:(qt + 1) * 128], C.cKT[:, layer, hh, c0:c0 + n], 0, n)]

            def v_of(kb):
                return C.cV[:, layer, kb, hh * 128:(hh + 1) * 128]

            def out_cb(qt, o_ps, o_pb, rinv, rb):
                ot, ob = osb.next()
                P.op("dve", lambda e: e.tensor_scalar(ot[:, :], o_ps[:, 0:128], rinv, None, ALU.mult), reads=[o_pb, rb], writes=[ob])
                ps, pb = C.psf.next()
                P.op("pe", lambda e: e.transpose(ps[:, 0:128], ot[:, :], C.ident_f[:, :]), reads=[ob, C.constb], writes=[pb])
                evac_copy(P, C, ocT[:, hh, qt * 128:(qt + 1) * 128], ps[:, 0:128], [pb], [ocTb])
            with AttnPools(C):
                attention(P, C, NT, lambda qt: 256, s_terms, v_of, 128, sc, out_cb, [qTb, C.cKVb])
        residual_linear(P, C, ocT, ocTb, 4, A.cross_wo[layer], A.h)
        P.barrier()


def ffn_dense(P, C, nc, A, g_row, experts, F, router=None, final=None):
    TB = 512
    NH = 2
    FCh = F // 128 // NH
    assert FCh * NH * 128 == F and FCh % 2 == 0
    with ExitStack() as ph:
        hnT, hnTb = sb(ph, nc, "f_hnT", [128, 16, TB], BF16)
        hT, hTb = sb(ph, nc, "f_hT", [128, FCh, TB], BF16)
        yacc, yb = sb(ph, nc, "f_yacc", [128, 4, D], F32)
        wa = sbring(ph, nc, "f_wa", [128, 16, 256], BF16, 4)
        wbr = sbring(ph, nc, "f_wb", [128, FCh, 128], BF16, 2)
        sg = sbring(ph, nc, "f_sg", [128, TB], F32, 2)
        gates, gb = sb(ph, nc, "f_gates", [128, 4, 8], F32)
        rt, rtb = sb(ph, nc, "f_rt", [128, 32], F32)
        wr, wrb = sb(ph, nc, "f_wr", [128, 16, 8], F32)
        xsT, xsTb = sb(ph, nc, "f_xsT", [128, 16, 128], F32)
        if router is not None:
            P.dma("sp", wr[:, :, :], router.rearrange("(kc p) e -> p kc e", p=128), writes=[wrb])

        def f32_cb(tt, xs, xsb):
            if router is None:
                return
            for g4 in range(4):
                ps, pb = C.psf.next()
                for j in range(4):
                    kc = g4 * 4 + j
                    P.op("pe", lambda e: e.transpose(ps[:, j * 128:(j + 1) * 128], xs[:, kc * 128:(kc + 1) * 128], C.ident_f[:, :]),
                         reads=[xsb, C.constb], writes=[pb], partial=True, signal=(j == 3))
                P.op("dve", lambda e: e.tensor_copy(xsT[:, g4 * 4:(g4 + 1) * 4, :], ps[:, :].rearrange("p (a b) -> p a b", a=4)),
                     reads=[pb], writes=[xsTb], partial=True)
            ps, pb = C.psf.next()
            for kc in range(16):
                P.op("pe", lambda e: e.matmul(ps[:, 0:8], xsT[:, kc, :], wr[:, kc, :], start=(kc == 0), stop=(kc == 15)),
                     reads=[xsTb, wrb], writes=[pb], partial=True, signal=(kc == 15))
            lg = rt[:, 0:8]
            P.op("dve", lambda e: e.tensor_copy(lg, ps[:, 0:8]), reads=[pb], writes=[rtb])
            P.op("dve", lambda e: e.max(rt[:, 8:16], lg), reads=[rtb], writes=[rtb])
            P.op("dve", lambda e: e.tensor_scalar(rt[:, 16:17], rt[:, 8:9], -1.0, None, ALU.mult), reads=[rtb], writes=[rtb])
            P.op("act", lambda e: e.activation(rt[:, 24:32], lg, AF.Exp, bias=rt[:, 16:17], scale=1.0), reads=[rtb], writes=[rtb])
            P.op("dve", lambda e: e.tensor_scalar(rt[:, 0:8], lg, rt[:, 9:10], None, ALU.is_ge), reads=[rtb], writes=[rtb])
            P.op("dve", lambda e: e.tensor_tensor(rt[:, 24:32], rt[:, 24:32], rt[:, 0:8], ALU.mult), reads=[rtb], writes=[rtb])
            P.op("dve", lambda e: e.reduce_sum(rt[:, 17:18], rt[:, 24:32], AX.X), reads=[rtb], writes=[rtb])
            P.op("dve", lambda e: e.reciprocal(rt[:, 18:19], rt[:, 17:18]), reads=[rtb], writes=[rtb])
            P.op("dve", lambda e: e.tensor_scalar(gates[:, tt, :], rt[:, 24:32], rt[:, 18:19], None, ALU.mult),
                 reads=[rtb], writes=[gb], partial=True)

        for blk in range(S // TB):
            t0 = blk * TB
            norm_T(P, C, A.h[t0:t0 + TB, :], TB, g_row, hnT, hnTb, f32_cb=f32_cb)
            first = True
            for ei, (wg, wu, wd) in enumerate(experts):
                for half in range(NH):
                    f0 = half * FCh * 128
                    for fp in range(FCh // 2):
                        gt, gbuf = load_panel(P, C, wg, 0, D, f0 + fp * 256, 256, wa)
                        ut, ubuf = load_panel(P, C, wu, 0, D, f0 + fp * 256, 256, wa)
                        for sub in range(2):
                            fc = fp * 2 + sub
                            psg, pgb = C.psf.next()
                            psu, pub = C.psf.next()
                            for (wt_, wb_, ps_, pb_) in ((gt, gbuf, psg, pgb), (ut, ubuf, psu, pub)):
                                for kc in range(16):
                                    P.op("pe", lambda e: e.matmul(ps_[:, 0:TB], wt_[:, kc, sub * 128:(sub + 1) * 128], hnT[:, kc, :],
                                                                  start=(kc == 0), stop=(kc == 15)),
                                         reads=[wb_, hnTb], writes=[pb_], partial=True, signal=(kc == 15))
                            st, stb = sg.next()
                            P.op("act", lambda e: e.activation(st[:, :], psg[:, 0:TB], AF.Silu), reads=[pgb], writes=[stb])
                            P.op("dve", lambda e: e.tensor_tensor(hT[:, fc, :], st[:, :], psu[:, 0:TB], ALU.mult),
                                 reads=[stb, pub], writes=[hTb], partial=True)
                    for npan in range(D // 128):
                        wt, wb = wbr.next()
                        P.dma("pool", wt[:, :, :],
                              wd[f0:f0 + FCh * 128, npan * 128:(npan + 1) * 128].rearrange("(fc p) n -> p fc n", p=128), writes=[wb])
                        for tt in range(4):
                            ps, pb = C.psf.next()
                            for fc in range(FCh):
                                P.op("pe", lambda e: e.matmul(ps[:, 0:128], hT[:, fc, tt * 128:(tt + 1) * 128], wt[:, fc, :],
                                                              start=(fc == 0), stop=(fc == FCh - 1)),
                                     reads=[wb, hTb], writes=[pb], partial=True, signal=(fc == FCh - 1))
                            ya = yacc[:, tt, npan * 128:(npan + 1) * 128]
                            if router is None:
                                if first:
                                    evac_copy(P, C, ya, ps[:, 0:128], [pb], [yb])
                                else:
                                    P.op("dve", lambda e: e.tensor_tensor(ya, ps[:, 0:128], ya, ALU.add), reads=[pb, yb], writes=[yb], partial=True)
                            elif first:
                                P.op("dve", lambda e: e.tensor_scalar(ya, ps[:, 0:128], gates[:, tt, ei:ei + 1], None, ALU.mult),
                                     reads=[pb, gb], writes=[yb], partial=True)
                            else:
                                P.op("dve", lambda e: e.scalar_tensor_tensor(ya, ps[:, 0:128], gates[:, tt, ei:ei + 1], ya, ALU.mult, ALU.add),
                                     reads=[pb, gb, yb], writes=[yb], partial=True)
                    first = False
            if final is not None:
                bcast_row(P, C, final[2], D, C.gbc[0], C.gbc[1])
            for tt in range(4):
                xt, xb = C.xring.next()
                r0 = t0 + tt * 128
                P.dma("sp", xt[:, :], A.h[r0:r0 + 128, :], writes=[xb])
                P.op("dve", lambda e: e.tensor_tensor(xt[:, :], xt[:, :], yacc[:, tt, :], ALU.add), reads=[xb, yb], writes=[xb])
                if final is None:
                    P.dma("sp", A.h[r0:r0 + 128, :], xt[:, :], reads=[xb])
                else:
                    fgb_t, fgb_b = C.gbc
                    out = final[0]
                    ss, ssb = C.ssring.next()
                    xs, xsb = C.xsring.next()
                    P.op("act", lambda e: e.activation(xs[:, :], xt[:, :], AF.Square, accum_out=ss[:, 0:1]), reads=[xb], writes=[xsb, ssb])
                    rstd_from_ss(P, ss[:, 1:2], ss[:, 0:1], D, [ssb], [ssb])
                    P.op("dve", lambda e: e.scalar_tensor_tensor(xs[:, :], xt[:, :], ss[:, 1:2], fgb_t[:, :], ALU.mult, ALU.mult),
                         reads=[xb, ssb, fgb_b], writes=[xsb])
                    P.dma("sp", out[r0:r0 + 128, :], xs[:, :], reads=[xsb])
        P.barrier()


def moe_sparse(P, C, nc, A, experts, regs):
    TB = 512
    F = DFE
    FC = F // 128
    I32 = mybir.dt.int32
    with ExitStack() as mo:
        gates, gb = sb(mo, nc, "m_gates", [128, 16, 8], F32)
        rkm, rkb = sb(mo, nc, "m_rkm", [128, 16, 8], F32)
        cnt_i, cib = sb(mo, nc, "m_cnti", [1, 8], I32)
        iota, iob = sb(mo, nc, "m_iota", [128, 512], F32)
        P.dma("sp", iota[:, :], A.c_iota, writes=[iob])
        with ExitStack() as ph:
            gbt, gbb = sb(ph, nc, "m_gb", [128, D], F32)
            C.gbc = (gbt, gbb)
            C.rowring = sbring(ph, nc, "m_row", [1, D], F32, 1)
            xring = sbring(ph, nc, "m_xt", [128, D], F32, 2)
            xsr = sbring(ph, nc, "m_xs", [128, D], F32, 1)
            ssr = sbring(ph, nc, "m_ss", [128, 2], F32, 4)
            hbr = sbring(ph, nc, "m_hb", [128, D], BF16, 2)
            xsT, xsTb = sb(ph, nc, "m_xsT", [128, 16, 128], F32)
            wr, wrb = sb(ph, nc, "m_wr", [128, 16, 8], F32)
            rt, rtb = sb(ph, nc, "m_rt", [128, 32], F32)
            sel, selb_ = sb(ph, nc, "m_sel", [128, 16, 8], F32)
            selh, selhb = sb(ph, nc, "m_selh", [128, 16, 8], BF16)
            utri, utb = sb(ph, nc, "m_utri", [128, 128], BF16)
            onesb, onb = sb(ph, nc, "m_onesb", [128, 128], BF16)
            cntf, cfb = sb(ph, nc, "m_cntf", [128, 8], F32)
            P.dma("pool", utri[:, :], A.c_utri, writes=[utb])
            P.op("dve", lambda e: e.memset(onesb[:, :], 1.0), writes=[onb])
            P.dma("sp", wr[:, :, :], A.od_router.rearrange("(kc p) e -> p kc e", p=128), writes=[wrb])
            bcast_row(P, C, A.norm_ffn_g[1:2, :], D, gbt, gbb)
            for tt in range(NT):
                xt, xb = xring.next()
                P.dma("sp", xt[:, :], A.h[tt * 128:(tt + 1) * 128, :], writes=[xb])
                ss, ssb = ssr.next()
                xs, xsb = xsr.next()
                P.op("act", lambda e: e.activation(xs[:, :], xt[:, :], AF.Square, accum_out=ss[:, 0:1]), reads=[xb], writes=[xsb, ssb])
                rstd_from_ss(P, ss[:, 1:2], ss[:, 0:1], D, [ssb], [ssb])
                P.op("dve", lambda e: e.scalar_tensor_tensor(xs[:, :], xt[:, :], ss[:, 1:2], gbt[:, :], ALU.mult, ALU.mult),
                     reads=[xb, ssb, gbb], writes=[xsb])
                hb_t, hb_b = hbr.next()
                P.op("act", lambda e: e.copy(hb_t[:, :], xs[:, :]), reads=[xsb], writes=[hb_b])
                P.dma("sp", A.hn_tm[tt * 128:(tt + 1) * 128, :], hb_t[:, :], reads=[hb_b])
                for g4 in range(4):
                    ps, pb = C.psf.next()
                    for j in range(4):
                        kc = g4 * 4 + j
                        P.op("pe", lambda e: e.transpose(ps[:, j * 128:(j + 1) * 128], xs[:, kc * 128:(kc + 1) * 128], C.ident_f[:, :]),
                             reads=[xsb, C.constb], writes=[pb], partial=True, signal=(j == 3))
                    P.op("dve", lambda e: e.tensor_copy(xsT[:, g4 * 4:(g4 + 1) * 4, :], ps[:, :].rearrange("p (a b) -> p a b", a=4)),
                         reads=[pb], writes=[xsTb], partial=True)
                ps, pb = C.psf.next()
                for kc in range(16):
                    P.op("pe", lambda e: e.matmul(ps[:, 0:8], xsT[:, kc, :], wr[:, kc, :], start=(kc == 0), stop=(kc == 15)),
                         reads=[xsTb, wrb], writes=[pb], partial=True, signal=(kc == 15))
                lg = rt[:, 0:8]
                P.op("dve", lambda e: e.tensor_copy(lg, ps[:, 0:8]), reads=[pb], writes=[rtb])
                P.op("dve", lambda e: e.max(rt[:, 8:16], lg), reads=[rtb], writes=[rtb])
                P.op("dve", lambda e: e.tensor_scalar(rt[:, 16:17], rt[:, 8:9], -1.0, None, ALU.mult), reads=[rtb], writes=[rtb])
                P.op("act", lambda e: e.activation(rt[:, 24:32], lg, AF.Exp, bias=rt[:, 16:17], scale=1.0), reads=[rtb], writes=[rtb])
                P.op("dve", lambda e: e.tensor_scalar(sel[:, tt, :], lg, rt[:, 9:10], None, ALU.is_ge), reads=[rtb], writes=[selb_], partial=True)
                P.op("dve", lambda e: e.tensor_copy(selh[:, tt, :], sel[:, tt, :]), reads=[selb_], writes=[selhb], partial=True)
                P.op("dve", lambda e: e.tensor_tensor(rt[:, 24:32], rt[:, 24:32], sel[:, tt, :], ALU.mult), reads=[rtb, selb_], writes=[rtb])
                P.op("dve", lambda e: e.reduce_sum(rt[:, 17:18], rt[:, 24:32], AX.X), reads=[rtb], writes=[rtb])
                P.op("dve", lambda e: e.reciprocal(rt[:, 18:19], rt[:, 17:18]), reads=[rtb], writes=[rtb])
                P.op("dve", lambda e: e.tensor_scalar(gates[:, tt, :], rt[:, 24:32], rt[:, 18:19], None, ALU.mult),
                     reads=[rtb], writes=[gb], partial=True)
            for tt in range(NT):
                ps, pb = C.psf.next()
                for t2 in range(tt + 1):
                    lhs = utri if t2 == tt else onesb
                    P.op("pe", lambda e: e.matmul(ps[:, 0:8], lhs[:, :], selh[:, t2, :], start=(t2 == 0), stop=(t2 == tt)),
                         reads=[selhb, utb, onb], writes=[pb], partial=True, signal=(t2 == tt))
                P.op("dve", lambda e: e.scalar_tensor_tensor(rkm[:, tt, :], ps[:, 0:8], 1.0, sel[:, tt, :], ALU.add, ALU.mult),
                     reads=[pb, selb_], writes=[rkb], partial=True)
            P.op("dve", lambda e: e.tensor_scalar(rkm[:, :, :], rkm[:, :, :], -1.0, None, ALU.add), reads=[rkb], writes=[rkb])
            ps, pb = C.psf.next()
            for t2 in range(NT):
                P.op("pe", lambda e: e.matmul(ps[:, 0:8], onesb[:, :], selh[:, t2, :], start=(t2 == 0), stop=(t2 == NT - 1)),
                     reads=[selhb, onb], writes=[pb], partial=True, signal=(t2 == NT - 1))
            P.op("dve", lambda e: e.tensor_copy(cntf[:, :], ps[:, 0:8]), reads=[pb], writes=[cfb])
            P.op("dve", lambda e: e.tensor_copy(cnt_i[0:1, :], cntf[0:1, :]), reads=[cfb], writes=[cib])
            P.barrier()
        with ExitStack() as ph:
            OH, ohb = sb(ph, nc, "m_OH", [128, 16, TB], BF16)
            OHT, ohtb = sb(ph, nc, "m_OHT", [128, 4, S], BF16)
            hnT, hnTb = sb(ph, nc, "m_hnT", [128, 16, TB], BF16)
            hT, hTb = sb(ph, nc, "m_hT", [128, FC, TB], BF16)
            ysl, yslb = sb(ph, nc, "m_ysl", [128, 4, D], BF16)
            wa = sbring(ph, nc, "m_wa", [128, 16, 256], BF16, 4)
            wbr = sbring(ph, nc, "m_wb", [128, FC // 2, 256], BF16, 2)
            sg = sbring(ph, nc, "m_sg", [128, TB], F32, 2)
            hnr = sbring(ph, nc, "m_hnr", [128, 512], BF16, 3)
            hn32 = hnT[:, :, :].bitcast(F32)
            rmw = Ring([(hn32[:, 2 * i:2 * i + 2, :], Buf(f"rmw{i}")) for i in range(8)])
            rmw_bufs = [b for (_, b) in rmw.items]
            rka, rkab = sb(ph, nc, "m_rka", [128, 16], F32)
            hdram = [Buf(f"hrow{tt}") for tt in range(NT)]
            for ei, (wg, wu, wd) in enumerate(experts):
                for g in range(S // TB):
                    def body(ei=ei, g=g, wg=wg, wu=wu, wd=wd):
                        P.op("dve", lambda e: e.tensor_scalar(rka[:, :], rkm[:, :, ei], float(-g * TB), None, ALU.add), reads=[rkb], writes=[rkab])
                        for tt in range(NT):
                            P.op("dve", lambda e: e.tensor_scalar(OH[:, tt, :], iota[:, :], rka[:, tt:tt + 1], None, ALU.is_equal),
                                 reads=[iob, rkab], writes=[ohb], partial=True)
                        for q in range(4):
                            acc = [C.psf.next() for _ in range(4)]
                            for tt in range(NT):
                                ht_, hb_ = hnr.next()
                                P.dma("sp", ht_[:, :], A.hn_tm[tt * 128:(tt + 1) * 128, q * 512:(q + 1) * 512], writes=[hb_])
                                for j in range(4):
                                    P.op("pe", lambda e: e.matmul(acc[j][0][:, 0:TB], ht_[:, j * 128:(j + 1) * 128], OH[:, tt, :],
                                                                  start=(tt == 0), stop=(tt == NT - 1)),
                                         reads=[hb_, ohb], writes=[acc[j][1]], partial=True, signal=(j == 3))
                            for j in range(4):
                                evac_copy(P, C, hnT[:, q * 4 + j, :], acc[j][0][:, 0:TB], [acc[j][1]], [hnTb] + rmw_bufs)
                        for fp in range(FC // 2):
                            gt, gbuf = load_panel(P, C, wg, 0, D, fp * 256, 256, wa)
                            ut, ubuf = load_panel(P, C, wu, 0, D, fp * 256, 256, wa)
                            for sub in range(2):
                                fc = fp * 2 + sub
                                psg, pgb = C.psf.next()
                                psu, pub = C.psf.next()
                                for (wt_, wb_, ps_, pb_) in ((gt, gbuf, psg, pgb), (ut, ubuf, psu, pub)):
                                    for kc in range(16):
                                        P.op("pe", lambda e: e.matmul(ps_[:, 0:TB], wt_[:, kc, sub * 128:(sub + 1) * 128], hnT[:, kc, :],
                                                                      start=(kc == 0), stop=(kc == 15)),
                                             reads=[wb_, hnTb], writes=[pb_], partial=True, signal=(kc == 15))
                                st, stb = sg.next()
                                P.op("act", lambda e: e.activation(st[:, :], psg[:, 0:TB], AF.Silu), reads=[pgb], writes=[stb])
                                P.op("dve", lambda e: e.tensor_tensor(hT[:, fc, :], st[:, :], psu[:, 0:TB], ALU.mult),
                                     reads=[stb, pub], writes=[hTb], partial=True)
                        FCq = FC // 2
                        for npan in range(D // 256):
                            n0 = npan * 256
                            accs = [C.psf.next() for _ in range(4)]
                            for half in range(2):
                                wt, wb = wbr.next()
                                P.dma("pool", wt[:, :, :],
                                      wd[half * FCq * 128:(half + 1) * FCq * 128, n0:n0 + 256].rearrange("(fc p) n -> p fc n", p=128), writes=[wb])
                                for blk in range(4):
                                    ps, pb = accs[blk]
                                    for fc in range(FCq):
                                        P.op("pe", lambda e: e.matmul(ps[:, 0:256], hT[:, half * FCq + fc, blk * 128:(blk + 1) * 128], wt[:, fc, :],
                                                                      start=(half == 0 and fc == 0), stop=(half == 1 and fc == FCq - 1)),
                                             reads=[wb, hTb], writes=[pb], partial=True, signal=(fc == FCq - 1))
                            for blk in range(4):
                                ps, pb = accs[blk]
                                evac_copy(P, C, ysl[:, blk, n0:n0 + 256], ps[:, 0:256], [pb], [yslb])
                        for blk in range(4):
                            for half in range(2):
                                tp, tpb = C.psb.next()
                                for t8 in range(8):
                                    tt = half * 8 + t8
                                    P.op("pe", lambda e: e.transpose(tp[:, t8 * 128:(t8 + 1) * 128], OH[:, tt, blk * 128:(blk + 1) * 128], C.ident_b[:, :]),
                                         reads=[ohb, C.constb], writes=[tpb], partial=True, signal=(t8 == 7))
                                evac_copy(P, C, OHT[:, blk, half * 1024:(half + 1) * 1024], tp[:, 0:1024], [tpb], [ohtb])
                        def hpiece(tt, nt_):
                            return A.h[tt * 128:(tt + 1) * 128, nt_ * 512:(nt_ + 1) * 512].rearrange("p (a b) -> p a b", a=2)

                        def issue_loads(tt):
                            bufs = []
                            for nt_ in range(4):
                                rt_, rb_ = rmw.next()
                                P.dma("sp", rt_, hpiece(tt, nt_), reads=[hdram[tt]], writes=[rb_, hnTb], partial=True)
                                bufs.append((rt_, rb_))
                            return bufs
                        nxt = issue_loads(0)
                        for tt in range(NT):
                            cur = nxt
                            if tt + 1 < NT:
                                nxt = issue_loads(tt + 1)
                            for nt_ in range(4):
                                ps, pb = C.psf.next()
                                for blk in range(4):
                                    P.op("pe", lambda e: e.matmul(ps[:, 0:512], OHT[:, blk, tt * 128:(tt + 1) * 128], ysl[:, blk, nt_ * 512:(nt_ + 1) * 512],
                                                                  start=(blk == 0), stop=(blk == 3)),
                                         reads=[ohtb, yslb], writes=[pb], partial=True, signal=(blk == 3))
                                rt_, rb_ = cur[nt_]
                                P.op("dve", lambda e: e.scalar_tensor_tensor(rt_, ps[:, 0:512].rearrange("p (a b) -> p a b", a=2),
                                                                             gates[:, tt, ei:ei + 1], rt_, ALU.mult, ALU.add),
                                     reads=[pb, gb, rb_], writes=[rb_], partial=True)
                            for nt_ in range(4):
                                rt_, rb_ = cur[nt_]
                                P.dma("sp", hpiece(tt, nt_), rt_, reads=[rb_], writes=[hdram[tt]], partial=True)
                    P.cond_region(regs, cnt_i[0:1, ei:ei + 1], cib, g * TB, body)
            P.barrier()
        with ExitStack() as ph:
            gbt, gbb = sb(ph, nc, "m_fgb", [128, D], F32)
            C.rowring = sbring(ph, nc, "m_row2", [1, D], F32, 1)
            xring = sbring(ph, nc, "m_xt2", [128, D], F32, 2)
            xsr = sbring(ph, nc, "m_xs2", [128, D], F32, 2)
            ssr = sbring(ph, nc, "m_ss2", [128, 2], F32, 4)
            bcast_row(P, C, A.final_norm_g, D, gbt, gbb)
            for tt in range(NT):
                xt, xb = xring.next()
                P.dma("sp", xt[:, :], A.h[tt * 128:(tt + 1) * 128, :], writes=[xb])
                ss, ssb = ssr.next()
                xs, xsb = xsr.next()
                P.op("act", lambda e: e.activation(xs[:, :], xt[:, :], AF.Square, accum_out=ss[:, 0:1]), reads=[xb], writes=[xsb, ssb])
                rstd_from_ss(P, ss[:, 1:2], ss[:, 0:1], D, [ssb], [ssb])
                P.op("dve", lambda e: e.scalar_tensor_tensor(xs[:, :], xt[:, :], ss[:, 1:2], gbt[:, :], ALU.mult, ALU.mult),
                     reads=[xb, ssb, gbb], writes=[xsb])
                P.dma("sp", A.out[tt * 128:(tt + 1) * 128, :], xs[:, :], reads=[xsb])
            P.barrier()


def build_program(debug=False):
    nc = bass.Bass("TRN2", target_bir_lowering=False)
    A = Ctx()

    def din(name, shape, dt=F32):
        return nc.dram_tensor(name, list(shape), dt, kind="ExternalInput").ap()

    A.x = din("x", [S, D])
    A.mem = din("mem", [256, D])
    A.rel_bias = din("rel_bias", [32, 8])
    A.mem_norm_g = din("mem_norm_g", [1, D])
    A.norm_mix_g = din("norm_mix_g", [2, D])
    A.norm_cross_g = din("norm_cross_g", [2, D])
    A.norm_ffn_g = din("norm_ffn_g", [2, D])
    A.cross_wq = din("cross_wq", [2, D, 512])
    A.cross_wkv = din("cross_wkv", [2, D, 1024])
    A.cross_wo = din("cross_wo", [2, 512, D])
    A.ev_w_in = din("ev_w_in", [D, 3136])
    A.ev_w_in_krp = din("ev_w_in_krp", [D, 64])
    A.ev_conv_w = din("ev_conv_w", [248, 128])
    A.pvec = din("pvec", [32, 128])
    A.ev_w_uq_n = din("ev_w_uq_n", [512, 1024])
    A.ev_w_uq_r = din("ev_w_uq_r", [512, 512])
    A.ev_w_uq_rp = din("ev_w_uq_rp", [512, 512])
    A.ev_w_ukv_k = din("ev_w_ukv_k", [512, 1024])
    A.ev_w_ukv_v = din("ev_w_ukv_v", [512, 1024])
    A.ev_w_out = din("ev_w_out", [D, D])
    A.ev_ffn_wg = din("ev_ffn_wg", [D, DFF])
    A.ev_ffn_wu = din("ev_ffn_wu", [D, DFF])
    A.ev_ffn_wd = din("ev_ffn_wd", [DFF, D])
    A.od_w_in = din("od_w_in", [D, 3 * D])
    A.od_lam = din("od_lam", [4, 128])
    A.od_subln_g = din("od_subln_g", [1, 256])
    A.od_w_out = din("od_w_out", [D, D])
    A.od_router = din("od_router", [D, 8])
    A.od_moe_wg = din("od_moe_wg", [NEXP, D, DFE])
    A.od_moe_wu = din("od_moe_wu", [NEXP, D, DFE])
    A.od_moe_wd = din("od_moe_wd", [NEXP, DFE, D])
    A.final_norm_g = din("final_norm_g", [1, D])
    A.c_ident = din("c_ident", [128, 128])
    A.c_anti = din("c_anti", [128, 128])
    A.c_rope = din("c_rope", [128, S])
    A.c_oh = din("c_oh", [32, 384])
    A.c_mrev = din("c_mrev", [128, 256])
    A.c_mrow = din("c_mrow", [2, 128])
    A.c_iota = din("c_iota", [128, 512])
    A.c_utri = din("c_utri", [128, 128])
    A.out = nc.dram_tensor("out", [S, D], F32, kind="ExternalOutput").ap()
    A.h = nc.dram_tensor("h_res", [S, D], F32).ap()
    A.zT = nc.dram_tensor("zT", [3200, S], F32).ap()
    A.catT = nc.dram_tensor("catT", [D, S], BF16).ap()
    A.qT = nc.dram_tensor("qT", [2560, S], BF16).ap()
    A.kT = nc.dram_tensor("kT", [2176, S], BF16).ap()
    A.vtm = nc.dram_tensor("vtm", [S, D], BF16).ap()
    A.tv = nc.dram_tensor("tv", [8, 512], F32).ap()
    A.hn_tm = nc.dram_tensor("hn_tm", [S, D], BF16).ap()
    if debug:
        A.dbg_h = [nc.dram_tensor(f"dbg_h{i}", [S, D], F32, kind="ExternalOutput").ap() for i in range(5)]
        A.dbg_cat = nc.dram_tensor("dbg_cat", [D, S], BF16, kind="ExternalOutput").ap()

    with ExitStack() as es:
        P = Prog(nc, es)
        C = Ctx()
        C.flip = 0
        C.psf = Ring([])
        for i in range(6):
            t = es.enter_context(nc.psum_tensor(f"psf{i}", [128, 512], F32))
            C.psf.items.append((t, Buf(f"psf{i}")))
        C.psb = Ring([])
        for i in range(2):
            t = es.enter_context(nc.psum_tensor(f"psb{i}", [128, 1024], BF16))
            C.psb.items.append((t, Buf(f"psb{i}")))
        C.constb = Buf("const")
        C.ident_f, _ = sb(es, nc, "ident_f", [128, 128], F32)
        C.ident_b, _ = sb(es, nc, "ident_b", [128, 128], BF16)
        C.anti_b, _ = sb(es, nc, "anti_b", [128, 128], BF16)
        C.ones_f, _ = sb(es, nc, "ones_f", [128, 128], F32)
        C.mrow, _ = sb(es, nc, "mrow", [1, 256], BF16)
        C.pcol, _ = sb(es, nc, "pcol", [128, 32], F32)
        C.cwcol, _ = sb(es, nc, "cwcol", [128, 248], F32)
        C.cKT, C.cKVb = sb(es, nc, "cKT", [128, 2, 4, 256], BF16)
        C.cV, _ = sb(es, nc, "cV", [128, 2, 2, 512], BF16)
        C.epsc, _ = sb(es, nc, "epsc", [128, 2], F32)
        cs = ExitStack()
        C.gbc = sb(cs, nc, "gbc", [128, D], F32)
        C.rowring = sbring(cs, nc, "row", [1, D], F32, 1)
        C.xring = sbring(cs, nc, "xt", [128, D], F32, 2)
        C.xsring = sbring(cs, nc, "xs", [128, D], F32, 1)
        C.ssring = sbring(cs, nc, "ss", [128, 2], F32, 4)
        C.stbf = sbring(cs, nc, "stbf", [128, 512], BF16, 4)
        C.stf = sbring(cs, nc, "stf", [128, 512], F32, 2)
        C.hpiece = sbring(cs, nc, "hpc", [128, 512], F32, 4)
        C.stat = sbring(cs, nc, "stat", [128, 16], F32, 6)
        C.ering = sbring(cs, nc, "er", [128, 512], BF16, 4)
        C.ptring = sbring(cs, nc, "ptr", [128, 512], BF16, 4)

        cb = C.constb
        P.dma("sp", C.ident_f[:, :], A.c_ident, writes=[cb], partial=True)
        P.dma("pool", C.ident_b[:, :], A.c_ident, writes=[cb], partial=True)
        P.dma("pool", C.anti_b[:, :], A.c_anti, writes=[cb], partial=True)
        P.dma("pool", C.mrow[0:1, 0:128], A.c_mrow[0:1, :], writes=[cb], partial=True)
        P.dma("pool", C.mrow[0:1, 128:256], A.c_mrow[1:2, :], writes=[cb], partial=True)
        P.op("dve", lambda e: e.memset(C.ones_f[:, :], 1.0), writes=[cb], partial=True)
        P.op("dve", lambda e: e.memset(C.epsc[:, :], EPS), writes=[cb], partial=True)
        C_EPS[0] = C.epsc
        with ExitStack() as ph:
            stg, stgb = sb(ph, nc, "p0stg", [128, 128], F32)
            P.dma("sp", stg[0:32, :], A.pvec, writes=[stgb])
            ps, pb = C.psf.next()
            P.op("pe", lambda e: e.transpose(ps[:, 0:32], stg[0:32, :], C.ident_f[0:32, 0:32]), reads=[stgb, cb], writes=[pb])
            P.op("dve", lambda e: e.tensor_copy(C.pcol[:, :], ps[:, 0:32]), reads=[pb], writes=[cb], partial=True)
            for (r0, nr) in ((0, 128), (128, 120)):
                stg2, stg2b = sb(ph, nc, f"p0stg{r0}", [128, 128], F32)
                P.dma("sp", stg2[0:nr, :], A.ev_conv_w[r0:r0 + nr, :], writes=[stg2b])
                ps, pb = C.psf.next()
                P.op("pe", lambda e: e.transpose(ps[:, 0:nr], stg2[0:nr, :], C.ident_f[0:nr, 0:nr]), reads=[stg2b, cb], writes=[pb])
                P.op("dve", lambda e: e.tensor_copy(C.cwcol[:, r0:r0 + nr], ps[:, 0:nr]), reads=[pb], writes=[cb], partial=True)
            P.barrier()

        with ExitStack() as ph:
            mT, mTb = sb(ph, nc, "memT", [128, 16, 256], BF16)
            panel = sbring(ph, nc, "p1pan", [128, 16, 512], BF16, 2)
            C.panel = panel
            norm_T(P, C, A.mem, 256, A.mem_norm_g, mT, mTb)
            for layer in range(2):
                def evk(ci, t0, tw, pss):
                    ps, pb = pss[0]
                    evac_copy(P, C, C.cKT[:, layer, ci, 0:256], ps[:, 0:256], [pb], [C.cKVb])
                proj_fm(P, C, mT, mTb, 16, 256, [A.cross_wkv[layer][:, 0:512]], 512, 128, evk, panel)

                def evv(tt, p0, pw, ps, pb):
                    evac_copy(P, C, C.cV[:, layer, tt, 0:512], ps[:, 0:512], [pb], [C.cKVb])
                proj_tm(P, C, mT, mTb, 16, 256, A.cross_wkv[layer][:, 512:1024], 512, evv, panel)
            P.barrier()

        hb_ = Buf("hcopy")
        for i in range(4):
            P.dma("sp", A.h[i * 512:(i + 1) * 512, :], A.x[i * 512:(i + 1) * 512, :], writes=[hb_], partial=True)
        P.barrier()

        with ExitStack() as ph:
            hnT, hnTb = sb(ph, nc, "l0_hnT", [128, 16, S], BF16)
            C.panel = sbring(ph, nc, "l0pan", [128, 16, 512], BF16, 2)
            norm_T(P, C, A.h, S, A.norm_mix_g[0:1, :], hnT, hnTb)

            def mk_ev(row0, M):
                def ev(ci, t0, tw, pss):
                    ps, pb = pss[0]
                    st, stb = C.stf.next()
                    evac_copy(P, C, st[0:M, 0:tw], ps[0:M, 0:tw], [pb], [stb], partial=False)
                    P.dma("sp", A.zT[row0 + ci * M: row0 + (ci + 1) * M, t0:t0 + tw], st[0:M, 0:tw], reads=[stb])
                return ev
            proj_fm(P, C, hnT, hnTb, 16, S, [A.ev_w_in[:, 0:3072]], 3072, 128, mk_ev(0, 128), C.panel)
            proj_fm(P, C, hnT, hnTb, 16, S, [A.ev_w_in[:, 3072:3136]], 64, 64, mk_ev(3072, 64), C.panel)
            proj_fm(P, C, hnT, hnTb, 16, S, [A.ev_w_in_krp], 64, 64, mk_ev(3136, 64), C.panel)
            P.barrier()

        with ExitStack() as ph:
            cv, cvb = sb(ph, nc, "cv", [128, 8, S], F32)
            vg = sbring(ph, nc, "vg", [128, S], F32, 4)
            up = sbring(ph, nc, "upad", [128, S + 32], F32, 2)
            sq = sbring(ph, nc, "sq", [128, 512], F32, 2)
            mr, mrb = sb(ph, nc, "mr", [128, 3, 512], F32)
            for cc in range(8):
                vt, vb = vg.next()
                gt, gtb = vg.next()
                P.dma("sp", vt[:, :], A.zT[cc * 128:(cc + 1) * 128, :], writes=[vb])
                P.dma("sp", gt[:, :], A.zT[1024 + cc * 128:1024 + (cc + 1) * 128, :], writes=[gtb])
                P.op("act", lambda e: e.activation(gt[:, :], gt[:, :], AF.Sigmoid), reads=[gtb], writes=[gtb])
                ut, ub = up.next()
                P.op("dve", lambda e: e.memset(ut[:, 0:32], 0.0), writes=[ub])
                P.op("dve", lambda e: e.tensor_tensor(ut[:, 32:32 + S], vt[:, :], gt[:, :], ALU.mult), reads=[vb, gtb, ub], writes=[ub], partial=True)
                for j in range(31):
                    wcol = C.cwcol[:, j * 8 + cc: j * 8 + cc + 1]
                    if j == 0:
                        P.op("dve", lambda e: e.tensor_scalar(cv[:, cc, :], ut[:, 2:2 + S], wcol, C.pcol[:, cc:cc + 1], ALU.mult, ALU.add),
                             reads=[ub, cb], writes=[cvb], partial=True)
                    else:
                        P.op("dve", lambda e: e.scalar_tensor_tensor(cv[:, cc, :], ut[:, 2 + j:2 + j + S], wcol, cv[:, cc, :], ALU.mult, ALU.add),
                             reads=[ub, cb, cvb], writes=[cvb], partial=True)
            for t0 in range(0, S, 512):
                psm, pmb = C.psf.next()
                pss_, psb_ = C.psf.next()
                for cc in range(8):
                    P.op("pe", lambda e: e.matmul(psm[:, :], C.ones_f[:, :], cv[:, cc, t0:t0 + 512], start=(cc == 0), stop=(cc == 7)),
                         reads=[cvb, cb], writes=[pmb], partial=True, signal=(cc == 7))
                for cc in range(8):
                    st, stb = sq.next()
                    P.op("act", lambda e: e.activation(st[:, :], cv[:, cc, t0:t0 + 512], AF.Square), reads=[cvb], writes=[stb])
                    P.op("pe", lambda e: e.matmul(pss_[:, :], C.ones_f[:, :], st[:, :], start=(cc == 0), stop=(cc == 7)),
                         reads=[stb, cb], writes=[psb_], partial=True, signal=True)
                P.op("dve", lambda e: e.tensor_scalar(mr[:, 0, :], psm[:, :], 1.0 / 1024, None, ALU.mult), reads=[pmb], writes=[mrb])
                P.op("dve", lambda e: e.tensor_tensor(mr[:, 1, :], mr[:, 0, :], mr[:, 0, :], ALU.mult), reads=[mrb], writes=[mrb])
                P.op("dve", lambda e: e.scalar_tensor_tensor(mr[:, 1, :], pss_[:, :], 1.0 / 1024, mr[:, 1, :], ALU.mult, ALU.subtract),
                     reads=[psb_, mrb], writes=[mrb])
                P.op("act", lambda e: e.activation(mr[:, 1, :], mr[:, 1, :], AF.Sqrt, bias=C_EPS[0][:, 0:1], scale=1.0), reads=[mrb], writes=[mrb])
                P.op("dve", lambda e: e.reciprocal(mr[:, 1, :], mr[:, 1, :]), reads=[mrb], writes=[mrb])
                for cc in range(8):
                    st, stb = sq.next()
                    P.op("dve", lambda e: e.tensor_tensor(st[:, :], cv[:, cc, t0:t0 + 512], mr[:, 0, :], ALU.subtract), reads=[cvb, mrb], writes=[stb])
                    P.op("dve", lambda e: e.tensor_tensor(st[:, :], st[:, :], mr[:, 1, :], ALU.mult), reads=[stb, mrb], writes=[stb])
                    so, sob = C.stbf.next()
                    P.op("act", lambda e: e.activation(so[:, :], st[:, :], AF.Silu, bias=C.pcol[:, 16 + cc:17 + cc], scale=C.pcol[:, 8 + cc:9 + cc]),
                         reads=[stb, cb], writes=[sob])
                    P.dma("sp", A.catT[cc * 128:(cc + 1) * 128, t0:t0 + 512], so[:, :], reads=[sob])
            P.barrier()

        with ExitStack() as ph:
            cqn, cqnb = sb(ph, nc, "cqn", [128, 4, S], BF16)
            ckvn, ckvnb = sb(ph, nc, "ckvn", [128, 4, S], BF16)
            rope, ropeb = sb(ph, nc, "rope", [64, 2, S], F32)
            sq = sbring(ph, nc, "sq4", [128, 512], F32, 3)
            C.panel = sbring(ph, nc, "p4pan", [128, 4, 512], BF16, 3)
            P.dma("sp", rope[:, 0, :], A.c_rope[0:64, :], writes=[ropeb], partial=True)
            P.dma("sp", rope[:, 1, :], A.c_rope[64:128, :], writes=[ropeb], partial=True)
            for (zrow, dst, dstb, gc0) in ((2048, cqn, cqnb, 24), (2560, ckvn, ckvnb, 28)):
              with ExitStack() as ph2:
                src, srcb = sb(ph2, nc, f"csrc{zrow}", [128, 4, S], F32)
                load_fm(P, C, src, srcb, A.zT, zrow, 4)
                for t0 in range(0, S, 512):
                    ps, pb = C.psf.next()
                    for c in range(4):
                        st, stb = sq.next()
                        P.op("act", lambda e: e.activation(st[:, :], src[:, c, t0:t0 + 512], AF.Square), reads=[srcb], writes=[stb])
                        P.op("pe", lambda e: e.matmul(ps[:, :], C.ones_f[:, :], st[:, :], start=(c == 0), stop=(c == 3)),
                             reads=[stb, cb], writes=[pb], partial=True)
                    rs, rsb = sq.next()
                    rstd_from_ss(P, rs[:, :], ps[:, :], 512, [pb], [rsb])
                    for c in range(4):
                        P.op("dve", lambda e: e.scalar_tensor_tensor(dst[:, c, t0:t0 + 512], src[:, c, t0:t0 + 512], C.pcol[:, gc0 + c:gc0 + c + 1],
                                                                     rs[:, :], ALU.mult, ALU.mult),
                             reads=[srcb, rsb, cb], writes=[dstb], partial=True)
                P.barrier()
            def rope_evac(x_ap, xp_ap, rd, t0, tw, dst_rows):
                a, ab = sq.next()
                b, bb = sq.next()
                P.op("dve", lambda e: e.tensor_tensor(a[0:64, 0:tw], x_ap, rope[:, 0, t0:t0 + tw], ALU.mult), reads=rd + [ropeb], writes=[ab])
                P.op("dve", lambda e: e.tensor_tensor(b[0:64, 0:tw], xp_ap, rope[:, 1, t0:t0 + tw], ALU.mult), reads=rd + [ropeb], writes=[bb])
                so, sob = C.stbf.next()
                P.op("dve", lambda e: e.tensor_tensor(so[0:64, 0:tw], a[0:64, 0:tw], b[0:64, 0:tw], ALU.add), reads=[ab, bb], writes=[sob])
                P.dma("sp", dst_rows[:, t0:t0 + tw], so[0:64, 0:tw], reads=[sob])
            with ExitStack() as ph2:
                kr, krb = sb(ph2, nc, "kr", [64, 2, S], F32)
                P.dma("sp", kr[:, 0, :], A.zT[3072:3136, :], writes=[krb], partial=True)
                P.dma("sp", kr[:, 1, :], A.zT[3136:3200, :], writes=[krb], partial=True)
                for t0 in range(0, S, 512):
                    rope_evac(kr[:, 0, t0:t0 + 512], kr[:, 1, t0:t0 + 512], [krb], t0, 512, A.kT[1024:1088, :])
                P.barrier()
            proj_fm(P, C, cqn, cqnb, 4, S, [A.ev_w_uq_n], 1024, 128, store_fm_bf16(P, C, A.qT, 0), C.panel)

            def ev_qr(ci, t0, tw, pss):
                (p1, b1), (p2, b2) = pss
                rope_evac(p1[0:64, 0:tw], p2[0:64, 0:tw], [b1, b2], t0, tw, A.qT[1024 + ci * 64:1024 + (ci + 1) * 64, :])
            proj_fm(P, C, cqn, cqnb, 4, S, [A.ev_w_uq_r, A.ev_w_uq_rp], 512, 64, ev_qr, C.panel)
            proj_fm(P, C, ckvn, ckvnb, 4, S, [A.ev_w_ukv_k], 1024, 128, store_fm_bf16(P, C, A.kT, 0), C.panel)

            def ev_v(tt, p0, pw, ps, pb):
                st, stb = C.stbf.next()
                evac_copy(P, C, st[:, 0:pw], ps[:, 0:pw], [pb], [stb], partial=False)
                P.dma("sp", A.vtm[tt * 128:(tt + 1) * 128, p0:p0 + pw], st[:, 0:pw], reads=[stb])
            proj_tm(P, C, ckvn, ckvnb, 4, S, A.ev_w_ukv_v, 1024, ev_v, C.panel)
            P.barrier()

        with ExitStack() as ph:
            qn = sbring(ph, nc, "a_qn", [128, S], BF16, 2)
            qr = sbring(ph, nc, "a_qr", [64, S], BF16, 2)
            kn = sbring(ph, nc, "a_kn", [128, S], BF16, 2)
            vv = sbring(ph, nc, "a_v", [128, 16, 128], BF16, 2)
            krt, krtb = sb(ph, nc, "a_kr", [64, S], BF16)
            osb = sbring(ph, nc, "a_o", [128, 128], F32, 2)
            P.dma("sp", krt[:, :], A.kT[1024:1088, :], writes=[krtb])
            sc = 192 ** -0.5
            for hh in range(8):
                qn_t, qn_b = qn.next()
                qr_t, qr_b = qr.next()
                kn_t, kn_b = kn.next()
                v_t, v_b = vv.next()
                P.dma("sp", qn_t[:, :], A.qT[hh * 128:(hh + 1) * 128, :], writes=[qn_b])
                P.dma("sp", qr_t[:, :], A.qT[1024 + hh * 64:1024 + (hh + 1) * 64, :], writes=[qr_b])
                P.dma("sp", kn_t[:, :], A.kT[hh * 128:(hh + 1) * 128, :], writes=[kn_b])
                P.dma("sp", v_t[:, :, :], A.vtm[:, hh * 128:(hh + 1) * 128].rearrange("(kc p) d -> p kc d", p=128), writes=[v_b])

                def s_terms(qt, c0, n):
                    q0 = qt * 128
                    terms = [(qn_t[:, q0:q0 + 128], kn_t[:, c0:c0 + n], 0, n),
                             (qr_t[:, q0:q0 + 128], krt[:, c0:c0 + n], 0, n)]
                    if c0 + n == q0 + 128:
                        terms.append((C.mrow[0:1, 0:128], C.mrow[0:1, 128:256], n - 128, 128))
                    return terms

                def out_cb(qt, o_ps, o_pb, rinv, rb):
                    ot, ob = osb.next()
                    P.op("dve", lambda e: e.tensor_scalar(ot[:, :], o_ps[:, 0:128], rinv, None, ALU.mult), reads=[o_pb, rb], writes=[ob])
                    out_T_store(P, C, ot, ob, 128, A.catT, 1024 + hh * 128, qt)
                with AttnPools(C):
                    attention(P, C, NT, lambda qt: (qt + 1) * 128, s_terms, lambda kb: v_t[:, kb, :], 128, sc, out_cb,
                              [qn_b, qr_b, kn_b, v_b, krtb, cb])
            P.barrier()

        with ExitStack() as ph:
            catS, catSb = sb(ph, nc, "catS", [128, 16, S], BF16)
            C.panel = sbring(ph, nc, "p6pan", [128, 16, 512], BF16, 2)
            load_fm(P, C, catS, catSb, A.catT, 0, 16)
            residual_linear(P, C, catS, catSb, 16, A.ev_w_out, A.h)
            P.barrier()
            if debug:
                P.dma("sp", A.dbg_h[0], A.h, reads=[])
                P.dma("sp", A.dbg_cat, A.catT, reads=[])
                P.barrier()
        with ExitStack() as ph:
            C.panel = sbring(ph, nc, "p7pan", [128, 16, 512], BF16, 2)
            cross_attention_layer(P, C, nc, A, 0)
            if debug:
                P.dma("sp", A.dbg_h[1], A.h, reads=[])
                P.barrier()
        ffn_dense(P, C, nc, A, A.norm_ffn_g[0:1, :], [(A.ev_ffn_wg, A.ev_ffn_wu, A.ev_ffn_wd)], DFF)
        if debug:
            P.dma("sp", A.dbg_h[2], A.h, reads=[])
            P.barrier()

        lambda_init = 0.8 - 0.6 * math.exp(-0.3 * 1)
        with ExitStack() as ph:
            hnT, hnTb = sb(ph, nc, "l1_hnT", [128, 16, S], BF16)
            C.panel = sbring(ph, nc, "l1pan", [128, 16, 512], BF16, 2)
            norm_T(P, C, A.h, S, A.norm_mix_g[1:2, :], hnT, hnTb)
            proj_fm(P, C, hnT, hnTb, 16, S, [A.od_w_in[:, 0:D]], D, 128, store_fm_bf16(P, C, A.qT, 0), C.panel)
            proj_fm(P, C, hnT, hnTb, 16, S, [A.od_w_in[:, D:2 * D]], D, 128, store_fm_bf16(P, C, A.kT, 0), C.panel)

            def ev_v1(tt, p0, pw, ps, pb):
                st, stb = C.stbf.next()
                evac_copy(P, C, st[:, 0:pw], ps[:, 0:pw], [pb], [stb], partial=False)
                P.dma("sp", A.vtm[tt * 128:(tt + 1) * 128, p0:p0 + pw], st[:, 0:pw], reads=[stb])
            proj_tm(P, C, hnT, hnTb, 16, S, A.od_w_in[:, 2 * D:3 * D], D, ev_v1, C.panel)
            P.barrier()

        with ExitStack() as ph:
            sc = 128 ** -0.5
            qh = sbring(ph, nc, "d_q", [128, S], BF16, 4)
            kh = sbring(ph, nc, "d_k", [128, S], BF16, 4)
            vv = sbring(ph, nc, "d_v", [128, 16, 256], BF16, 2)
            brev, brevb = sb(ph, nc, "brev", [128, 8, 256], F32)
            bhi, bhib = sb(ph, nc, "bhi", [128, 8, 256], BF16)
            blo, blob = sb(ph, nc, "blo", [128, 8, 256], BF16)
            mrev, mrevb = sb(ph, nc, "mrev", [128, 256], F32)
            tmpf = sbring(ph, nc, "d_tmp", [128, 256], F32, 8)
            sgb, sgbb = sb(ph, nc, "sgb", [128, 256], F32)
            lam, lamb = sb(ph, nc, "lam", [128, 8], F32)
            lrow, lrowb = sb(ph, nc, "lrow", [1, 4, 128], F32)
            rb32, rb32b = sb(ph, nc, "rb32", [32, 8], F32)
            oh, ohb = sb(ph, nc, "oh", [32, 384], F32)
            vrow, vrowb = sb(ph, nc, "vrow", [8, 384], F32)
            P.dma("sp", rb32[:, :], A.rel_bias, writes=[rb32b])
            P.dma("sp", oh[:, :], A.c_oh, writes=[ohb])
            P.dma("sp", mrev[:, :], A.c_mrev, writes=[mrevb])
            ps, pb = C.psf.next()
            P.op("pe", lambda e: e.matmul(ps[0:8, 0:384], rb32[:, :], oh[:, :], start=True, stop=True), reads=[rb32b, ohb], writes=[pb])
            P.op("dve", lambda e: e.tensor_copy(vrow[:, :], ps[0:8, 0:384]), reads=[pb], writes=[vrowb])
            P.dma("sp", A.tv[:, 0:384], vrow[:, :], reads=[vrowb], writes=[brevb])
            for hh in range(8):
                src = bass.AP(tensor=A.tv.tensor, offset=hh * 512, ap=[[1, 128], [1, 256]])
                P.dma("sp", brev[:, hh, :], src, reads=[brevb], writes=[brevb], partial=True)
            for hh in range(8):
                t1, t1b = tmpf.next()
                P.op("dve", lambda e: e.tensor_scalar(t1[:, :], brev[:, hh, :], brev[:, hh, 0:1], 1.0 / sc, ALU.subtract, ALU.mult),
                     reads=[brevb], writes=[t1b])
                P.op("dve", lambda e: e.tensor_tensor(t1[:, :], t1[:, :], mrev[:, :], ALU.add), reads=[t1b, mrevb], writes=[t1b])
                P.op("dve", lambda e: e.tensor_copy(bhi[:, hh, :], t1[:, :]), reads=[t1b], writes=[bhib], partial=True)
                t2, t2b = tmpf.next()
                P.op("dve", lambda e: e.tensor_copy(t2[:, :], bhi[:, hh, :]), reads=[bhib], writes=[t2b])
                P.op("dve", lambda e: e.tensor_tensor(blo[:, hh, :], t1[:, :], t2[:, :], ALU.subtract), reads=[t1b, t2b], writes=[blob], partial=True)
            P.dma("sp", lrow[0:1, :, :], A.od_lam.rearrange("(o a) d -> o a d", o=1), writes=[lrowb])
            P.op("dve", lambda e: e.tensor_tensor(lrow[0:1, 0, :], lrow[0:1, 0, :], lrow[0:1, 1, :], ALU.mult), reads=[lrowb], writes=[lrowb])
            P.op("dve", lambda e: e.tensor_tensor(lrow[0:1, 2, :], lrow[0:1, 2, :], lrow[0:1, 3, :], ALU.mult), reads=[lrowb], writes=[lrowb])
            P.op("dve", lambda e: e.reduce_sum(lrow[0:1, 1, 0:1], lrow[0:1, 0, :], AX.X), reads=[lrowb], writes=[lrowb])
            P.op("dve", lambda e: e.reduce_sum(lrow[0:1, 1, 1:2], lrow[0:1, 2, :], AX.X), reads=[lrowb], writes=[lrowb])
            P.op("act", lambda e: e.activation(lrow[0:1, 1, 2:4], lrow[0:1, 1, 0:2], AF.Exp), reads=[lrowb], writes=[lrowb])
            P.op("dve", lambda e: e.tensor_tensor(lrow[0:1, 1, 4:5], lrow[0:1, 1, 3:4], lrow[0:1, 1, 2:3], ALU.subtract), reads=[lrowb], writes=[lrowb])
            P.op("dve", lambda e: e.tensor_scalar(lrow[0:1, 1, 4:5], lrow[0:1, 1, 4:5], -lambda_init, None, ALU.add), reads=[lrowb], writes=[lrowb])
            ps, pb = C.psf.next()
            P.op("pe", lambda e: e.matmul(ps[:, 0:1], C.ones_f[0:1, 0:128], lrow[0:1, 1, 4:5], start=True, stop=True), reads=[lrowb, cb], writes=[pb])
            P.op("dve", lambda e: e.tensor_copy(lam[:, 0:1], ps[:, 0:1]), reads=[pb], writes=[lamb])
            bcast_row(P, C, A.od_subln_g, 256, sgb, sgbb, mul=(1.0 - lambda_init))
            for hh in range(8):
                q_t = [qh.next(), qh.next()]
                k_t = [kh.next(), kh.next()]
                v_t, v_b = vv.next()
                for c in range(2):
                    P.dma("sp", q_t[c][0][:, :], A.qT[(hh * 2 + c) * 128:(hh * 2 + c + 1) * 128, :], writes=[q_t[c][1]])
                    P.dma("sp", k_t[c][0][:, :], A.kT[(hh * 2 + c) * 128:(hh * 2 + c + 1) * 128, :], writes=[k_t[c][1]])
                P.dma("sp", v_t[:, :, :], A.vtm[:, hh * 256:(hh + 1) * 256].rearrange("(kc p) d -> p kc d", p=128), writes=[v_b])
                o0 = {}

                def make_spec(c, qq, qb_, kk, kb_):
                    def s_terms(qt, c0, n):
                        q0 = qt * 128
                        terms = [(qq[:, q0:q0 + 128], kk[:, c0:c0 + n], 0, n)]
                        for (kb0, col0) in ((q0 - 128, 0), (q0, 128)):
                            if kb0 >= c0 and kb0 < c0 + n:
                                for bt in (bhi, blo):
                                    terms.append((C.anti_b[:, :], bt[:, hh, col0:col0 + 128], kb0 - c0, 128))
                        return terms

                    def out_cb(qt, o_ps, o_pb, rinv, rb):
                        if c == 0:
                            t0_, t0b = tmpf.next()
                            P.op("dve", lambda e: e.tensor_scalar(t0_[:, :], o_ps[:, 0:256], rinv, None, ALU.mult), reads=[o_pb, rb], writes=[t0b])
                            o0[qt] = (t0_, t0b)
                        else:
                            t0_, t0b = o0[qt]
                            P.op("dve", lambda e: e.tensor_tensor(lam[:, 1:2], rinv, lam[:, 0:1], ALU.mult), reads=[rb, lamb], writes=[lamb])
                            P.op("dve", lambda e: e.scalar_tensor_tensor(t0_[:, :], o_ps[:, 0:256], lam[:, 1:2], t0_[:, :], ALU.mult, ALU.add),
                                 reads=[o_pb, lamb, t0b], writes=[t0b])
                            jk, jkb = tmpf.next()
                            ss, ssb = C.ssring.next()
                            P.op("act", lambda e: e.activation(jk[:, :], t0_[:, :], AF.Square, accum_out=ss[:, 0:1]), reads=[t0b], writes=[jkb, ssb])
                            rstd_from_ss(P, ss[:, 1:2], ss[:, 0:1], 256, [ssb], [ssb])
                            P.op("dve", lambda e: e.scalar_tensor_tensor(t0_[:, :], t0_[:, :], ss[:, 1:2], sgb[:, :], ALU.mult, ALU.mult),
                                 reads=[t0b, ssb, sgbb], writes=[t0b])
                            out_T_store(P, C, t0_, t0b, 256, A.catT, hh * 256, qt)
                    return s_terms, out_cb, [qb_, kb_, v_b, bhib, blob, cb]
                specs = [make_spec(c, q_t[c][0], q_t[c][1], k_t[c][0], k_t[c][1]) for c in range(2)]
                def gens():
                    for qt in range(NT):
                        for c in range(2):
                            s_terms, out_cb, bufs = specs[c]
                            yield attention_gen(P, C, qt, (qt + 1) * 128, s_terms, lambda kb: v_t[:, kb, :], 256, sc, out_cb, bufs)
                with AttnPools(C):
                    run_gens(gens(), 2)
            P.barrier()

        with ExitStack() as ph:
            catS, catSb = sb(ph, nc, "catS1", [128, 16, S], BF16)
            C.panel = sbring(ph, nc, "p11pan", [128, 16, 512], BF16, 2)
            load_fm(P, C, catS, catSb, A.catT, 0, 16)
            residual_linear(P, C, catS, catSb, 16, A.od_w_out, A.h)
            P.barrier()
            if debug:
                P.dma("sp", A.dbg_h[3], A.h, reads=[])
                P.barrier()
        with ExitStack() as ph:
            C.panel = sbring(ph, nc, "p12pan", [128, 16, 512], BF16, 2)
            cross_attention_layer(P, C, nc, A, 1)
            if debug:
                P.dma("sp", A.dbg_h[4], A.h, reads=[])
                P.barrier()
        experts = [(A.od_moe_wg[e], A.od_moe_wu[e], A.od_moe_wd[e]) for e in range(NEXP)]
        P.barrier()
        cs.close()
        regs = nc.alloc_registers("moe_cnt", engines=list(nc.engines.keys()))
        moe_sparse(P, C, nc, A, experts, regs)
        P.barrier()
    nc._marks = P.marks
    return nc


def _t5_bucket(rel):
    half, max_exact = 16, 8
    ret = (rel > 0).astype(np.int32) * half
    n = np.abs(rel)
    nf = np.maximum(n, 1).astype(np.float32)
    large = max_exact + (np.log(nf / max_exact) / math.log(128 / max_exact) * (half - max_exact)).astype(np.int32)
    large = np.minimum(large, half - 1)
    return ret + np.where(n < max_exact, n, large)


def host_constants():
    c = {}
    c["c_ident"] = np.eye(128, dtype=np.float32)
    c["c_anti"] = np.ascontiguousarray(np.eye(128, dtype=np.float32)[::-1])
    pos = np.arange(S, dtype=np.float32)
    inv = np.power(np.float32(10000.0), -np.arange(0, 64, 2, dtype=np.float32) / np.float32(64)).astype(np.float32)
    ang = (pos[None, :] * inv[:, None]).astype(np.float32)
    cs, sn = np.cos(ang).astype(np.float32), np.sin(ang).astype(np.float32)
    c["c_rope"] = np.concatenate([cs, cs, -sn, sn], axis=0).astype(np.float32)
    rel = np.arange(384, dtype=np.int32) - 255
    bk = _t5_bucket(rel)
    oh = np.zeros((32, 384), np.float32)
    oh[bk, np.arange(384)] = 1.0
    oh[:, 383] = 0.0
    c["c_oh"] = oh
    m = np.zeros((128, 256), np.float32)
    m[64:128, 192:256] = NEGM
    c["c_mrev"] = m
    mr = np.zeros((2, 128), np.float32)
    mr[0, 0:64] = 1.0
    mr[1, 64:128] = NEGM
    c["c_mrow"] = mr
    c["c_iota"] = np.ascontiguousarray(np.broadcast_to(np.arange(512, dtype=np.float32)[None, :], (128, 512)))
    c["c_utri"] = np.triu(np.ones((128, 128), np.float32), k=1)
    return c


def host_layout(inp):
    g = {}
    f = lambda a: np.ascontiguousarray(a, dtype=np.float32)
    g["rel_bias"] = f(inp["rel_bias"])
    g["mem_norm_g"] = f(inp["mem_norm_g"]).reshape(1, D)
    for k in ("norm_mix_g", "norm_cross_g", "norm_ffn_g", "cross_wq", "cross_wkv", "cross_wo"):
        g[k] = f(inp[k])
    w_in = f(inp["ev_w_in"][0])
    g["ev_w_in"] = w_in
    kr = w_in[:, 3072:3136]
    g["ev_w_in_krp"] = f(np.concatenate([kr[:, 32:64], kr[:, 0:32]], axis=1))
    g["ev_conv_w"] = f(inp["ev_conv_w"][0]).reshape(248, 128)
    g["pvec"] = f(np.concatenate([inp["ev_conv_b"][0].reshape(8, 128), inp["ev_ln_g"][0].reshape(8, 128),
                                  inp["ev_ln_b"][0].reshape(8, 128), inp["ev_q_norm_g"][0].reshape(4, 128),
                                  inp["ev_kv_norm_g"][0].reshape(4, 128)], axis=0))
    uq = f(inp["ev_w_uq"][0]).reshape(512, 8, 192)
    g["ev_w_uq_n"] = f(uq[:, :, 0:128].reshape(512, 1024))
    g["ev_w_uq_r"] = f(uq[:, :, 128:192].reshape(512, 512))
    g["ev_w_uq_rp"] = f(np.concatenate([uq[:, :, 160:192], uq[:, :, 128:160]], axis=2).reshape(512, 512))
    ukv = f(inp["ev_w_ukv"][0]).reshape(512, 8, 256)
    g["ev_w_ukv_k"] = f(ukv[:, :, 0:128].reshape(512, 1024))
    g["ev_w_ukv_v"] = f(ukv[:, :, 128:256].reshape(512, 1024))
    g["ev_w_out"] = f(inp["ev_w_out"][0])
    g["ev_ffn_wg"] = f(inp["ev_ffn_wg"][0])
    g["ev_ffn_wu"] = f(inp["ev_ffn_wu"][0])
    g["ev_ffn_wd"] = f(inp["ev_ffn_wd"][0])
    g["od_w_in"] = f(inp["od_w_in"][0])
    g["od_lam"] = f(np.concatenate([inp["od_lambda_q1"], inp["od_lambda_k1"], inp["od_lambda_q2"], inp["od_lambda_k2"]], axis=0))
    g["od_subln_g"] = f(inp["od_subln_g"]).reshape(1, 256)
    g["od_w_out"] = f(inp["od_w_out"][0])
    g["od_router"] = f(inp["od_router"][0])
    g["od_moe_wg"] = f(inp["od_moe_wg"][0])
    g["od_moe_wu"] = f(inp["od_moe_wu"][0])
    g["od_moe_wd"] = f(inp["od_moe_wd"][0])
    g["final_norm_g"] = f(inp["final_norm_g"]).reshape(1, D)
    g.update(host_constants())
    return g


def kernel(**inputs):
    n = 8
    shared = host_layout(inputs)
    x = np.ascontiguousarray(inputs["x"], dtype=np.float32)
    mem = np.ascontiguousarray(inputs["mem"], dtype=np.float32)
    nc = build_program()
    in_maps = []
    for b in range(n):
        m = dict(shared)
        m["x"] = x[b]
        m["mem"] = mem[b]
        in_maps.append(m)
    res = run_bass_kernel_spmd(nc, in_maps, core_ids=list(range(n)))
    return np.stack([np.asarray(r["out"], dtype=np.float32) for r in res.results], axis=0)
```
